# Optimizing a Trainium2 kernel written in Bass

```python
import jax
import jax.numpy as jnp
from jax import lax
import numpy as np


D_MODEL = 1024
BATCH = 4
SEQ = 8192
DEPTH = 4

GRID_W = 64
CTX_LEN = 256
HEAD_DIM = 64
D_MIX = D_MODEL
CONV_HEADS = 4
CONV_W = CONV_HEADS * HEAD_DIM
CONV_K = 3
SG_HEADS = 4
SG_W = SG_HEADS * HEAD_DIM
SG_CHUNK = 128
NA_HEADS = 4
NA_W = NA_HEADS * HEAD_DIM
NA_KH_MAX = 8
NA_KW = 16
MLA_HEADS = 4
MLA_NOPE = 64
MLA_ROPE = 32
MLA_V = 64
MLA_Q_RANK = 256
MLA_KV_RANK = 128
MLA_W = MLA_HEADS * MLA_V
MLA_SCALE = (MLA_NOPE + MLA_ROPE) ** -0.5
ATTN_QBLOCK = 128
ROPE_BASE = 10000.0
IN_CONV = 3 * CONV_W
IN_SG = 2 * SG_W
IN_NA = 3 * NA_W
IN_MLA = MLA_Q_RANK + MLA_KV_RANK + MLA_ROPE
SG0 = IN_CONV
NA0 = SG0 + IN_SG
MLA0 = NA0 + IN_NA
IN_COLS = MLA0 + IN_MLA
N_GROUPS = 4
EXPERTS_PER_GROUP = 8
N_EXPERTS = N_GROUPS * EXPERTS_PER_GROUP
TOP_K_INNER = 2
D_EXPERT = 512
MOE_BLOCK = 128
N_MOD = 6
EPS = 1e-6

kernel_name = 'hybrid_flow_backbone'


def rms_norm(x, g):
    xf = x.astype(jnp.float32)
    y = xf * lax.rsqrt(jnp.mean(xf * xf, axis=-1, keepdims=True) + EPS)
    return (y * g.astype(jnp.float32)).astype(x.dtype)


def layer_norm_plain(x):
    xf = x.astype(jnp.float32)
    mu = jnp.mean(xf, axis=-1, keepdims=True)
    var = jnp.mean(jnp.square(xf - mu), axis=-1, keepdims=True)
    return ((xf - mu) * lax.rsqrt(var + EPS)).astype(x.dtype)


def modulate(h, shift, scale):
    return h * (1.0 + scale) + shift


def split_heads(t, n_heads):
    return t.reshape(t.shape[:-1] + (n_heads, t.shape[-1] // n_heads))


def axial_rope(n_tok):
    t = jnp.arange(n_tok)
    row = (t // GRID_W).astype(jnp.float32)
    col = (t % GRID_W).astype(jnp.float32)
    n_freq = MLA_ROPE // 4
    inv_freq = ROPE_BASE ** (-jnp.arange(n_freq, dtype=jnp.float32) / n_freq)
    ang_r = row[:, None] * inv_freq
    ang_c = col[:, None] * inv_freq
    ang = jnp.concatenate([ang_r, ang_r, ang_c, ang_c], axis=-1)
    return jnp.cos(ang), jnp.sin(ang)


def apply_rope(x, cos, sin):
    x0, x1, x2, x3 = jnp.split(x, 4, axis=-1)
    rot = jnp.concatenate([-x1, x0, -x3, x2], axis=-1)
    return (x.astype(jnp.float32) * cos + rot.astype(jnp.float32) * sin).astype(x.dtype)


def short_conv_mixer(z, conv_w):
    b_gate, c_gate, hx = jnp.split(z, 3, axis=-1)
    u = c_gate * hx
    y = lax.conv_general_dilated(
        u, conv_w[:, None, :].astype(u.dtype), window_strides=(1,),
        padding=((CONV_K // 2, CONV_K // 2),), dimension_numbers=('NWC', 'WIO', 'NWC'),
        feature_group_count=CONV_W)
    return b_gate * y


def chunk_gating_mixer(z, sg_w, sg_b):
    bsz, n, _ = z.shape
    u, v = jnp.split(jax.nn.gelu(z), 2, axis=-1)
    v = layer_norm_plain(v).reshape(bsz, n // SG_CHUNK, SG_CHUNK, SG_HEADS, HEAD_DIM)
    v = jnp.einsum('hpq,bcqhd->bcphd', sg_w, v) + sg_b.T[None, None, :, :, None]
    return u * v.reshape(bsz, n, SG_W)


def neighborhood_attention(q, k, v, k_ctx, v_ctx, rpb):
    bsz, n, n_h, dh = q.shape
    rows = n // GRID_W
    kh = min(NA_KH_MAX, rows)
    kw = NA_KW
    scale = dh ** -0.5
    r = jnp.arange(rows)
    band = jnp.clip(r - kh // 2, 0, rows - kh)[:, None] + jnp.arange(kh)[None, :]
    col = jnp.arange(GRID_W)
    c0 = jnp.clip(col - kw // 2, 0, GRID_W - kw)
    in_win = (col[None, :] >= c0[:, None]) & (col[None, :] < c0[:, None] + kw)
    dr = band - r[:, None] + (NA_KH_MAX - 1)
    dc = jnp.clip(col[None, :] - col[:, None] + (kw - 1), 0, 2 * kw - 2)
    bias = rpb.astype(jnp.float32)[:, dr[:, None, :, None], dc[None, :, None, :]]
    bias = jnp.where(in_win[None, None, :, None, :], bias, -jnp.inf)
    qg = q.reshape(bsz, rows, GRID_W, n_h, dh)
    k_band = k.reshape(bsz, rows, GRID_W, n_h, dh)[:, band]
    v_band = v.reshape(bsz, rows, GRID_W, n_h, dh)[:, band]
    s_loc = jnp.einsum('brqhd,brjkhd->bhrqjk', qg, k_band).astype(jnp.float32) * scale + bias[None]
    s_ctx = jnp.einsum('brqhd,bchd->bhrqc', qg, k_ctx).astype(jnp.float32) * scale
    n_loc = kh * GRID_W
    s = jnp.concatenate([s_loc.reshape(bsz, n_h, rows, GRID_W, n_loc), s_ctx], axis=-1)
    p = jax.nn.softmax(s, axis=-1).astype(v.dtype)
    p_loc = p[..., :n_loc].reshape(bsz, n_h, rows, GRID_W, kh, GRID_W)
    o = (jnp.einsum('bhrqjk,brjkhd->brqhd', p_loc, v_band)
         + jnp.einsum('bhrqc,bchd->brqhd', p[..., n_loc:], v_ctx))
    return o.reshape(bsz, n, n_h * dh)


def dense_attention(q, k, v):
    s = jnp.einsum('bqhd,bkhd->bhqk', q, k).astype(jnp.float32) * (q.shape[-1] ** -0.5)
    p = jax.nn.softmax(s, axis=-1).astype(v.dtype)
    return jnp.einsum('bhqk,bkhd->bqhd', p, v)


def mla_q(cq, q_norm_g, w_uq):
    q = split_heads(rms_norm(cq, q_norm_g) @ w_uq, MLA_HEADS)
    return q[..., :MLA_NOPE], q[..., MLA_NOPE:]


def mla_kv(ckv_kr, kv_norm_g, w_ukv):
    ckv, k_rope = ckv_kr[..., :MLA_KV_RANK], ckv_kr[..., MLA_KV_RANK:]
    kv = split_heads(rms_norm(ckv, kv_norm_g) @ w_ukv, MLA_HEADS)
    return kv[..., :MLA_NOPE], k_rope, kv[..., MLA_NOPE:]


def mla_attend(q_nope, q_rope, k_nope, k_rope, v):
    s = (jnp.einsum('bqhd,bkhd->bhqk', q_nope, k_nope)
         + jnp.einsum('bqhr,bkr->bhqk', q_rope, k_rope)).astype(jnp.float32) * MLA_SCALE
    p = jax.nn.softmax(s, axis=-1).astype(v.dtype)
    return jnp.einsum('bhqk,bkhd->bqhd', p, v)


def mla_blocked(q_nope, q_rope, k_nope, k_rope, v):
    bsz, n = q_nope.shape[:2]
    nb = n // ATTN_QBLOCK

    def to_blocks(t):
        return jnp.moveaxis(t.reshape((bsz, nb, ATTN_QBLOCK) + t.shape[2:]), 1, 0)

    o = lax.map(lambda qs: mla_attend(qs[0], qs[1], k_nope, k_rope, v),
                (to_blocks(q_nope), to_blocks(q_rope)))
    return jnp.moveaxis(o, 0, 1).reshape(bsz, n, MLA_W)


def merge_groups(ys, out_norm_g, w_out):
    normed = []
    off = 0
    for y in ys:
        w = y.shape[-1]
        normed.append(rms_norm(y, out_norm_g[off:off + w]))
        off += w
    return jnp.concatenate(normed, axis=-1) @ w_out


def hier_moe(h, w_grp, b_grp, w_exp, b_exp, w_gate, w_up, w_down):
    n_tok, d = h.shape
    hf = h.astype(jnp.float32)
    grp_logits = hf @ w_grp.astype(jnp.float32) + b_grp.astype(jnp.float32)
    grp_prob = jax.nn.softmax(grp_logits, axis=-1)
    _, g_sel = lax.top_k(grp_logits, 1)
    p_grp = jnp.take_along_axis(grp_prob, g_sel, axis=-1)
    exp_logits = (hf @ w_exp.astype(jnp.float32) + b_exp.astype(jnp.float32)).reshape(
        n_tok, N_GROUPS, EXPERTS_PER_GROUP)
    idx = jnp.broadcast_to(g_sel[:, :, None], (n_tok, 1, EXPERTS_PER_GROUP))
    in_grp = jnp.take_along_axis(exp_logits, idx, axis=1)[:, 0]
    top_v, top_i = lax.top_k(in_grp, TOP_K_INNER)
    gates = p_grp * jax.nn.softmax(top_v, axis=-1)
    expert_id = (g_sel * EXPERTS_PER_GROUP + top_i).reshape(-1)
    n_asg = n_tok * TOP_K_INNER
    order = jnp.argsort(expert_id)
    sorted_e = expert_id[order]
    counts = jnp.bincount(expert_id, length=N_EXPERTS)
    start = jnp.cumsum(counts) - counts
    padded = (counts + MOE_BLOCK - 1) // MOE_BLOCK * MOE_BLOCK
    padded_end = jnp.cumsum(padded)
    padded_start = padded_end - padded
    dest_sorted = padded_start[sorted_e] + (jnp.arange(n_asg) - start[sorted_e])
    n_blk = -(-n_asg // MOE_BLOCK) + N_EXPERTS
    cap = n_blk * MOE_BLOCK
    row_tok = jnp.full((cap,), n_tok, jnp.int32).at[dest_sorted].set(
        (order // TOP_K_INNER).astype(jnp.int32))
    h_pad = jnp.concatenate([h, jnp.zeros((1, d), h.dtype)], axis=0)
    x_buf = h_pad[row_tok].reshape(n_blk, MOE_BLOCK, d)
    blk_expert = jnp.clip(jnp.searchsorted(padded_end, jnp.arange(n_blk) * MOE_BLOCK, side='right'),
                          0, N_EXPERTS - 1)

    def expert_block(args):
        xb, e = args
        return (jax.nn.silu(xb @ w_gate[e]) * (xb @ w_up[e])) @ w_down[e]

    y_buf = lax.map(expert_block, (x_buf, blk_expert)).reshape(cap, d)
    dest = jnp.zeros((n_asg,), dest_sorted.dtype).at[order].set(dest_sorted)
    y = y_buf[dest].reshape(n_tok, TOP_K_INNER, d).astype(jnp.float32) * gates[..., None]
    return jnp.sum(y, axis=1).astype(h.dtype)


def trunk_layer(x, xc, c_act, c_ctx_act, cos, sin, w_ada, b_ada, norm1_g, norm2_g, w_in,
                conv_w, sg_w, sg_b, na_rpb, mla_q_norm_g, mla_w_uq, mla_kv_norm_g, mla_w_ukv,
                out_norm_g, w_out, w_grp, b_grp, w_exp, b_exp, w_gate, w_up, w_down, update_ctx):
    bsz, n_lat, d = x.shape
    n_ctx = xc.shape[1]
    mod = c_act @ w_ada + b_ada
    mod_c = c_ctx_act @ w_ada + b_ada
    sh1, sc1, g1, sh2, sc2, g2 = jnp.split(mod[:, None, :], N_MOD, axis=-1)
    csh1, csc1, cg1, csh2, csc2, cg2 = jnp.split(mod_c, N_MOD, axis=-1)

    h = modulate(rms_norm(x, norm1_g), sh1, sc1)
    hc = modulate(rms_norm(xc, norm1_g), csh1, csc1)

    z = h @ w_in
    za, zb, zn, zd = jnp.split(z, [SG0, NA0, MLA0], axis=-1)
    if update_ctx:
        zc = hc @ w_in
        zca, zcb, zcn, zcd = jnp.split(zc, [SG0, NA0, MLA0], axis=-1)
        zcn_kv, zcd_kv = zcn[..., NA_W:], zcd[..., MLA_Q_RANK:]
    else:
        zcn_kv = hc @ w_in[:, NA0 + NA_W:MLA0]
        zcd_kv = hc @ w_in[:, MLA0 + MLA_Q_RANK:]

    ya = short_conv_mixer(za, conv_w)
    yb = chunk_gating_mixer(zb, sg_w, sg_b)
    q_na, k_na, v_na = [split_heads(t, NA_HEADS) for t in jnp.split(zn, 3, axis=-1)]
    kc_na, vc_na = [split_heads(t, NA_HEADS) for t in jnp.split(zcn_kv, 2, axis=-1)]
    yc = neighborhood_attention(q_na, k_na, v_na, kc_na, vc_na, na_rpb)
    q_nope, q_rope = mla_q(zd[..., :MLA_Q_RANK], mla_q_norm_g, mla_w_uq)
    q_rope = apply_rope(q_rope, cos[:, None, :], sin[:, None, :])
    k_nope, k_rope, v_m = mla_kv(zd[..., MLA_Q_RANK:], mla_kv_norm_g, mla_w_ukv)
    k_rope = apply_rope(k_rope, cos, sin)
    ck_nope, ck_rope, cv_m = mla_kv(zcd_kv, mla_kv_norm_g, mla_w_ukv)
    yd = mla_blocked(q_nope, q_rope,
                     jnp.concatenate([k_nope, ck_nope], axis=1),
                     jnp.concatenate([k_rope, ck_rope], axis=1),
                     jnp.concatenate([v_m, cv_m], axis=1))
    x = x + g1 * merge_groups([ya, yb, yc, yd], out_norm_g, w_out)
    h2 = modulate(rms_norm(x, norm2_g), sh2, sc2)
    if not update_ctx:
        f = hier_moe(h2.reshape(-1, d), w_grp, b_grp, w_exp, b_exp, w_gate, w_up, w_down)
        return x + g2 * f.reshape(bsz, n_lat, d), xc

    yac = short_conv_mixer(zca, conv_w)
    ybc = chunk_gating_mixer(zcb, sg_w, sg_b)
    ycc = dense_attention(split_heads(zcn[..., :NA_W], NA_HEADS), kc_na, vc_na).reshape(bsz, n_ctx, NA_W)
    cq_nope, cq_rope = mla_q(zcd[..., :MLA_Q_RANK], mla_q_norm_g, mla_w_uq)
    ydc = mla_attend(cq_nope, cq_rope, ck_nope, ck_rope, cv_m).reshape(bsz, n_ctx, MLA_W)
    xc = xc + cg1 * merge_groups([yac, ybc, ycc, ydc], out_norm_g, w_out)
    h2c = modulate(rms_norm(xc, norm2_g), csh2, csc2)
    f = hier_moe(jnp.concatenate([h2.reshape(-1, d), h2c.reshape(-1, d)], axis=0),
                 w_grp, b_grp, w_exp, b_exp, w_gate, w_up, w_down)
    x = x + g2 * f[:bsz * n_lat].reshape(bsz, n_lat, d)
    xc = xc + cg2 * f[bsz * n_lat:].reshape(bsz, n_ctx, d)
    return x, xc


def setup_inputs(seed: int = 0) -> dict:
    key = jax.random.key(seed)
    ks = jax.random.split(key, 27)
    L, D = DEPTH, D_MODEL

    def nrm(i, shape, s):
        return jax.random.normal(ks[i], shape, jnp.float32) * s

    def gain(i, shape):
        return 1.0 + nrm(i, shape, 0.1)

    return {
        'x': nrm(0, (BATCH, SEQ, D), 1.0),
        'c': nrm(1, (BATCH, D), 1.0),
        'ctx': nrm(2, (BATCH, CTX_LEN, D), 1.0),
        'c_ctx': nrm(3, (D,), 1.0),
        'w_ada': nrm(4, (L, D, N_MOD * D), 0.5 * D ** -0.5),
        'b_ada': nrm(5, (L, N_MOD * D), 0.02),
        'norm1_g': gain(6, (L, D)),
        'norm2_g': gain(7, (L, D)),
        'w_in': nrm(8, (L, D, IN_COLS), D ** -0.5),
        'conv_w': nrm(9, (L, CONV_K, CONV_W), CONV_K ** -0.5),
        'sg_w': nrm(10, (L, SG_HEADS, SG_CHUNK, SG_CHUNK), SG_CHUNK ** -0.5),
        'sg_b': gain(11, (L, SG_HEADS, SG_CHUNK)),
        'na_rpb': nrm(12, (L, NA_HEADS, 2 * NA_KH_MAX - 1, 2 * NA_KW - 1), 0.2),
        'mla_q_norm_g': gain(13, (L, MLA_Q_RANK)),
        'mla_w_uq': nrm(14, (L, MLA_Q_RANK, MLA_HEADS * (MLA_NOPE + MLA_ROPE)), MLA_Q_RANK ** -0.5),
        'mla_kv_norm_g': gain(15, (L, MLA_KV_RANK)),
        'mla_w_ukv': nrm(16, (L, MLA_KV_RANK, MLA_HEADS * (MLA_NOPE + MLA_V)), MLA_KV_RANK ** -0.5),
        'out_norm_g': gain(17, (L, D_MIX)),
        'w_out': nrm(18, (L, D_MIX, D), D_MIX ** -0.5),
        'w_grp': nrm(19, (L, D, N_GROUPS), D ** -0.5),
        'b_grp': nrm(20, (L, N_GROUPS), 0.01),
        'w_exp': nrm(21, (L, D, N_EXPERTS), D ** -0.5),
        'b_exp': nrm(22, (L, N_EXPERTS), 0.01),
        'w_gate': nrm(23, (L, N_EXPERTS, D, D_EXPERT), D ** -0.5),
        'w_up': nrm(24, (L, N_EXPERTS, D, D_EXPERT), D ** -0.5),
        'w_down': nrm(25, (L, N_EXPERTS, D_EXPERT, D), D_EXPERT ** -0.5),
        'final_norm_g': gain(26, (D,)),
    }


def reference(x, c, ctx, c_ctx, w_ada, b_ada, norm1_g, norm2_g, w_in, conv_w, sg_w, sg_b, na_rpb,
              mla_q_norm_g, mla_w_uq, mla_kv_norm_g, mla_w_ukv, out_norm_g, w_out,
              w_grp, b_grp, w_exp, b_exp, w_gate, w_up, w_down, final_norm_g):
    cos, sin = axial_rope(x.shape[1])
    c_act = jax.nn.silu(c)
    c_ctx_act = jax.nn.silu(c_ctx)
    xc = ctx
    for l in range(DEPTH):
        x, xc = trunk_layer(
            x, xc, c_act, c_ctx_act, cos, sin, w_ada[l], b_ada[l], norm1_g[l], norm2_g[l], w_in[l],
            conv_w[l], sg_w[l], sg_b[l], na_rpb[l], mla_q_norm_g[l], mla_w_uq[l], mla_kv_norm_g[l],
            mla_w_ukv[l], out_norm_g[l], w_out[l], w_grp[l], b_grp[l], w_exp[l], b_exp[l],
            w_gate[l], w_up[l], w_down[l], update_ctx=(l < DEPTH - 1))
    return rms_norm(x, final_norm_g)
```

```python
import numpy as np
import ml_dtypes
from contextlib import ExitStack
import concourse.bass as bass
import concourse.mybir as mybir
from concourse.bass_utils import run_bass_kernel_spmd

F32 = mybir.dt.float32
BF16 = mybir.dt.bfloat16
I32 = mybir.dt.int32
AF = mybir.ActivationFunctionType
ALU = mybir.AluOpType
AX = mybir.AxisListType

D = 1024
KC = 8
IN_COLS = 2464
SG0, NA0, MLA0 = 768, 1280, 2048
NE = 32
DE = 512
BLK = 512
EPS = 1e-6
MLA_SCALE = 96.0 ** -0.5
NEG = -30000.0
SAME_ENG_SYNC = True


class Buf:
    __slots__ = ("name", "writers", "readers", "gen", "excl")

    def __init__(self, name):
        self.name = name
        self.excl = (len(name) > 1 and name[0] == "p" and (name[1].isupper() or name[1] == "m"))
        self.writers = []
        self.readers = []
        self.gen = []


class Op:
    __slots__ = ("eng", "fn", "deps", "is_dma", "dbuf", "signal", "sem", "count")

    def __init__(self, eng, fn, is_dma, dbuf):
        self.eng = eng
        self.fn = fn
        self.deps = []
        self.is_dma = is_dma
        self.dbuf = dbuf
        self.signal = is_dma
        self.sem = None
        self.count = 0


ENGS = ("sp", "act", "dve", "pool", "pe")


class Prog:
    def __init__(self, nc, es, n_dsem=84):
        self.nc = nc
        self.csem = {e: es.enter_context(nc.semaphore("c_" + e)) for e in ("act", "dve", "pool", "pe")}
        self.ccount = {e: 0 for e in self.csem}
        n_sw = 24
        self.dsem = {False: [es.enter_context(nc.semaphore("d%d" % i)) for i in range(n_dsem - n_sw)],
                     True: [es.enter_context(nc.semaphore("w%d" % i)) for i in range(n_sw)]}
        self.dcount = {False: [0] * (n_dsem - n_sw), True: [0] * n_sw}
        self.waited = {e: {} for e in ENGS}
        self.nstage = 0
        self.begin()

    def begin(self):
        self.ops = {e: [] for e in ENGS}
        self.dmap = {}

    def buf(self, name):
        return Buf(name)

    def bufs(self, name, n):
        return [Buf("%s%d" % (name, i)) for i in range(n)]

    def add(self, eng, fn, reads=(), writes=(), dbuf=None, nowaw=False):
        op = Op(eng, fn, dbuf is not None, dbuf)
        deps = []
        xr = [b for b in reads if b.excl]
        reads = [b for b in reads if not b.excl]
        for b in reads:
            deps.extend(b.writers)
        newgen = []
        for b in xr:
            g = list(b.writers) + list(b.readers)
            deps.extend(g)
            newgen.append((b, g))
        writes = tuple(writes) + tuple(xr)
        for b in writes:
            if b in xr:
                continue
            if nowaw and not b.readers and b.writers:
                deps.extend(b.gen)
            else:
                g = list(b.writers) + list(b.readers)
                deps.extend(g)
                newgen.append((b, g))
        seen = set()
        for d in deps:
            if id(d) in seen:
                continue
            seen.add(id(d))
            if d.eng == "pe" and eng == "pe" and not d.is_dma and dbuf is None:
                continue
            if (not SAME_ENG_SYNC) and d.eng == eng and not d.is_dma and dbuf is None:
                continue
            d.signal = True
            op.deps.append(d)
        for b in reads:
            b.readers.append(op)
        ng = {id(b): g for b, g in newgen}
        for b in writes:
            if id(b) in ng:
                b.gen = ng[id(b)]
                b.writers = [op]
                b.readers = []
            else:
                b.writers.append(op)
        self.ops[eng].append(op)
        return op

    def mm(self, out, lhsT, rhs, start, stop, r, w):
        return self.add("pe", lambda e: e.matmul(out, lhsT, rhs, start=start, stop=stop), r, w, nowaw=not start)

    def tr(self, out, in_, ident, r, w, nowaw=True):
        return self.add("pe", lambda e: e.transpose(out, in_, ident), r, w, nowaw=nowaw)

    def act(self, out, in_, func, r, w, bias=None, scale=None, accum=None, nowaw=False):
        kw = {}
        if bias is not None:
            kw["bias"] = bias
        if scale is not None:
            kw["scale"] = scale
        if accum is not None:
            kw["accum_out"] = accum
        return self.add("act", lambda e: e.activation(out, in_, func, **kw), r, w, nowaw=nowaw)

    def tt(self, eng, out, in0, in1, op, r, w, nowaw=False):
        return self.add(eng, lambda e: e.tensor_tensor(out, in0, in1, op), r, w, nowaw=nowaw)

    def ts(self, eng, out, in0, s1, s2, op0, op1, r, w, nowaw=False):
        if s2 is None:
            return self.add(eng, lambda e: e.tensor_scalar(out, in0, s1, None, op0), r, w, nowaw=nowaw)
        return self.add(eng, lambda e: e.tensor_scalar(out, in0, s1, s2, op0, op1), r, w, nowaw=nowaw)

    def stt(self, eng, out, in0, scalar, in1, op0, op1, r, w, nowaw=False):
        return self.add(eng, lambda e: e.scalar_tensor_tensor(out, in0, scalar, in1, op0, op1), r, w, nowaw=nowaw)

    def cp(self, eng, out, in_, r, w, nowaw=False):
        if eng == "act":
            return self.add(eng, lambda e: e.copy(out, in_), r, w, nowaw=nowaw)
        return self.add(eng, lambda e: e.tensor_copy(out, in_), r, w, nowaw=nowaw)

    def memset(self, eng, ap, val, w, nowaw=False):
        return self.add(eng, lambda e: e.memset(ap, val), (), w, nowaw=nowaw)

    def red(self, eng, out, in_, op, r, w, nowaw=False):
        return self.add(eng, lambda e: e.tensor_reduce(out, in_, AX.X, op), r, w, nowaw=nowaw)

    def dma(self, q, out, in_, dbuf, r, w, nowaw=False, slow=False):
        if slow:
            return self.add(q, lambda e: e.dma_start(out=out, in_=in_, allow_slow_non_contiguous=True),
                            r, w, dbuf=dbuf, nowaw=nowaw)
        return self.add(q, lambda e: e.dma_start(out=out, in_=in_), r, w, dbuf=dbuf, nowaw=nowaw)

    def gather(self, out, in_, idx, elem_off, dbuf, r, w, nowaw=False, bound=None):
        if bound is not None:
            return self.add("pool", lambda e: e.indirect_dma_start(
                out=out, out_offset=None, in_=in_,
                in_offset=bass.IndirectOffsetOnAxis(ap=idx, axis=0), element_offset=elem_off,
                bounds_check=bound, oob_is_err=False),
                r, w, dbuf=dbuf, nowaw=nowaw)
        return self.add("pool", lambda e: e.indirect_dma_start(
            out=out, out_offset=None, in_=in_,
            in_offset=bass.IndirectOffsetOnAxis(ap=idx, axis=0), element_offset=elem_off),
            r, w, dbuf=dbuf, nowaw=nowaw)

    def scatter(self, out, idx, in_, dbuf, r, w):
        return self.add("pool", lambda e: e.indirect_dma_start(
            out=out, out_offset=bass.IndirectOffsetOnAxis(ap=idx, axis=0), in_=in_, in_offset=None),
            r, w, dbuf=dbuf)

    def end(self):
        nc = self.nc
        for e in ENGS:
            for op in self.ops[e]:
                if op.is_dma:
                    sw = (e == "pool")
                    k = (id(op.dbuf), sw)
                    if k not in self.dmap:
                        n = sum(1 for kk in self.dmap if kk[1] == sw)
                        assert n < len(self.dsem[sw]), "out of DMA semaphores"
                        self.dmap[k] = n
                    si = self.dmap[k]
                    self.dcount[sw][si] += 16
                    op.sem = self.dsem[sw][si]
                    op.count = self.dcount[sw][si]
                elif op.signal:
                    self.ccount[e] += 1
                    op.sem = self.csem[e]
                    op.count = self.ccount[e]
        final = [(self.dsem[sw][si], self.dcount[sw][si]) for (_, sw), si in self.dmap.items()]
        ops = self.ops
        waited = self.waited
        engmap = {"sp": "sync", "act": "scalar", "dve": "vector", "pool": "gpsimd", "pe": "tensor"}

        def run(ename, eng):
            wd = waited[ename]
            for op in ops[ename]:
                need = {}
                for d in op.deps:
                    k = id(d.sem)
                    if k not in need or need[k][1] < d.count:
                        need[k] = (d.sem, d.count)
                todo = []
                for k, (s, c) in need.items():
                    if wd.get(k, 0) < c:
                        todo.append((s, c))
                        wd[k] = c
                for s, c in todo[:-1]:
                    eng.wait_ge(s, c)
                ins = op.fn(eng)
                if todo:
                    ins._wait_ge(todo[-1][0], todo[-1][1])
                if op.signal:
                    ins.then_inc(op.sem, 16 if op.is_dma else 1)
            if ename == "sp":
                for s, c in final:
                    if wd.get(id(s), 0) < c:
                        eng.wait_ge(s, c)
                        wd[id(s)] = c

        with nc.Block() as block:
            for ename in ENGS:
                getattr(block, engmap[ename])(lambda eng, ename=ename: run(ename, eng))
        self.nstage += 1
        self.begin()


def rope_tables(NL, NCX):
    t = np.arange(NL)
    row = (t // 64).astype(np.float32)
    col = (t % 64).astype(np.float32)
    inv_freq = (np.float32(10000.0) ** (-np.arange(8, dtype=np.float32) / np.float32(8))).astype(np.float32)
    ang_r = row[:, None] * inv_freq
    ang_c = col[:, None] * inv_freq
    ang = np.concatenate([ang_r, ang_r, ang_c, ang_c], axis=-1).astype(np.float32)
    cos = np.cos(ang).astype(np.float32)
    sin = np.sin(ang).astype(np.float32)
    NT = NL + NCX
    cosT = np.zeros((128, NT), np.float32)
    sinT = np.zeros((128, NT), np.float32)
    cosT[64:96, :NL] = cos.T
    sinT[64:96, :NL] = sin.T
    cosT[64:96, NL:] = 1.0
    return cosT, sinT


def na_plan(R):
    def band(r):
        s = min(max(r - 4, 0), R - 8)
        return s
    variants = {}
    plan = []
    for j in range(R // 2):
        rows = (2 * j, 2 * j + 1)
        ms = set()
        for r in rows:
            s = band(r)
            for kr in range(s, s + 8):
                ms.add(kr // 2)
        lst = []
        for m in sorted(ms):
            key = []
            for qp in range(2):
                s = band(rows[qp])
                for kp in range(2):
                    kr = 2 * m + kp
                    key.append((s <= kr < s + 8, kr - rows[qp] + 7))
            key = tuple(key)
            if key not in variants:
                variants[key] = len(variants)
            lst.append((m, variants[key]))
        plan.append(lst)
    return plan, variants


def na_bias_tables(rpb, variants):
    L = rpb.shape[0]
    NV = len(variants)
    qc = np.arange(64)
    kc = np.arange(64)
    c0 = np.clip(qc - 8, 0, 48)
    in_win = (kc[:, None] >= c0[None, :]) & (kc[:, None] < c0[None, :] + 16)
    dc = np.clip(kc[:, None] - qc[None, :] + 15, 0, 30)
    out = np.full((L, 128, 4, NV, 128), NEG, np.float32)
    for key, v in variants.items():
        i = 0
        for qp in range(2):
            for kp in range(2):
                ok, dr = key[i]
                i += 1
                if not ok:
                    continue
                g = rpb[:, :, dr, :][:, :, dc]
                g = np.where(in_win[None, None], g, np.float32(NEG))
                out[:, kp * 64:(kp + 1) * 64, :, v, qp * 64:(qp + 1) * 64] = g.transpose(0, 2, 1, 3)
    return out.astype(ml_dtypes.bfloat16)


class Cfg:
    def __init__(self, NL=8192, NCX=256, L=4, debug=False, stop=None, part=None):
        self.part = part
        self.NL, self.NCX, self.L = NL, NCX, L
        self.NT = NL + NCX
        self.NTL = NL // 128
        self.NTC = NCX // 128
        self.NTT = self.NT // 128
        self.R = NL // 64
        self.debug = debug
        self.stop = stop
        nasg = 2 * self.NT
        self.NBLK = (nasg + NE * (BLK - 1)) // BLK
        self.plan, self.variants = na_plan(self.R)
        self.NV = len(self.variants)

    def groups(self, with_ctx=True):
        gs = []
        for g in range(self.NTL // 4):
            gs.append((list(range(4 * g, 4 * g + 4)), 0))
        if with_ctx:
            gs.append((list(range(self.NTL, self.NTL + self.NTC)), 1))
        return gs


def build(cfg):
    nc = bass.Bass("TRN2", target_bir_lowering=False)
    NL, NCX, NT, L = cfg.NL, cfg.NCX, cfg.NT, cfg.L
    NTT, NTL = cfg.NTT, cfg.NTL
    NV, NBLK = cfg.NV, cfg.NBLK

    def din(name, shape, dt=F32):
        return nc.dram_tensor(name, list(shape), dt, kind="ExternalInput").ap()

    skind = "ExternalOutput" if cfg.debug else "Internal"

    def dscr(name, shape, dt=F32):
        return nc.dram_tensor(name, list(shape), dt, kind=skind).ap()

    x_in = din("x", [NL, D])
    ctx_in = din("ctx", [NCX, D])
    cc_in = din("cc", [2, D])
    w_ada = din("w_ada", [L, D, 6 * D])
    b_ada = din("b_ada", [L, 6 * D])
    norm1_g = din("norm1_g", [L, D])
    norm2_g = din("norm2_g", [L, D])
    w_in = din("w_in", [L, D, IN_COLS])
    conv_w = din("conv_w", [L, 3, 256])
    sg_w = din("sg_w", [L, 4, 128, 128])
    sg_b = din("sg_b", [L, 4, 128])
    na_bias = din("na_bias", [L, 128, 4, NV, 128], BF16)
    q_norm_g = din("mla_q_norm_g", [L, 256])
    w_uq = din("mla_w_uq", [L, 256, 384])
    kv_norm_g = din("mla_kv_norm_g", [L, 128])
    w_ukv = din("mla_w_ukv", [L, 128, 512])
    out_norm_g = din("out_norm_g", [L, D])
    w_out = din("w_out", [L, D, D])
    w_grp = din("w_grp", [L, D, 4])
    b_grp = din("b_grp", [L, 4])
    w_exp = din("w_exp", [L, D, NE])
    b_exp = din("b_exp", [L, NE])
    tiny = cfg.stop is not None and cfg.stop[1] < 8 and cfg.stop[0] == 0
    w_gate = din("w_gate", [L * NE * D, DE] if not tiny else [128, DE])
    w_up = din("w_up", [L * NE * D, DE] if not tiny else [128, DE])
    w_down = din("w_down", [L * NE * DE, D] if not tiny else [128, D])
    final_g = din("final_norm_g", [1, D])
    cosT_in = din("cosT", [128, NT])
    sinT_in = din("sinT", [128, NT])
    consts_f = din("consts_f", [128, 128 + 128 + 32 + 1 + 64])
    consts_b = din("consts_b", [128, 128 + 128 + 128], BF16)

    out = nc.dram_tensor("out", [NL, D], F32, kind="ExternalOutput").ap()

    X = dscr("X", [NT, D])
    MOD = dscr("MOD", [2, 6 * D])
    UT = dscr("UT", [256, NT])
    BGT = dscr("BGT", [256, NT])
    Y = dscr("Y", [NT, D])
    QN_T = dscr("QN_T", [256, NT], BF16)
    KN_T = dscr("KN_T", [256, NT], BF16)
    VN = dscr("VN", [NT, 260], BF16)
    QM_T = dscr("QM_T", [4, 96, NT], BF16)
    KM_T = dscr("KM_T", [4, 96, NT], BF16)
    VM = dscr("VM", [NT, 260], BF16)
    H2 = dscr("H2", [NT, D], BF16)
    XBUF = dscr("XBUF", [NBLK * BLK, D], BF16)
    YBUF = dscr("YBUF", [NBLK * BLK, D])

    es = ExitStack()
    with es:
        P = Prog(nc, es)

        uid = [0]

        def sb(st, name, shape, dt=F32):
            uid[0] += 1
            return st.enter_context(nc.sbuf_tensor("%s_%d" % (name, uid[0]), list(shape), dt))

        def ps(st, name, shape, dt=F32):
            uid[0] += 1
            return st.enter_context(nc.psum_tensor("%s_%d" % (name, uid[0]), list(shape), dt))

        cf = sb(es, "cf", [128, 353])
        cb = sb(es, "cb", [128, 384], BF16)
        ident_f = cf[:, 0:128]
        ones_f = cf[:, 128:256]
        iota_e = cf[:, 256:288]
        iota_p = cf[:, 288:289]
        blkstart = cf[:, 289:353]
        ident_b = cb[:, 0:128]
        ones_b = cb[:, 128:256]
        triu_b = cb[:, 256:384]
        cboth = sb(es, "cboth", [128, KC, 2])
        mask1 = sb(es, "mask1", [128, NTT, NE])
        mask2 = sb(es, "mask2", [128, NTT, NE])
        rank12 = sb(es, "rank12", [128, 2, NTT])
        gw12 = sb(es, "gw12", [128, 2, NTT])
        dest_i = sb(es, "dest_i", [128, 2, NTT], I32)
        carry = sb(es, "carry", [128, NE])
        widx = sb(es, "widx", [128, 2, NBLK], I32)

        def s0():
            with ExitStack() as st:
                craw = sb(st, "craw", [128, KC, 2])
                b_cf, b_cb, b_craw, b_cboth = P.buf("cf"), P.buf("cb"), P.buf("craw"), P.buf("cboth")
                P.dma("sp", cf[:], consts_f, b_cf, (), (b_cf,))
                P.dma("sp", cb[:], consts_b, b_cb, (), (b_cb,))
                for v in range(2):
                    P.dma("sp", craw[:, :, v], cc_in[v].rearrange("(c p) -> p c", p=128), b_craw, (), (b_craw,),
                          nowaw=True, slow=True)
                P.act(cboth[:], craw[:], AF.Silu, (b_craw,), (b_cboth,))
                zt = sb(st, "zt", [128, 4 * D], BF16)
                b_zt = P.buf("zt")
                P.memset("dve", zt[:], 0.0, (b_zt,))
                for b in range(NBLK):
                    P.dma("sp" if b % 2 else "act",
                          XBUF[b * BLK:(b + 1) * BLK, :].rearrange("(p i) d -> p (i d)", p=128), zt[:], b_zt,
                          (b_zt,), ())
                P.end()

        def x_src(l, t):
            if l == 0:
                if t < NTL:
                    return x_in[t * 128:(t + 1) * 128, :]
                return ctx_in[(t - NTL) * 128:(t - NTL + 1) * 128, :]
            return X[t * 128:(t + 1) * 128, :]

        def s1(l):
            with ExitStack() as st:
                wa = sb(st, "wa", [128, 2, KC, 512])
                bada = sb(st, "bada", [2, 6 * D])
                g12 = sb(st, "g12", [2, 2 * D])
                modsb = sb(st, "modsb", [2, 6 * D])
                pm = [ps(st, "pm%d" % i, [128, 512]) for i in range(2)]
                b_wa = P.bufs("wa", 2)
                b_pm = P.bufs("pm", 2)
                b_bada, b_g12, b_mod = P.buf("bada"), P.buf("g12"), P.buf("mod")
                for v in range(2):
                    P.dma("act", bada[v:v + 1, :], b_ada[l:l + 1, :], b_bada, (), (b_bada,), nowaw=True)
                    P.dma("act", g12[v:v + 1, 0:D], norm1_g[l:l + 1, :], b_g12, (), (b_g12,), nowaw=True)
                    P.dma("act", g12[v:v + 1, D:2 * D], norm2_g[l:l + 1, :], b_g12, (), (b_g12,), nowaw=True)
                for j in range(12):
                    s = j % 2
                    P.dma("sp", wa[:, s], w_ada[l][:, j * 512:(j + 1) * 512].rearrange("(c p) n -> p c n", p=128),
                          b_wa[s], (), (b_wa[s],))
                    for c in range(KC):
                        P.mm(pm[s][0:2, :], cboth[:, c, :], wa[:, s, c, :], c == 0, c == KC - 1,
                             (b_wa[s],), (b_pm[s],))
                    P.tt("dve", modsb[:, j * 512:(j + 1) * 512], pm[s][0:2, :], bada[:, j * 512:(j + 1) * 512],
                         ALU.add, (b_pm[s], b_bada), (b_mod,), nowaw=True)
                for k, go in ((1, 0), (4, D)):
                    P.stt("dve", modsb[:, k * D:(k + 1) * D], modsb[:, k * D:(k + 1) * D], 1.0,
                          g12[:, go:go + D], ALU.add, ALU.mult, (b_mod, b_g12), (b_mod,))
                P.dma("sp", MOD, modsb[:], b_mod, (b_mod,), ())
                P.end()

        def load_bc(q, tile_v, k, b):
            for v in range(2):
                P.dma(q, tile_v[:, v, :], MOD[v:v + 1, k * D:(k + 1) * D].partition_broadcast(128), b, (), (b,),
                      nowaw=True)

        def rms_rstd(ssq, rstd, n, r, w):
            P.ts("dve", rstd, ssq, 1.0 / n, EPS, ALU.mult, ALU.add, r, w)
            P.act(rstd, rstd, AF.Sqrt, w, w)
            P.add("dve", lambda e: e.reciprocal(rstd, rstd), w, w)

        def s2(l, last):
            with ExitStack() as st:
                win = sb(st, "win", [128, KC, IN_COLS], BF16)
                wkr = sb(st, "wkr", [128, KC, 2, 96], BF16)
                wuq = sb(st, "wuq", [128, 2, 384], BF16)
                wuqrot = sb(st, "wuqrot", [128, 2, 4, 96], BF16)
                wukv = sb(st, "wukv", [128, 512], BF16)
                wukv_v = sb(st, "wukv_v", [128, 256], BF16)
                sgw32 = sb(st, "sgw32", [128, 4, 128])
                sgwb = sb(st, "sgwb", [128, 4, 128], BF16)
                sgwT = sb(st, "sgwT", [128, 4, 128], BF16)
                sgb = sb(st, "sgb", [128, 4])
                qkvg = sb(st, "qkvg", [128, 3])
                gm_bc = sb(st, "gm_bc", [128, 2, D])
                sh_bc = sb(st, "sh_bc", [128, 2, D])
                cos_t = sb(st, "cos_t", [128, 2, 512])
                sin_t = sb(st, "sin_t", [128, 2, 512])
                xt = sb(st, "xt", [128, 2, D])
                junk = sb(st, "junk", [128, D], BF16)
                xn = sb(st, "xn", [128, 2, D])
                hb = sb(st, "hb", [128, 2, D], BF16)
                hT = sb(st, "hT", [128, 2, KC, 512], BF16)
                stat = sb(st, "stat", [128, 2, 8])
                cg_sb = sb(st, "cg_sb", [128, 2, 512])
                u_sb = sb(st, "u_sb", [128, 2, 512])
                bg_sb = sb(st, "bg_sb", [128, 2, 512])
                qk_sb = sb(st, "qk_sb", [128, 2, 512], BF16)
                vaug = sb(st, "vaug", [128, 2, 4, 65], BF16)
                vaug2 = sb(st, "vaug2", [128, 2, 4, 65], BF16)
                zb = sb(st, "zb", [128, 512])
                gt1 = sb(st, "gt1", [128, 512])
                gt2 = sb(st, "gt2", [128, 512])
                gg = sb(st, "gg", [128, 512])
                vn = sb(st, "vn", [128, 256], BF16)
                yb = sb(st, "yb", [128, 2, 256])
                cq_b = sb(st, "cq_b", [128, 2, 384], BF16)
                cqnT = sb(st, "cqnT", [128, 3, 512], BF16)
                rt1 = sb(st, "rt1", [128, 512])
                rt2 = sb(st, "rt2", [128, 512])
                qT = sb(st, "qT", [128, 2, 512], BF16)
                kn_sb = sb(st, "kn_sb", [128, 2, 512], BF16)
                kr_sb = sb(st, "kr_sb", [128, 512], BF16)
                pA = [ps(st, "pA%d" % i, [128, 1024], BF16) for i in range(4)]
                pB = [ps(st, "pB%d" % i, [128, 512]) for i in range(4)]
                b_pA = P.bufs("pA", 4)
                b_pB = P.bufs("pB", 4)
                b_w = P.buf("w")
                b_xt = P.bufs("xt", 2)
                b_stat = P.bufs("stat", 2)
                b_junk, b_xn = P.buf("junk"), P.bufs("xn", 2)
                b_hb = P.bufs("hb", 2)
                b_hT = P.bufs("hT", 2)
                b_cs = P.bufs("cs", 2)
                b_cg, b_u, b_bg, b_qk = P.bufs("cg", 2), P.bufs("u", 2), P.bufs("bg", 2), P.bufs("qk", 2)
                b_va, b_va2 = P.bufs("va", 2), P.bufs("va2", 2)
                b_zb, b_gt1, b_gt2, b_gg, b_vn = P.buf("zb"), P.buf("gt1"), P.buf("gt2"), P.buf("gg"), P.buf("vn")
                b_yb = P.bufs("yb", 2)
                b_cqb = P.bufs("cqb", 2)
                b_cqnT = P.buf("cqnT")
                b_rt1, b_rt2 = P.buf("rt1"), P.buf("rt2")
                b_qT = P.bufs("qT", 2)
                b_kn = P.bufs("kn", 2)
                b_kr = P.buf("kr")

                for c in range(KC):
                    for h2 in range(2):
                        P.dma("pool", win[:, c, h2 * 1232:(h2 + 1) * 1232],
                              w_in[l][c * 128:(c + 1) * 128, h2 * 1232:(h2 + 1) * 1232], b_w, (), (b_w,), nowaw=True)
                for c in range(2):
                    P.dma("pool", wuq[:, c, :], w_uq[l][c * 128:(c + 1) * 128, :], b_w, (), (b_w,), nowaw=True)
                P.dma("pool", wukv[:], w_ukv[l], b_w, (), (b_w,), nowaw=True)
                P.dma("act", sgw32[:], sg_w[l].rearrange("h p q -> p h q"), b_w, (), (b_w,), nowaw=True)
                P.dma("act", sgb[:], sg_b[l].rearrange("h p -> p h"), b_w, (), (b_w,), nowaw=True, slow=True)
                P.dma("act", qkvg[:, 0:2], q_norm_g[l].rearrange("(c p) -> p c", p=128), b_w, (), (b_w,),
                      nowaw=True, slow=True)
                P.dma("act", qkvg[:, 2:3], kv_norm_g[l].rearrange("(c p) -> p c", p=128), b_w, (), (b_w,),
                      nowaw=True, slow=True)
                load_bc("act", gm_bc, 1, b_w)
                load_bc("act", sh_bc, 0, b_w)
                b_wd = P.buf("wd")
                P.memset("pool", wkr[:], 0.0, (b_wd,))
                P.memset("pool", wuqrot[:], 0.0, (b_wd,))
                KR0 = MLA0 + 384
                P.cp("dve", wkr[:, :, 0, 64:96], win[:, :, KR0:KR0 + 32], (b_w,), (b_wd,))
                for (dst, src, sgn) in ((0, 8, -1.0), (8, 0, 1.0), (16, 24, -1.0), (24, 16, 1.0)):
                    P.ts("dve", wkr[:, :, 1, 64 + dst:72 + dst], win[:, :, KR0 + src:KR0 + src + 8], sgn, None,
                         ALU.mult, None, (b_w,), (b_wd,))
                    for h in range(4):
                        P.ts("dve", wuqrot[:, :, h, 64 + dst:72 + dst],
                             wuq[:, :, h * 96 + 64 + src:h * 96 + 72 + src], sgn, None, ALU.mult, None,
                             (b_w,), (b_wd,))
                for h in range(4):
                    P.cp("dve", wukv_v[:, h * 64:(h + 1) * 64], wukv[:, h * 128 + 64:(h + 1) * 128], (b_w,), (b_wd,))
                P.cp("dve", sgwb[:], sgw32[:], (b_w,), (b_wd,))
                for h in range(4):
                    P.tr(pA[0][:, h * 128:(h + 1) * 128], sgwb[:, h, :], ident_b, (b_wd,), (b_pA[0],), nowaw=(h > 0))
                P.cp("dve", sgwT[:].rearrange("p h q -> p (h q)"), pA[0][:, 0:512], (b_pA[0],), (b_wd,))
                for s in range(2):
                    P.memset("pool", vaug[:, s, :, 64:65], 1.0, (b_va[s],))
                    P.memset("pool", vaug2[:, s, :, 64:65], 1.0, (b_va2[s],))

                pbi = [0]

                def nextpb():
                    i = pbi[0] % 4
                    pbi[0] += 1
                    return i

                evi = [0]

                def evac_eng():
                    evi[0] += 1
                    return "act" if evi[0] % 2 else "dve"

                groups = cfg.groups(True)
                def front(gi, tiles, v):
                    nt = len(tiles)
                    N = 128 * nt
                    t0 = tiles[0] * 128
                    gs = gi % 2
                    P.dma("sp", cos_t[:, gs, 0:N], cosT_in[:, t0:t0 + N], b_cs[gs], (), (b_cs[gs],), nowaw=False)
                    P.dma("sp", sin_t[:, gs, 0:N], sinT_in[:, t0:t0 + N], b_cs[gs], (), (b_cs[gs],), nowaw=True)
                    for i, t in enumerate(tiles):
                        s = (gi * 4 + i) % 2
                        P.dma("sp", xt[:, s, :], x_src(l, t), b_xt[s], (), (b_xt[s],))
                        P.act(junk[:], xt[:, s, :], AF.Square, (b_xt[s],), (b_junk, b_stat[s]), accum=stat[:, s, 0:1])
                        rms_rstd(stat[:, s, 0:1], stat[:, s, 1:2], D, (b_stat[s],), (b_stat[s],))
                        P.stt("dve", xn[:, s, :], xt[:, s, :], stat[:, s, 1:2], gm_bc[:, v, :], ALU.mult, ALU.mult,
                              (b_xt[s], b_stat[s], b_w), (b_xn[s],))
                        P.tt("pool", hb[:, s, :], xn[:, s, :], sh_bc[:, v, :], ALU.add, (b_xn[s], b_w), (b_hb[s],))
                        for c in range(KC):
                            P.tr(pA[c // 2][:, (c % 2) * 512 + i * 128:(c % 2) * 512 + (i + 1) * 128],
                                 hb[:, s, c * 128:(c + 1) * 128], ident_b, (b_hb[s],), (b_pA[c // 2],),
                                 nowaw=not (i == 0 and c % 2 == 0))
                    for c in range(KC):
                        P.cp("act" if (c // 2) % 2 == 0 else "dve", hT[:, gs, c, 0:N],
                             pA[c // 2][:, (c % 2) * 512:(c % 2) * 512 + N],
                             (b_pA[c // 2],), (b_hT[gs],), nowaw=(c > 0))


                def back(gi, tiles, v):
                    nt = len(tiles)
                    N = 128 * nt
                    t0 = tiles[0] * 128
                    gs = gi % 2
                    def fm_block(col0, width=128):
                        pi = nextpb()
                        for c in range(KC):
                            P.mm(pB[pi][0:width, 0:N], win[:, c, col0:col0 + width], hT[:, gs, c, 0:N],
                                 c == 0, c == KC - 1, (b_w, b_hT[gs]), (b_pB[pi],))
                        return pi

                    for blk in range(2):
                        pi = fm_block(blk * 128)
                        P.cp("act", bg_sb[:, blk, 0:N], pB[pi][:, 0:N], (b_pB[pi],), (b_bg[blk],))
                        P.dma("sp", BGT[blk * 128:(blk + 1) * 128, t0:t0 + N], bg_sb[:, blk, 0:N], b_bg[blk],
                              (b_bg[blk],), ())
                    for blk in range(2):
                        pi = fm_block(256 + blk * 128)
                        P.cp("act", cg_sb[:, blk, 0:N], pB[pi][:, 0:N], (b_pB[pi],), (b_cg[blk],))
                    for blk in range(2):
                        pi = fm_block(512 + blk * 128)
                        P.tt("dve", u_sb[:, blk, 0:N], pB[pi][:, 0:N], cg_sb[:, blk, 0:N], ALU.mult,
                             (b_pB[pi], b_cg[blk]), (b_u[blk],))
                        P.dma("sp", UT[blk * 128:(blk + 1) * 128, t0:t0 + N], u_sb[:, blk, 0:N], b_u[blk],
                              (b_u[blk],), ())
                    for blk in range(2):
                        pi = fm_block(NA0 + blk * 128)
                        P.act(qk_sb[:, blk, 0:N], pB[pi][:, 0:N], AF.Copy, (b_pB[pi],), (b_qk[blk],), scale=0.125)
                        P.dma("sp", QN_T[blk * 128:(blk + 1) * 128, t0:t0 + N], qk_sb[:, blk, 0:N], b_qk[blk],
                              (b_qk[blk],), ())
                    for blk in range(2):
                        pi = fm_block(NA0 + 256 + blk * 128)
                        P.cp("act", qk_sb[:, blk, 0:N], pB[pi][:, 0:N], (b_pB[pi],), (b_qk[blk],))
                        P.dma("sp", KN_T[blk * 128:(blk + 1) * 128, t0:t0 + N], qk_sb[:, blk, 0:N], b_qk[blk],
                              (b_qk[blk],), ())
                    pr = []
                    for j in range(2):
                        pi = nextpb()
                        for c in range(KC):
                            P.mm(pB[pi][0:96, 0:N], wkr[:, c, j, :], hT[:, gs, c, 0:N], c == 0, c == KC - 1,
                                 (b_wd, b_hT[gs]), (b_pB[pi],))
                        pr.append(pi)
                    P.tt("dve", rt1[64:96, 0:N], pB[pr[0]][64:96, 0:N], cos_t[64:96, gs, 0:N], ALU.mult,
                         (b_pB[pr[0]], b_cs[gs]), (b_rt1,))
                    P.tt("dve", rt2[64:96, 0:N], pB[pr[1]][64:96, 0:N], sin_t[64:96, gs, 0:N], ALU.mult,
                         (b_pB[pr[1]], b_cs[gs]), (b_rt2,))
                    P.tt("pool", kr_sb[64:96, 0:N], rt1[64:96, 0:N], rt2[64:96, 0:N], ALU.add,
                         (b_rt1, b_rt2), (b_kr,))
                    for h in range(4):
                        P.dma("sp", KM_T[h, 64:96, t0:t0 + N], kr_sb[64:96, 0:N], b_kr, (b_kr,), ())

                    for i, t in enumerate(tiles):
                        s = (gi * 4 + i) % 2
                        tok = slice(i * 128, (i + 1) * 128)
                        pi = nextpb()
                        for c in range(KC):
                            P.mm(pB[pi][:, 0:512], hT[:, gs, c, tok], win[:, c, SG0:SG0 + 512], c == 0, c == KC - 1,
                                 (b_w, b_hT[gs]), (b_pB[pi],))
                        P.cp("act", zb[:], pB[pi][:, 0:512], (b_pB[pi],), (b_zb,))
                        P.tt("dve", gt1[:], zb[:], zb[:], ALU.mult, (b_zb,), (b_gt1,))
                        P.ts("dve", gt1[:], gt1[:], 0.044715, 1.0, ALU.mult, ALU.add, (b_gt1,), (b_gt1,))
                        P.tt("dve", gt1[:], gt1[:], zb[:], ALU.mult, (b_gt1, b_zb), (b_gt1,))
                        P.act(gt2[:], gt1[:], AF.Sigmoid, (b_gt1,), (b_gt2,), scale=1.5957691216057308)
                        P.tt("dve", gg[:], gt2[:], zb[:], ALU.mult, (b_gt2, b_zb), (b_gg,))
                        P.red("dve", stat[:, s, 2:3], gg[:, 256:512], ALU.add, (b_gg,), (b_stat[s],))
                        P.act(junk[:, 0:256], gg[:, 256:512], AF.Square, (b_gg,), (b_junk, b_stat[s]),
                              accum=stat[:, s, 3:4])
                        P.ts("dve", stat[:, s, 2:3], stat[:, s, 2:3], 1.0 / 256, None, ALU.mult, None,
                             (b_stat[s],), (b_stat[s],))
                        P.tt("dve", stat[:, s, 4:5], stat[:, s, 2:3], stat[:, s, 2:3], ALU.mult,
                             (b_stat[s],), (b_stat[s],))
                        P.stt("dve", stat[:, s, 3:4], stat[:, s, 3:4], 1.0 / 256, stat[:, s, 4:5],
                              ALU.mult, ALU.subtract, (b_stat[s],), (b_stat[s],))
                        P.ts("dve", stat[:, s, 3:4], stat[:, s, 3:4], EPS, None, ALU.add, None,
                             (b_stat[s],), (b_stat[s],))
                        P.act(stat[:, s, 3:4], stat[:, s, 3:4], AF.Sqrt, (b_stat[s],), (b_stat[s],))
                        P.add("dve", lambda e, s=s: e.reciprocal(stat[:, s, 3:4], stat[:, s, 3:4]),
                              (b_stat[s],), (b_stat[s],))
                        P.ts("dve", vn[:], gg[:, 256:512], stat[:, s, 2:3], stat[:, s, 3:4], ALU.subtract, ALU.mult,
                             (b_gg, b_stat[s]), (b_vn,))
                        pj = nextpb()
                        for h in range(4):
                            P.mm(pB[pj][:, h * 64:(h + 1) * 64], sgwT[:, h, :], vn[:, h * 64:(h + 1) * 64],
                                 True, True, (b_wd, b_vn), (b_pB[pj],))
                        for h in range(4):
                            P.stt("dve", yb[:, s, h * 64:(h + 1) * 64], pB[pj][:, h * 64:(h + 1) * 64],
                                  sgb[:, h:h + 1], gg[:, h * 64:(h + 1) * 64], ALU.add, ALU.mult,
                                  (b_pB[pj], b_gg, b_w), (b_yb[s],), nowaw=(h > 0))
                        P.dma("sp", Y[t * 128:(t + 1) * 128, 256:512], yb[:, s, :], b_yb[s], (b_yb[s],), ())
                        pi = nextpb()
                        for c in range(KC):
                            P.mm(pB[pi][:, 0:256], hT[:, gs, c, tok], win[:, c, NA0 + 512:NA0 + 768], c == 0,
                                 c == KC - 1, (b_w, b_hT[gs]), (b_pB[pi],))
                        P.cp("act", vaug[:, s, :, 0:64], pB[pi][:, 0:256].rearrange("p (h d) -> p h d", h=4),
                             (b_pB[pi],), (b_va[s],))
                        P.dma("sp", VN[t * 128:(t + 1) * 128, :], vaug[:, s].rearrange("p h d -> p (h d)"),
                              b_va[s], (b_va[s],), ())
                        pi = nextpb()
                        for c in range(KC):
                            P.mm(pB[pi][:, 0:384], hT[:, gs, c, tok], win[:, c, MLA0:MLA0 + 384], c == 0,
                                 c == KC - 1, (b_w, b_hT[gs]), (b_pB[pi],))
                        P.act(junk[:, 0:256], pB[pi][:, 0:256], AF.Square, (b_pB[pi],), (b_junk, b_stat[s]),
                              accum=stat[:, s, 5:6])
                        P.act(junk[:, 256:384], pB[pi][:, 256:384], AF.Square, (b_pB[pi],), (b_junk, b_stat[s]),
                              accum=stat[:, s, 6:7])
                        rms_rstd(stat[:, s, 5:6], stat[:, s, 5:6], 256, (b_stat[s],), (b_stat[s],))
                        rms_rstd(stat[:, s, 6:7], stat[:, s, 6:7], 128, (b_stat[s],), (b_stat[s],))
                        P.act(cq_b[:, s, 0:256], pB[pi][:, 0:256], AF.Copy, (b_pB[pi], b_stat[s]), (b_cqb[s],),
                              scale=stat[:, s, 5:6])
                        P.act(cq_b[:, s, 256:384], pB[pi][:, 256:384], AF.Copy, (b_pB[pi], b_stat[s]), (b_cqb[s],),
                              scale=stat[:, s, 6:7], nowaw=True)
                        for b3 in range(3):
                            P.tr(pA[b3][:, i * 128:(i + 1) * 128], cq_b[:, s, b3 * 128:(b3 + 1) * 128], ident_b,
                                 (b_cqb[s],), (b_pA[b3],), nowaw=(i > 0))
                    for b3 in range(3):
                        P.ts("dve", cqnT[:, b3, 0:N], pA[b3][:, 0:N], qkvg[:, b3:b3 + 1], None, ALU.mult, None,
                             (b_pA[b3], b_w), (b_cqnT,), nowaw=(b3 > 0))
                    for h in range(4):
                        hs = h % 2
                        p1 = nextpb()
                        for c in range(2):
                            P.mm(pB[p1][0:96, 0:N], wuq[:, c, h * 96:(h + 1) * 96], cqnT[:, c, 0:N], c == 0, c == 1,
                                 (b_w, b_cqnT), (b_pB[p1],))
                        p2 = nextpb()
                        for c in range(2):
                            P.mm(pB[p2][0:96, 0:N], wuqrot[:, c, h, :], cqnT[:, c, 0:N], c == 0, c == 1,
                                 (b_wd, b_cqnT), (b_pB[p2],))
                        P.cp("act", qT[0:64, hs, 0:N], pB[p1][0:64, 0:N], (b_pB[p1],), (b_qT[hs],))
                        P.tt("dve", rt1[64:96, 0:N], pB[p1][64:96, 0:N], cos_t[64:96, gs, 0:N], ALU.mult,
                             (b_pB[p1], b_cs[gs]), (b_rt1,))
                        P.tt("dve", rt2[64:96, 0:N], pB[p2][64:96, 0:N], sin_t[64:96, gs, 0:N], ALU.mult,
                             (b_pB[p2], b_cs[gs]), (b_rt2,))
                        P.tt("pool", qT[64:96, hs, 0:N], rt1[64:96, 0:N], rt2[64:96, 0:N], ALU.add,
                             (b_rt1, b_rt2), (b_qT[hs],), nowaw=True)
                        P.dma("sp", QM_T[h, :, t0:t0 + N], qT[0:96, hs, 0:N], b_qT[hs], (b_qT[hs],), ())
                    for h in range(4):
                        hs = h % 2
                        pi = nextpb()
                        P.mm(pB[pi][0:64, 0:N], wukv[:, h * 128:h * 128 + 64], cqnT[:, 2, 0:N], True, True,
                             (b_w, b_cqnT), (b_pB[pi],))
                        P.cp("act", kn_sb[0:64, hs, 0:N], pB[pi][0:64, 0:N], (b_pB[pi],), (b_kn[hs],))
                        P.dma("sp", KM_T[h, 0:64, t0:t0 + N], kn_sb[0:64, hs, 0:N], b_kn[hs], (b_kn[hs],), ())
                    for i, t in enumerate(tiles):
                        s = (gi * 4 + i) % 2
                        pi = nextpb()
                        P.mm(pB[pi][:, 0:256], cqnT[:, 2, i * 128:(i + 1) * 128], wukv_v[:], True, True,
                             (b_wd, b_cqnT), (b_pB[pi],))
                        P.cp("act", vaug2[:, s, :, 0:64], pB[pi][:, 0:256].rearrange("p (h d) -> p h d", h=4),
                             (b_pB[pi],), (b_va2[s],))
                        P.dma("sp", VM[t * 128:(t + 1) * 128, :], vaug2[:, s].rearrange("p h d -> p (h d)"),
                              b_va2[s], (b_va2[s],), ())
                if groups:
                    front(0, *groups[0])
                for gi, (tiles, v) in enumerate(groups):
                    if gi + 1 < len(groups):
                        front(gi + 1, *groups[gi + 1])
                    back(gi, tiles, v)
                P.end()
        def s3(l, last):
            with ExitStack() as st:
                ut = sb(st, "ut", [128, 2, 2, 514])
                bgt = sb(st, "bgt", [128, 2, 2, 512])
                cw = sb(st, "cw", [128, 2, 3])
                acc = sb(st, "acc", [128, 2, 512])
                ya = sb(st, "ya", [128, 2, 512])
                yat = sb(st, "yat", [128, 2, 256])
                pF = [ps(st, "pF%d" % i, [128, 512]) for i in range(2)]
                b_ut, b_bgt = P.bufs("ut", 2), P.bufs("bgt", 2)
                b_cw, b_acc, b_ya = P.buf("cw"), P.bufs("acc", 2), P.bufs("ya", 2)
                b_yat, b_pF = P.bufs("yat", 2), P.bufs("pF", 2)
                for blk in range(2):
                    for k3 in range(3):
                        P.dma("act", cw[:, blk, k3:k3 + 1],
                              conv_w[l][k3, blk * 128:(blk + 1) * 128].rearrange("(p o) -> p o", o=1),
                              b_cw, (), (b_cw,), nowaw=True, slow=True)
                cnt = 0
                for gi, (tiles, v) in enumerate(cfg.groups(not last)):
                    nt = len(tiles)
                    N = 128 * nt
                    t0 = tiles[0] * 128
                    s0_, s1_ = (0, NL) if v == 0 else (NL, NT)
                    gs = gi % 2
                    lo = max(t0 - 1, s0_)
                    hi = min(t0 + N + 1, s1_)
                    off = lo - (t0 - 1)
                    if t0 - 1 < s0_:
                        P.memset("pool", ut[:, gs, :, 0:1], 0.0, (b_ut[gs],))
                    if t0 + N + 1 > s1_:
                        P.memset("pool", ut[:, gs, :, N + 1:N + 2], 0.0, (b_ut[gs],), nowaw=True)
                    for blk in range(2):
                        P.dma("sp", ut[:, gs, blk, off:off + hi - lo], UT[blk * 128:(blk + 1) * 128, lo:hi],
                              b_ut[gs], (), (b_ut[gs],), nowaw=True)
                        P.dma("sp", bgt[:, gs, blk, 0:N], BGT[blk * 128:(blk + 1) * 128, t0:t0 + N],
                              b_bgt[gs], (), (b_bgt[gs],), nowaw=True)
                    for blk in range(2):
                        P.ts("dve", acc[:, blk, 0:N], ut[:, gs, blk, 0:N], cw[:, blk, 0:1], None, ALU.mult, None,
                             (b_ut[gs], b_cw), (b_acc[blk],))
                        P.stt("dve", acc[:, blk, 0:N], ut[:, gs, blk, 1:N + 1], cw[:, blk, 1:2], acc[:, blk, 0:N],
                              ALU.mult, ALU.add, (b_ut[gs], b_cw, b_acc[blk]), (b_acc[blk],))
                        P.stt("dve", acc[:, blk, 0:N], ut[:, gs, blk, 2:N + 2], cw[:, blk, 2:3], acc[:, blk, 0:N],
                              ALU.mult, ALU.add, (b_ut[gs], b_cw, b_acc[blk]), (b_acc[blk],))
                        P.tt("pool", ya[:, blk, 0:N], acc[:, blk, 0:N], bgt[:, gs, blk, 0:N], ALU.mult,
                             (b_acc[blk], b_bgt[gs]), (b_ya[blk],))
                    for i, t in enumerate(tiles):
                        s = cnt % 2
                        cnt += 1
                        for blk in range(2):
                            P.tr(pF[s][:, blk * 128:(blk + 1) * 128], ya[:, blk, i * 128:(i + 1) * 128], ident_f,
                                 (b_ya[blk],), (b_pF[s],), nowaw=(blk > 0))
                        P.cp("act", yat[:, s, :], pF[s][:, 0:256], (b_pF[s],), (b_yat[s],))
                        P.dma("sp", Y[t * 128:(t + 1) * 128, 0:256], yat[:, s, :], b_yat[s], (b_yat[s],), ())
                P.end()

        def s4(l, last):
            with ExitStack() as st:
                kn = sb(st, "kn", [128, 2, NT], BF16)
                vns = sb(st, "vns", [128, NTT, 260], BF16)
                bias = sb(st, "bias", [128, 4, NV, 128], BF16)
                qt = sb(st, "qt", [128, 2, 2, 128], BF16)
                pp = sb(st, "pp", [128, 2, 8, 128], BF16)
                yc = sb(st, "yc", [128, 2, 256])
                rec = sb(st, "rec", [128, 2, 4])
                pS = [ps(st, "pS%d" % i, [128, 2, 512]) for i in range(2)]
                pO = [ps(st, "pO%d" % i, [128, 512]) for i in range(2)]
                b_kv, b_bias = P.buf("kv"), P.buf("bias")
                b_qt, b_pp, b_yc, b_rec = P.bufs("qt", 2), P.bufs("pp", 2), P.bufs("yc", 2), P.bufs("rec", 2)
                b_pS, b_pO = P.bufs("pS", 2), P.bufs("pO", 2)
                for blk in range(2):
                    for h0 in range(0, NT, 2048):
                        h1 = min(NT, h0 + 2048)
                        P.dma("sp", kn[:, blk, h0:h1], KN_T[blk * 128:(blk + 1) * 128, h0:h1], b_kv, (), (b_kv,),
                              nowaw=True)
                for t0_ in range(0, NTT, 8):
                    t1_ = min(NTT, t0_ + 8)
                    P.dma("act", vns[:, t0_:t1_, :], VN[t0_ * 128:t1_ * 128, :].rearrange("(t p) f -> p t f", p=128),
                          b_kv, (), (b_kv,), nowaw=True)
                P.dma("act", bias[:].rearrange("p h v q -> p (h v q)"),
                      na_bias[l].rearrange("p h v q -> p (h v q)"), b_bias, (), (b_bias,))
                tiles = list(range(NTL)) + ([] if last else list(range(NTL, NTT)))
                ctx_chunks = [(m, None) for m in range(NTL, NTT)]
                cnt = 0
                for ti, t in enumerate(tiles):
                    chunks = (cfg.plan[t] + ctx_chunks) if t < NTL else ctx_chunks
                    nch = len(chunks)
                    s = ti % 2
                    P.dma("sp", qt[:, s], QN_T[:, t * 128:(t + 1) * 128].rearrange("(b p) q -> p b q", p=128),
                          b_qt[s], (), (b_qt[s],))
                    for h in range(4):
                        u = cnt % 2
                        cnt += 1
                        hb_, base = h // 2, 64 * (h % 2)
                        for i, (m, var) in enumerate(chunks):
                            o = pS[u][:, i // 4, (i % 4) * 128:(i % 4 + 1) * 128]
                            P.mm(o, kn[base:base + 64, hb_, m * 128:(m + 1) * 128], qt[base:base + 64, s, hb_, :],
                                 True, var is None, (b_kv, b_qt[s]), (b_pS[u],))
                            if var is not None:
                                P.mm(o, ident_b, bias[:, h, var, :], False, True, (b_bias,), (b_pS[u],))
                        P.act(pp[:, u, 0:nch, :], pS[u][:].rearrange("p a (b q) -> p (a b) q", q=128)[:, 0:nch, :],
                              AF.Exp, (b_pS[u],), (b_pp[u],))
                        for i, (m, var) in enumerate(chunks):
                            P.mm(pO[s][:, h * 65:(h + 1) * 65], pp[:, u, i, :], vns[:, m, h * 65:(h + 1) * 65],
                                 i == 0, i == nch - 1, (b_pp[u], b_kv), (b_pO[s],))
                    cnt = cnt
                    o4 = pO[s][:, 0:260].rearrange("p (h d) -> p h d", h=4)
                    P.add("dve", lambda e, o4=o4, s=s: e.reciprocal(rec[:, s, :], o4[:, :, 64]),
                          (b_pO[s],), (b_rec[s],))
                    for h in range(4):
                        P.ts("dve", yc[:, s, h * 64:(h + 1) * 64], pO[s][:, h * 65:h * 65 + 64], rec[:, s, h:h + 1],
                             None, ALU.mult, None, (b_pO[s], b_rec[s]), (b_yc[s],), nowaw=(h > 0))
                    P.dma("sp", Y[t * 128:(t + 1) * 128, 512:768], yc[:, s, :], b_yc[s], (b_yc[s],), ())
                P.end()

        def s5(l, last):
            with ExitStack() as st:
                km = sb(st, "km", [128, 4, NT], BF16)
                vms = sb(st, "vms", [128, NTT, 260], BF16)
                qm = sb(st, "qm", [128, 2, 4, 512], BF16)
                pp = sb(st, "pp", [128, 4, 512], BF16)
                yd = sb(st, "yd", [128, 2, 4, 256])
                rec = sb(st, "rec", [128, 2, 4])
                NS = 5
                pS = [ps(st, "pS%d" % i, [128, 512]) for i in range(NS)]
                pO = [ps(st, "pO%d" % i, [128, 512]) for i in range(2)]
                b_kv = P.buf("kv")
                b_qm, b_pp, b_yd, b_rec = P.bufs("qm", 2), P.bufs("pp", 4), P.bufs("yd", 2), P.bufs("rec", 2)
                b_pS, b_pO = P.bufs("pS", NS), P.bufs("pO", 2)
                for h in range(4):
                    for h0 in range(0, NT, 2048):
                        h1 = min(NT, h0 + 2048)
                        P.dma("sp", km[0:96, h, h0:h1], KM_T[h, :, h0:h1], b_kv, (), (b_kv,), nowaw=True)
                for t0_ in range(0, NTT, 8):
                    t1_ = min(NTT, t0_ + 8)
                    P.dma("act", vms[:, t0_:t1_, :], VM[t0_ * 128:t1_ * 128, :].rearrange("(t p) f -> p t f", p=128),
                          b_kv, (), (b_kv,), nowaw=True)
                all_chunks = list(range(NTT))
                ctx_only = list(range(NTL, NTT))
                cnt = 0
                si = 0
                for gi, (tiles, v) in enumerate(cfg.groups(not last)):
                    nt = len(tiles)
                    N = 128 * nt
                    t0 = tiles[0] * 128
                    gs = gi % 2
                    chunks = all_chunks if v == 0 else ctx_only
                    nch = len(chunks)
                    for h in range(4):
                        P.dma("sp", qm[0:96, gs, h, 0:N], QM_T[h, :, t0:t0 + N], b_qm[gs], (), (b_qm[gs],), nowaw=True)
                    for h in range(4):
                        u = cnt % 2
                        cnt += 1
                        pend = []

                        def qk(ci):
                            nonlocal si
                            m = chunks[ci]
                            k = si % NS
                            si += 1
                            P.mm(pS[k][:, 0:N], km[0:96, h, m * 128:(m + 1) * 128], qm[0:96, gs, h, 0:N], True, True,
                                 (b_kv, b_qm[gs]), (b_pS[k],))
                            pend.append((ci, k))

                        def pv():
                            ci, k = pend.pop(0)
                            m = chunks[ci]
                            pslot = ci % 4
                            P.act(pp[:, pslot, 0:N], pS[k][:, 0:N], AF.Exp, (b_pS[k],), (b_pp[pslot],), scale=MLA_SCALE)
                            for sub in range(nt):
                                P.mm(pO[u][:, sub * 65:(sub + 1) * 65], pp[:, pslot, sub * 128:(sub + 1) * 128],
                                     vms[:, m, h * 65:(h + 1) * 65], ci == 0 and sub == 0,
                                     ci == nch - 1 and sub == nt - 1,
                                     (b_pp[pslot], b_kv), (b_pO[u],))

                        LOOK = 2
                        for ci in range(nch):
                            qk(ci)
                            if len(pend) > LOOK:
                                pv()
                        while pend:
                            pv()
                        o4 = pO[u][:, 0:nt * 65].rearrange("p (s d) -> p s d", d=65)
                        P.add("dve", lambda e, o4=o4, u=u, nt=nt: e.reciprocal(rec[:, u, 0:nt], o4[:, :, 64]),
                              (b_pO[u],), (b_rec[u],))
                        for sub in range(nt):
                            P.ts("dve", yd[:, gs, sub, h * 64:(h + 1) * 64], pO[u][:, sub * 65:sub * 65 + 64],
                                 rec[:, u, sub:sub + 1], None, ALU.mult, None, (b_pO[u], b_rec[u]), (b_yd[gs],),
                                 nowaw=not (h == 0 and sub == 0))
                    for sub, t in enumerate(tiles):
                        P.dma("sp", Y[t * 128:(t + 1) * 128, 768:1024], yd[:, gs, sub, :], b_yd[gs], (b_yd[gs],), ())
                P.end()
        def s6(l, last):
            with ExitStack() as st:
                wout = sb(st, "wout", [128, KC, D], BF16)
                wr = sb(st, "wr", [128, KC, 36], BF16)
                brt = sb(st, "brt", [128, 36])
                outg = sb(st, "outg", [128, KC])
                g1_bc = sb(st, "g1_bc", [128, 2, D])
                gm2_bc = sb(st, "gm2_bc", [128, 2, D])
                sh2_bc = sb(st, "sh2_bc", [128, 2, D])
                yt = sb(st, "yt", [128, 2, D])
                junk = sb(st, "junk", [128, D], BF16)
                ynb = sb(st, "ynb", [128, 2, D], BF16)
                ynT = sb(st, "ynT", [128, 2, KC, 128], BF16)
                xt = sb(st, "xt", [128, 2, D])
                xnew = sb(st, "xnew", [128, 2, D])
                tmp = sb(st, "tmp", [128, D])
                h2b = sb(st, "h2b", [128, 2, D], BF16)
                h2T = sb(st, "h2T", [128, 2, KC, 128], BF16)
                stat = sb(st, "stat", [128, 2, 16])
                lg = sb(st, "lg", [128, 2, 36])
                rtmp = sb(st, "rtmp", [128, 2, 4, NE])
                amask = sb(st, "amask", [128, 2, NE], BF16)
                pA = [ps(st, "pA%d" % i, [128, 1024], BF16) for i in range(2)]
                pO = [ps(st, "pO%d" % i, [128, 2, 512]) for i in range(2)]
                pR = [ps(st, "pR%d" % i, [128, 512]) for i in range(2)]
                b_w = P.buf("w")
                b_yt, b_ynb, b_ynT, b_xt = P.bufs("yt", 2), P.bufs("ynb", 2), P.bufs("ynT", 2), P.bufs("xt", 2)
                b_xnew, b_h2b, b_h2T = P.bufs("xnew", 2), P.bufs("h2b", 2), P.bufs("h2T", 2)
                b_stat, b_lg, b_rtmp, b_am = P.bufs("stat", 2), P.bufs("lg", 2), P.bufs("rtmp", 2), P.bufs("am", 2)
                b_junk, b_tmp = P.buf("junk"), P.buf("tmp")
                b_pA, b_pO, b_pR = P.bufs("pA", 2), P.bufs("pO", 2), P.bufs("pR", 2)
                b_rout, b_carry = P.buf("rout"), P.buf("carry")
                for c in range(KC):
                    P.dma("pool", wout[:, c, :], w_out[l][c * 128:(c + 1) * 128, :], b_w, (), (b_w,), nowaw=True)
                P.dma("pool", wr[:, :, 0:4], w_grp[l].rearrange("(c p) n -> p c n", p=128), b_w, (), (b_w,), nowaw=True)
                P.dma("pool", wr[:, :, 4:36], w_exp[l].rearrange("(c p) n -> p c n", p=128), b_w, (), (b_w,), nowaw=True)
                P.dma("act", brt[:, 0:4], b_grp[l:l + 1, :].partition_broadcast(128), b_w, (), (b_w,), nowaw=True)
                P.dma("act", brt[:, 4:36], b_exp[l:l + 1, :].partition_broadcast(128), b_w, (), (b_w,), nowaw=True)
                P.dma("act", outg[:], out_norm_g[l].rearrange("(c p) -> p c", p=128), b_w, (), (b_w,), nowaw=True,
                      slow=True)
                load_bc("act", g1_bc, 2, b_w)
                load_bc("act", gm2_bc, 4, b_w)
                load_bc("act", sh2_bc, 3, b_w)
                P.memset("dve", carry[:], 0.0, (b_carry,))
                tiles = [(t, 0) for t in range(NTL)] + ([] if last else [(t, 1) for t in range(NTL, NTT)])
                def phaseA(ti, t, v):
                    s = ti % 2
                    rows = slice(t * 128, (t + 1) * 128)
                    P.dma("sp", yt[:, s, :], Y[rows, :], b_yt[s], (), (b_yt[s],))
                    P.dma("sp", xt[:, s, :], x_src(l, t), b_xt[s], (), (b_xt[s],))
                    for g in range(4):
                        P.act(junk[:, g * 256:(g + 1) * 256], yt[:, s, g * 256:(g + 1) * 256], AF.Square,
                              (b_yt[s],), (b_junk, b_stat[s]), accum=stat[:, s, g:g + 1])
                    rms_rstd(stat[:, s, 0:4], stat[:, s, 4:8], 256, (b_stat[s],), (b_stat[s],))
                    for g in range(4):
                        P.act(ynb[:, s, g * 256:(g + 1) * 256], yt[:, s, g * 256:(g + 1) * 256], AF.Copy,
                              (b_yt[s], b_stat[s]), (b_ynb[s],), scale=stat[:, s, 4 + g:5 + g], nowaw=(g > 0))
                    for c in range(KC):
                        P.tr(pA[s][:, c * 128:(c + 1) * 128], ynb[:, s, c * 128:(c + 1) * 128], ident_b,
                             (b_ynb[s],), (b_pA[s],), nowaw=(c > 0))
                    for c in range(KC):
                        P.ts("dve", ynT[:, s, c, :], pA[s][:, c * 128:(c + 1) * 128],
                             outg[:, c:c + 1], None, ALU.mult, None, (b_pA[s], b_w), (b_ynT[s],), nowaw=(c > 0))
                    for half in range(2):
                        for c in range(KC):
                            P.mm(pO[s][:, half, :], ynT[:, s, c, :], wout[:, c, half * 512:(half + 1) * 512],
                                 c == 0, c == KC - 1, (b_ynT[s], b_w), (b_pO[s],))
                    P.tt("dve", tmp[:], pO[s][:].rearrange("p a b -> p (a b)"), g1_bc[:, v, :], ALU.mult,
                         (b_pO[s], b_w), (b_tmp,))
                    P.tt("pool", xnew[:, s, :], tmp[:], xt[:, s, :], ALU.add, (b_tmp, b_xt[s]), (b_xnew[s],))
                    P.dma("sp", X[rows, :], xnew[:, s, :], b_xnew[s], (b_xnew[s],), ())
                    P.act(junk[:], xnew[:, s, :], AF.Square, (b_xnew[s],), (b_junk, b_stat[s]), accum=stat[:, s, 8:9])
                    rms_rstd(stat[:, s, 8:9], stat[:, s, 9:10], D, (b_stat[s],), (b_stat[s],))
                    P.stt("dve", tmp[:], xnew[:, s, :], stat[:, s, 9:10], gm2_bc[:, v, :], ALU.mult, ALU.mult,
                          (b_xnew[s], b_stat[s], b_w), (b_tmp,))
                    P.tt("pool", h2b[:, s, :], tmp[:], sh2_bc[:, v, :], ALU.add, (b_tmp, b_w), (b_h2b[s],))
                    P.dma("sp", H2[rows, :], h2b[:, s, :], b_h2b[s], (b_h2b[s],), ())
                    for c in range(KC):
                        P.tr(pA[s][:, c * 128:(c + 1) * 128], h2b[:, s, c * 128:(c + 1) * 128], ident_b,
                             (b_h2b[s],), (b_pA[s],), nowaw=(c > 0))
                    P.cp("act", h2T[:, s].rearrange("p c q -> p (c q)"), pA[s][:, :], (b_pA[s],), (b_h2T[s],))
                    for c in range(KC):
                        P.mm(pR[s][:, 0:36], h2T[:, s, c, :], wr[:, c, :], c == 0, c == KC - 1,
                             (b_h2T[s], b_w), (b_pR[s],))
                    LG = lg[:, s, :]
                    P.tt("dve", LG, pR[s][:, 0:36], brt[:], ALU.add, (b_pR[s], b_w), (b_lg[s],))

                def phaseB(ti, t, v):
                    s = ti % 2
                    R_, W_ = (b_lg[s], b_stat[s], b_rtmp[s]), (b_stat[s], b_rtmp[s])
                    sm = stat[:, s, 10:11]
                    P.red("dve", sm, lg[:, s, 0:4], ALU.max, R_, W_)
                    goh = rtmp[:, s, 0, 0:4]
                    P.ts("dve", goh, lg[:, s, 0:4], sm, None, ALU.is_ge, None, R_, W_)
                    P.ts("dve", stat[:, s, 11:12], sm, -1.0, None, ALU.mult, None, R_, W_)
                    P.act(rtmp[:, s, 0, 4:8], lg[:, s, 0:4], AF.Exp, R_, W_, bias=stat[:, s, 11:12],
                          accum=stat[:, s, 12:13])
                    pg = stat[:, s, 13:14]
                    P.add("dve", lambda e, pg=pg, s=s: e.reciprocal(pg, stat[:, s, 12:13]), R_, W_)
                    ml = rtmp[:, s, 1, :]
                    pen = rtmp[:, s, 2, :]
                    P.ts("dve", pen.rearrange("p (g e) -> p g e", g=4),
                         goh.unsqueeze(2).to_broadcast([128, 4, 8]), -1.0, 1.0e4, ALU.add, ALU.mult, R_, W_)
                    P.tt("dve", ml, lg[:, s, 4:36], pen, ALU.add, R_, W_)
                    v1 = stat[:, s, 14:15]
                    v2 = stat[:, s, 15:16]
                    P.red("dve", v1, ml, ALU.max, R_, W_)
                    m1 = mask1[:, t, :]
                    m2 = mask2[:, t, :]
                    RW = W_ + (b_rout,)
                    P.ts("dve", m1, ml, v1, None, ALU.is_ge, None, R_, RW)
                    P.stt("dve", ml, m1, -1.0e4, ml, ALU.mult, ALU.add, R_ + (b_rout,), W_)
                    P.red("dve", v2, ml, ALU.max, R_, W_)
                    P.ts("dve", m2, ml, v2, None, ALU.is_ge, None, R_, RW)
                    dv = stat[:, s, 11:12]
                    P.tt("dve", dv, v2, v1, ALU.subtract, R_, W_)
                    P.act(dv, dv, AF.Exp, R_, W_)
                    P.ts("dve", dv, dv, 1.0, None, ALU.add, None, R_, W_)
                    P.add("dve", lambda e, dv=dv: e.reciprocal(dv, dv), R_, W_)
                    P.tt("dve", gw12[:, 0, t:t + 1], dv, pg, ALU.mult, R_, RW)
                    P.tt("dve", gw12[:, 1, t:t + 1], pg, gw12[:, 0, t:t + 1], ALU.subtract, R_ + (b_rout,), RW)
                    P.tt("dve", amask[:, s, :], m1, m2, ALU.add, (b_rout,), (b_am[s],))
                    P.mm(pR[s][:, 64:96], triu_b, amask[:, s, :], True, True, (b_am[s],), (b_pR[s],))
                    P.mm(pR[s][:, 128:160], ones_b, amask[:, s, :], True, True, (b_am[s],), (b_pR[s],))
                    rk = rtmp[:, s, 3, :]
                    P.tt("dve", rk, pR[s][:, 64:96], carry[:], ALU.add, (b_pR[s], b_carry) + R_, W_)
                    P.tt("dve", carry[:], carry[:], pR[s][:, 128:160], ALU.add, (b_pR[s], b_carry), (b_carry,))
                    for k, mk in ((0, m1), (1, m2)):
                        P.tt("dve", pen, rk, mk, ALU.mult, R_ + (b_rout,), W_)
                        P.red("dve", rank12[:, k, t:t + 1], pen, ALU.add, R_, RW)
                ntl_ = len(tiles)
                if ntl_:
                    phaseA(0, *tiles[0])
                for ti, (t, v) in enumerate(tiles):
                    if ti + 1 < ntl_:
                        phaseA(ti + 1, *tiles[ti + 1])
                    phaseB(ti, t, v)
                P.end()

        def s7(l, last):
            with ExitStack() as st:
                pad = sb(st, "pad", [128, NE])
                pend = sb(st, "pend", [128, NE])
                pstart = sb(st, "pstart", [128, NE])
                big = sb(st, "big", [128, NTT, NE])
                destf = sb(st, "destf", [128, 2, NTT])
                cmp_ = sb(st, "cmp", [128, NBLK, NE])
                ebf = sb(st, "ebf", [128, NBLK])
                wif = sb(st, "wif", [128, 2, NBLK])
                h2r = sb(st, "h2r", [128, 3, D], BF16)
                b_p, b_big, b_dest, b_cmp, b_eb = P.buf("p"), P.buf("big"), P.buf("dest"), P.buf("cmp"), P.buf("eb")
                b_h2r = P.bufs("h2r", 3)
                JJ = (2 * NT) // BLK + 1
                cmpp = sb(st, "cmpp", [128, NE, JJ])
                P.tt("dve", cmpp[:], carry[:].unsqueeze(2).to_broadcast([128, NE, JJ]),
                     blkstart[:, 0:JJ].unsqueeze(1).to_broadcast([128, NE, JJ]), ALU.is_gt, (), (b_p,))
                P.red("dve", pad[:], cmpp[:], ALU.add, (b_p,), (b_p,))
                P.ts("dve", pad[:], pad[:], float(BLK), None, ALU.mult, None, (b_p,), (b_p,))
                P.cp("dve", pend[:, 0:1], pad[:, 0:1], (b_p,), (b_p,))
                for e_ in range(1, NE):
                    P.tt("dve", pend[:, e_:e_ + 1], pend[:, e_ - 1:e_], pad[:, e_:e_ + 1], ALU.add, (b_p,), (b_p,))
                P.tt("dve", pstart[:], pend[:], pad[:], ALU.subtract, (b_p,), (b_p,))
                ntt_used = NTL if last else NTT
                for k, mk in ((0, mask1), (1, mask2)):
                    P.tt("dve", big[:, 0:ntt_used, :], mk[:, 0:ntt_used, :],
                         pstart[:].unsqueeze(1).to_broadcast([128, ntt_used, NE]), ALU.mult, (b_p,), (b_big,))
                    P.red("dve", destf[:, k, 0:ntt_used], big[:, 0:ntt_used, :], ALU.add, (b_big,), (b_dest,))
                    P.tt("dve", destf[:, k, 0:ntt_used], destf[:, k, 0:ntt_used], rank12[:, k, 0:ntt_used], ALU.add,
                         (b_dest,), (b_dest,))
                P.cp("dve", dest_i[:, :, 0:ntt_used], destf[:, :, 0:ntt_used], (b_dest,), (b_dest,))
                P.tt("dve", cmp_[:], pend[:].unsqueeze(1).to_broadcast([128, NBLK, NE]),
                     blkstart[:, 0:NBLK].unsqueeze(2).to_broadcast([128, NBLK, NE]), ALU.is_le, (b_p,), (b_cmp,))
                P.red("dve", ebf[:], cmp_[:], ALU.add, (b_cmp,), (b_eb,))
                P.ts("dve", ebf[:], ebf[:], float(NE - 1), None, ALU.min, None, (b_eb,), (b_eb,))
                P.ts("dve", wif[:, 0, :], ebf[:], float(D), iota_p, ALU.mult, ALU.add, (b_eb,), (b_eb,))
                P.ts("dve", wif[:, 1, :], ebf[:], float(DE), iota_p, ALU.mult, ALU.add, (b_eb,), (b_eb,))
                P.cp("dve", widx[:], wif[:], (b_eb,), (b_eb,))
                for t in range(ntt_used):
                    s = t % 3
                    P.dma("sp", h2r[:, s, :], H2[t * 128:(t + 1) * 128, :], b_h2r[s], (), (b_h2r[s],))
                    for k in range(2):
                        P.scatter(XBUF, dest_i[:, k, t:t + 1], h2r[:, s, :], b_h2r[s], (b_h2r[s], b_dest), ())
                P.end()

        def s8(l, last):
            with ExitStack() as st:
                wg = sb(st, "wg", [128, 3, KC, DE], BF16)
                wu = sb(st, "wu", [128, 3, KC, DE], BF16)
                wd = sb(st, "wd", [128, 3, 4, D], BF16)
                xr = sb(st, "xr", [128, 2, 4, D], BF16)
                xT = sb(st, "xT", [128, 2, KC, 512], BF16)
                sg_ = sb(st, "sg", [128, 2, 512])
                hid = sb(st, "hid", [128, 2, 4, 512], BF16)
                yo = sb(st, "yo", [128, 2, D])
                pA = [ps(st, "pA%d" % i, [128, 1024], BF16) for i in range(2)]
                pG = [ps(st, "pG%d" % i, [128, 512]) for i in range(4)]
                pY = [ps(st, "pY%d" % i, [128, 512]) for i in range(2)]
                b_wg, b_wu, b_wd = P.bufs("wg", 3), P.bufs("wu", 3), P.bufs("wd", 3)
                b_xr, b_xT, b_sg, b_hid, b_yo = P.bufs("xr", 2), P.bufs("xT", 2), P.bufs("sg", 2), P.bufs("hid", 2), P.bufs("yo", 2)
                b_pA, b_pG, b_pY = P.bufs("pA", 2), P.bufs("pG", 4), P.bufs("pY", 2)
                nblk = NBLK
                pai = 0
                pgi = 0
                yi = 0
                for b in range(nblk):
                    s = b % 2
                    ws = b % 3
                    for c in range(KC):
                        P.gather(wg[:, ws, c, :], w_gate, widx[:, 0, b:b + 1], (l * NE * D + c * 128) * DE,
                                 b_wg[ws], (), (b_wg[ws],), nowaw=True)
                    for c in range(KC):
                        P.gather(wu[:, ws, c, :], w_up, widx[:, 0, b:b + 1], (l * NE * D + c * 128) * DE,
                                 b_wu[ws], (), (b_wu[ws],), nowaw=True)
                    for f in range(4):
                        P.gather(wd[:, ws, f, :], w_down, widx[:, 1, b:b + 1], (l * NE * DE + f * 128) * D,
                                 b_wd[ws], (), (b_wd[ws],), nowaw=True)
                    P.dma("sp", xr[:, s], XBUF[b * BLK:(b + 1) * BLK, :].rearrange("(i p) d -> p i d", p=128),
                          b_xr[s], (), (b_xr[s],))
                    for c in range(KC):
                        a = pai % 2
                        pai += 1
                        for i in range(4):
                            P.tr(pA[a][:, i * 128:(i + 1) * 128], xr[:, s, i, c * 128:(c + 1) * 128], ident_b,
                                 (b_xr[s],), (b_pA[a],), nowaw=(i > 0))
                        P.cp("act" if c % 2 else "dve", xT[:, s, c, :], pA[a][:, 0:512], (b_pA[a],), (b_xT[s],),
                             nowaw=(c > 0))
                    for f in range(4):
                        g0 = pgi % 4
                        g1 = (pgi + 1) % 4
                        pgi += 2
                        for c in range(KC):
                            P.mm(pG[g0][:, :], wg[:, ws, c, f * 128:(f + 1) * 128], xT[:, s, c, :], c == 0, c == KC - 1,
                                 (b_wg[ws], b_xT[s]), (b_pG[g0],))
                        for c in range(KC):
                            P.mm(pG[g1][:, :], wu[:, ws, c, f * 128:(f + 1) * 128], xT[:, s, c, :], c == 0, c == KC - 1,
                                 (b_wu[ws], b_xT[s]), (b_pG[g1],))
                        fs = f % 2
                        P.act(sg_[:, fs, :], pG[g0][:, :], AF.Silu, (b_pG[g0],), (b_sg[fs],))
                        P.tt("dve", hid[:, s, f, :], sg_[:, fs, :], pG[g1][:, :], ALU.mult, (b_sg[fs], b_pG[g1]),
                             (b_hid[s],), nowaw=(f > 0))
                    for i in range(4):
                        ys = yi % 2
                        yi += 1
                        for half in range(2):
                            for f in range(4):
                                P.mm(pY[half][:, :], hid[:, s, f, i * 128:(i + 1) * 128],
                                     wd[:, ws, f, half * 512:(half + 1) * 512], f == 0, f == 3,
                                     (b_hid[s], b_wd[ws]), (b_pY[half],))
                            P.cp("act" if half else "dve", yo[:, ys, half * 512:(half + 1) * 512], pY[half][:, :],
                                 (b_pY[half],), (b_yo[ys],), nowaw=(half > 0))
                        P.dma("sp", YBUF[b * BLK + i * 128:b * BLK + (i + 1) * 128, :], yo[:, ys, :], b_yo[ys],
                              (b_yo[ys],), ())
                P.end()

        def s9(l, last):
            with ExitStack() as st:
                g2_bc = sb(st, "g2_bc", [128, 2, D])
                fg_bc = sb(st, "fg_bc", [128, D])
                y1 = sb(st, "y1", [128, 4, D])
                y2 = sb(st, "y2", [128, 4, D])
                xt = sb(st, "xt", [128, 4, D])
                f1 = sb(st, "f1", [128, D])
                xo = sb(st, "xo", [128, 4, D])
                junk = sb(st, "junk", [128, D], BF16)
                stat = sb(st, "stat", [128, 4, 2])
                b_w = P.buf("w")
                b_y1, b_y2, b_xt, b_xo, b_stat = P.bufs("y1", 4), P.bufs("y2", 4), P.bufs("xt", 4), P.bufs("xo", 4), P.bufs("stat", 4)
                b_f1, b_junk = P.buf("f1"), P.buf("junk")
                load_bc("act", g2_bc, 5, b_w)
                if last:
                    P.dma("act", fg_bc[:], final_g.partition_broadcast(128), b_w, (), (b_w,), nowaw=True)
                tiles = [(t, 0) for t in range(NTL)] + ([] if last else [(t, 1) for t in range(NTL, NTT)])
                for ti, (t, v) in enumerate(tiles):
                    s = ti % 4
                    rows = slice(t * 128, (t + 1) * 128)
                    P.gather(y1[:, s, :], YBUF, dest_i[:, 0, t:t + 1], 0, b_y1[s], (), (b_y1[s],))
                    P.gather(y2[:, s, :], YBUF, dest_i[:, 1, t:t + 1], 0, b_y2[s], (), (b_y2[s],))
                    P.dma("sp", xt[:, s, :], X[rows, :], b_xt[s], (), (b_xt[s],))
                    P.ts("dve", f1[:], y1[:, s, :], gw12[:, 0, t:t + 1], None, ALU.mult, None, (b_y1[s],), (b_f1,))
                    P.stt("dve", f1[:], y2[:, s, :], gw12[:, 1, t:t + 1], f1[:], ALU.mult, ALU.add,
                          (b_y2[s], b_f1), (b_f1,))
                    P.tt("pool", f1[:], f1[:], g2_bc[:, v, :], ALU.mult, (b_f1, b_w), (b_f1,))
                    P.tt("dve", xo[:, s, :], f1[:], xt[:, s, :], ALU.add, (b_f1, b_xt[s]), (b_xo[s],))
                    if not last:
                        P.dma("sp", X[rows, :], xo[:, s, :], b_xo[s], (b_xo[s],), ())
                    else:
                        P.act(junk[:], xo[:, s, :], AF.Square, (b_xo[s],), (b_junk, b_stat[s]), accum=stat[:, s, 0:1])
                        rms_rstd(stat[:, s, 0:1], stat[:, s, 1:2], D, (b_stat[s],), (b_stat[s],))
                        P.stt("dve", xo[:, s, :], xo[:, s, :], stat[:, s, 1:2], fg_bc[:], ALU.mult, ALU.mult,
                              (b_xo[s], b_stat[s], b_w), (b_xo[s],))
                        P.dma("sp", out[rows, :], xo[:, s, :], b_xo[s], (b_xo[s],), ())
                P.end()

        stages = [s1, s2, s3, s4, s5, s6, s7, s8, s9]
        s0()
        done = cfg.stop is not None and cfg.stop[1] == 0
        for l in range(L):
            if done:
                break
            last = (l == L - 1)
            for si_, fn in enumerate(stages):
                if si_ == 0:
                    fn(l)
                else:
                    fn(l, last)
                if cfg.stop is not None and cfg.stop == (l, si_ + 1):
                    done = True
                    break
            if done:
                break
    return nc


def make_consts(cfg):
    cf = np.zeros((128, 353), np.float32)
    cf[:, 0:128] = np.eye(128, dtype=np.float32)
    cf[:, 128:256] = 1.0
    cf[:, 256:288] = np.arange(32, dtype=np.float32)[None, :]
    cf[:, 288] = np.arange(128, dtype=np.float32)
    cf[:, 289:353] = (np.arange(64, dtype=np.float32) * BLK)[None, :]
    cbm = np.zeros((128, 384), np.float32)
    cbm[:, 0:128] = np.eye(128)
    cbm[:, 128:256] = 1.0
    cbm[:, 256:384] = np.triu(np.ones((128, 128), np.float32), k=1)
    return cf, cbm.astype(ml_dtypes.bfloat16)


def core_inputs(cfg, inputs, b):
    L = cfg.L
    f = lambda a: np.ascontiguousarray(np.asarray(a, dtype=np.float32))
    cosT, sinT = rope_tables(cfg.NL, cfg.NCX)
    cf, cbm = make_consts(cfg)
    m = {
        "x": f(inputs["x"][b]),
        "ctx": f(inputs["ctx"][b]),
        "cc": f(np.stack([np.asarray(inputs["c"][b]), np.asarray(inputs["c_ctx"])], axis=0)),
        "na_bias": na_bias_tables(np.asarray(inputs["na_rpb"], dtype=np.float32)[:L], cfg.variants),
        "w_gate": f(inputs["w_gate"][:L]).reshape(L * NE * D, DE),
        "w_up": f(inputs["w_up"][:L]).reshape(L * NE * D, DE),
        "w_down": f(inputs["w_down"][:L]).reshape(L * NE * DE, D),
        "final_norm_g": f(inputs["final_norm_g"]).reshape(1, D),
        "cosT": cosT, "sinT": sinT, "consts_f": cf, "consts_b": cbm,
    }
    for k in ("w_ada", "b_ada", "norm1_g", "norm2_g", "w_in", "conv_w", "sg_w", "sg_b", "mla_q_norm_g",
              "mla_w_uq", "mla_kv_norm_g", "mla_w_ukv", "out_norm_g", "w_out", "w_grp", "b_grp", "w_exp", "b_exp"):
        m[k] = f(inputs[k][:L])
    return m


_NC_CACHE = {}


def kernel(**inputs):
    cfg = Cfg()
    if "nc" not in _NC_CACHE:
        _NC_CACHE["nc"] = build(cfg)
    nc = _NC_CACHE["nc"]
    per_b = [core_inputs(cfg, inputs, b) for b in range(4)]
    in_maps = [per_b[i % 4] for i in range(8)]
    res = run_bass_kernel_spmd(nc, in_maps, core_ids=list(range(8)))
    return np.stack([np.asarray(res.results[b]["out"], dtype=np.float32) for b in range(4)], axis=0)
```

```python
import numpy as np
import ml_dtypes
from contextlib import ExitStack
import concourse.bass as bass
import concourse.mybir as mybir
from concourse.bass_utils import run_bass_kernel_spmd

F32 = mybir.dt.float32
BF16 = mybir.dt.bfloat16
I32 = mybir.dt.int32
AF = mybir.ActivationFunctionType
ALU = mybir.AluOpType
AX = mybir.AxisListType

D = 1024
KC = 8
IN_COLS = 2464
SG0, NA0, MLA0 = 768, 1280, 2048
NE = 32
DE = 512
BLK = 512
EPS = 1e-6
MLA_SCALE = 96.0 ** -0.5
NEG = -30000.0
SAME_ENG_SYNC = True


class Buf:
    __slots__ = ("name", "writers", "readers", "gen", "excl")

    def __init__(self, name):
        self.name = name
        self.excl = (len(name) > 1 and name[0] == "p" and (name[1].isupper() or name[1] == "m"))
        self.writers = []
        self.readers = []
        self.gen = []


class Op:
    __slots__ = ("eng", "fn", "deps", "is_dma", "dbuf", "signal", "sem", "count")

    def __init__(self, eng, fn, is_dma, dbuf):
        self.eng = eng
        self.fn = fn
        self.deps = []
        self.is_dma = is_dma
        self.dbuf = dbuf
        self.signal = is_dma
        self.sem = None
        self.count = 0


ENGS = ("sp", "act", "dve", "pool", "pe")


class Prog:
    def __init__(self, nc, es, n_dsem=84):
        self.nc = nc
        self.csem = {e: es.enter_context(nc.semaphore("c_" + e)) for e in ("act", "dve", "pool", "pe")}
        self.ccount = {e: 0 for e in self.csem}
        n_sw = 24
        self.dsem = {False: [es.enter_context(nc.semaphore("d%d" % i)) for i in range(n_dsem - n_sw)],
                     True: [es.enter_context(nc.semaphore("w%d" % i)) for i in range(n_sw)]}
        self.dcount = {False: [0] * (n_dsem - n_sw), True: [0] * n_sw}
        self.waited = {e: {} for e in ENGS}
        self.nstage = 0
        self.begin()

    def begin(self):
        self.ops = {e: [] for e in ENGS}
        self.dmap = {}

    def buf(self, name):
        return Buf(name)

    def bufs(self, name, n):
        return [Buf("%s%d" % (name, i)) for i in range(n)]

    def add(self, eng, fn, reads=(), writes=(), dbuf=None, nowaw=False):
        op = Op(eng, fn, dbuf is not None, dbuf)
        deps = []
        xr = [b for b in reads if b.excl]
        reads = [b for b in reads if not b.excl]
        for b in reads:
            deps.extend(b.writers)
        newgen = []
        for b in xr:
            g = list(b.writers) + list(b.readers)
            deps.extend(g)
            newgen.append((b, g))
        writes = tuple(writes) + tuple(xr)
        for b in writes:
            if b in xr:
                continue
            if nowaw and not b.readers and b.writers:
                deps.extend(b.gen)
            else:
                g = list(b.writers) + list(b.readers)
                deps.extend(g)
                newgen.append((b, g))
        seen = set()
        for d in deps:
            if id(d) in seen:
                continue
            seen.add(id(d))
            if d.eng == "pe" and eng == "pe" and not d.is_dma and dbuf is None:
                continue
            if (not SAME_ENG_SYNC) and d.eng == eng and not d.is_dma and dbuf is None:
                continue
            d.signal = True
            op.deps.append(d)
        for b in reads:
            b.readers.append(op)
        ng = {id(b): g for b, g in newgen}
        for b in writes:
            if id(b) in ng:
                b.gen = ng[id(b)]
                b.writers = [op]
                b.readers = []
            else:
                b.writers.append(op)
        self.ops[eng].append(op)
        return op

    def mm(self, out, lhsT, rhs, start, stop, r, w):
        return self.add("pe", lambda e: e.matmul(out, lhsT, rhs, start=start, stop=stop), r, w, nowaw=not start)

    def tr(self, out, in_, ident, r, w, nowaw=True):
        return self.add("pe", lambda e: e.transpose(out, in_, ident), r, w, nowaw=nowaw)

    def act(self, out, in_, func, r, w, bias=None, scale=None, accum=None, nowaw=False):
        kw = {}
        if bias is not None:
            kw["bias"] = bias
        if scale is not None:
            kw["scale"] = scale
        if accum is not None:
            kw["accum_out"] = accum
        return self.add("act", lambda e: e.activation(out, in_, func, **kw), r, w, nowaw=nowaw)

    def tt(self, eng, out, in0, in1, op, r, w, nowaw=False):
        return self.add(eng, lambda e: e.tensor_tensor(out, in0, in1, op), r, w, nowaw=nowaw)

    def ts(self, eng, out, in0, s1, s2, op0, op1, r, w, nowaw=False):
        if s2 is None:
            return self.add(eng, lambda e: e.tensor_scalar(out, in0, s1, None, op0), r, w, nowaw=nowaw)
        return self.add(eng, lambda e: e.tensor_scalar(out, in0, s1, s2, op0, op1), r, w, nowaw=nowaw)

    def stt(self, eng, out, in0, scalar, in1, op0, op1, r, w, nowaw=False):
        return self.add(eng, lambda e: e.scalar_tensor_tensor(out, in0, scalar, in1, op0, op1), r, w, nowaw=nowaw)

    def cp(self, eng, out, in_, r, w, nowaw=False):
        if eng == "act":
            return self.add(eng, lambda e: e.copy(out, in_), r, w, nowaw=nowaw)
        return self.add(eng, lambda e: e.tensor_copy(out, in_), r, w, nowaw=nowaw)

    def memset(self, eng, ap, val, w, nowaw=False):
        return self.add(eng, lambda e: e.memset(ap, val), (), w, nowaw=nowaw)

    def red(self, eng, out, in_, op, r, w, nowaw=False):
        return self.add(eng, lambda e: e.tensor_reduce(out, in_, AX.X, op), r, w, nowaw=nowaw)

    def dma(self, q, out, in_, dbuf, r, w, nowaw=False, slow=False):
        if slow:
            return self.add(q, lambda e: e.dma_start(out=out, in_=in_, allow_slow_non_contiguous=True),
                            r, w, dbuf=dbuf, nowaw=nowaw)
        return self.add(q, lambda e: e.dma_start(out=out, in_=in_), r, w, dbuf=dbuf, nowaw=nowaw)

    def gather(self, out, in_, idx, elem_off, dbuf, r, w, nowaw=False, bound=None):
        if bound is not None:
            return self.add("pool", lambda e: e.indirect_dma_start(
                out=out, out_offset=None, in_=in_,
                in_offset=bass.IndirectOffsetOnAxis(ap=idx, axis=0), element_offset=elem_off,
                bounds_check=bound, oob_is_err=False),
                r, w, dbuf=dbuf, nowaw=nowaw)
        return self.add("pool", lambda e: e.indirect_dma_start(
            out=out, out_offset=None, in_=in_,
            in_offset=bass.IndirectOffsetOnAxis(ap=idx, axis=0), element_offset=elem_off),
            r, w, dbuf=dbuf, nowaw=nowaw)

    def scatter(self, out, idx, in_, dbuf, r, w):
        return self.add("pool", lambda e: e.indirect_dma_start(
            out=out, out_offset=bass.IndirectOffsetOnAxis(ap=idx, axis=0), in_=in_, in_offset=None),
            r, w, dbuf=dbuf)

    def end(self):
        nc = self.nc
        for e in ENGS:
            for op in self.ops[e]:
                if op.is_dma:
                    sw = (e == "pool")
                    k = (id(op.dbuf), sw)
                    if k not in self.dmap:
                        n = sum(1 for kk in self.dmap if kk[1] == sw)
                        assert n < len(self.dsem[sw]), "out of DMA semaphores"
                        self.dmap[k] = n
                    si = self.dmap[k]
                    self.dcount[sw][si] += 16
                    op.sem = self.dsem[sw][si]
                    op.count = self.dcount[sw][si]
                elif op.signal:
                    self.ccount[e] += 1
                    op.sem = self.csem[e]
                    op.count = self.ccount[e]
        final = [(self.dsem[sw][si], self.dcount[sw][si]) for (_, sw), si in self.dmap.items()]
        ops = self.ops
        waited = self.waited
        engmap = {"sp": "sync", "act": "scalar", "dve": "vector", "pool": "gpsimd", "pe": "tensor"}

        def run(ename, eng):
            wd = waited[ename]
            for op in ops[ename]:
                need = {}
                for d in op.deps:
                    k = id(d.sem)
                    if k not in need or need[k][1] < d.count:
                        need[k] = (d.sem, d.count)
                todo = []
                for k, (s, c) in need.items():
                    if wd.get(k, 0) < c:
                        todo.append((s, c))
                        wd[k] = c
                for s, c in todo[:-1]:
                    eng.wait_ge(s, c)
                ins = op.fn(eng)
                if todo:
                    ins._wait_ge(todo[-1][0], todo[-1][1])
                if op.signal:
                    ins.then_inc(op.sem, 16 if op.is_dma else 1)
            if ename == "sp":
                for s, c in final:
                    if wd.get(id(s), 0) < c:
                        eng.wait_ge(s, c)
                        wd[id(s)] = c

        with nc.Block() as block:
            for ename in ENGS:
                getattr(block, engmap[ename])(lambda eng, ename=ename: run(ename, eng))
        self.nstage += 1
        self.begin()


def rope_tables(NL, NCX):
    t = np.arange(NL)
    row = (t // 64).astype(np.float32)
    col = (t % 64).astype(np.float32)
    inv_freq = (np.float32(10000.0) ** (-np.arange(8, dtype=np.float32) / np.float32(8))).astype(np.float32)
    ang_r = row[:, None] * inv_freq
    ang_c = col[:, None] * inv_freq
    ang = np.concatenate([ang_r, ang_r, ang_c, ang_c], axis=-1).astype(np.float32)
    cos = np.cos(ang).astype(np.float32)
    sin = np.sin(ang).astype(np.float32)
    NT = NL + NCX
    cosT = np.zeros((128, NT), np.float32)
    sinT = np.zeros((128, NT), np.float32)
    cosT[64:96, :NL] = cos.T
    sinT[64:96, :NL] = sin.T
    cosT[64:96, NL:] = 1.0
    return cosT, sinT


def na_plan(R):
    def band(r):
        s = min(max(r - 4, 0), R - 8)
        return s
    variants = {}
    plan = []
    for j in range(R // 2):
        rows = (2 * j, 2 * j + 1)
        ms = set()
        for r in rows:
            s = band(r)
            for kr in range(s, s + 8):
                ms.add(kr // 2)
        lst = []
        for m in sorted(ms):
            key = []
            for qp in range(2):
                s = band(rows[qp])
                for kp in range(2):
                    kr = 2 * m + kp
                    key.append((s <= kr < s + 8, kr - rows[qp] + 7))
            key = tuple(key)
            if key not in variants:
                variants[key] = len(variants)
            lst.append((m, variants[key]))
        plan.append(lst)
    return plan, variants


def na_bias_tables(rpb, variants):
    L = rpb.shape[0]
    NV = len(variants)
    qc = np.arange(64)
    kc = np.arange(64)
    c0 = np.clip(qc - 8, 0, 48)
    in_win = (kc[:, None] >= c0[None, :]) & (kc[:, None] < c0[None, :] + 16)
    dc = np.clip(kc[:, None] - qc[None, :] + 15, 0, 30)
    out = np.full((L, 128, 4, NV, 128), NEG, np.float32)
    for key, v in variants.items():
        i = 0
        for qp in range(2):
            for kp in range(2):
                ok, dr = key[i]
                i += 1
                if not ok:
                    continue
                g = rpb[:, :, dr, :][:, :, dc]
                g = np.where(in_win[None, None], g, np.float32(NEG))
                out[:, kp * 64:(kp + 1) * 64, :, v, qp * 64:(qp + 1) * 64] = g.transpose(0, 2, 1, 3)
    return out.astype(ml_dtypes.bfloat16)


class Cfg:
    def __init__(self, NL=8192, NCX=256, L=4, debug=False, stop=None, part=None):
        self.part = part
        self.NL, self.NCX, self.L = NL, NCX, L
        self.NT = NL + NCX
        self.NTL = NL // 128
        self.NTC = NCX // 128
        self.NTT = self.NT // 128
        self.R = NL // 64
        self.debug = debug
        self.stop = stop
        nasg = 2 * self.NT
        self.NBLK = (nasg + NE * (BLK - 1)) // BLK
        self.plan, self.variants = na_plan(self.R)
        self.NV = len(self.variants)

    def groups(self, with_ctx=True):
        gs = []
        for g in range(self.NTL // 4):
            gs.append((list(range(4 * g, 4 * g + 4)), 0))
        if with_ctx:
            gs.append((list(range(self.NTL, self.NTL + self.NTC)), 1))
        return gs


def build(cfg):
    nc = bass.Bass("TRN2", target_bir_lowering=False)
    NL, NCX, NT, L = cfg.NL, cfg.NCX, cfg.NT, cfg.L
    NTT, NTL = cfg.NTT, cfg.NTL
    NV, NBLK = cfg.NV, cfg.NBLK

    def din(name, shape, dt=F32):
        return nc.dram_tensor(name, list(shape), dt, kind="ExternalInput").ap()

    skind = "ExternalOutput" if cfg.debug else "Internal"

    def dscr(name, shape, dt=F32):
        return nc.dram_tensor(name, list(shape), dt, kind=skind).ap()

    x_in = din("x", [NL, D])
    ctx_in = din("ctx", [NCX, D])
    cc_in = din("cc", [2, D])
    w_ada = din("w_ada", [L, D, 6 * D])
    b_ada = din("b_ada", [L, 6 * D])
    norm1_g = din("norm1_g", [L, D])
    norm2_g = din("norm2_g", [L, D])
    w_in = din("w_in", [L, D, IN_COLS])
    conv_w = din("conv_w", [L, 3, 256])
    sg_w = din("sg_w", [L, 4, 128, 128])
    sg_b = din("sg_b", [L, 4, 128])
    na_bias = din("na_bias", [L, 128, 4, NV, 128], BF16)
    q_norm_g = din("mla_q_norm_g", [L, 256])
    w_uq = din("mla_w_uq", [L, 256, 384])
    kv_norm_g = din("mla_kv_norm_g", [L, 128])
    w_ukv = din("mla_w_ukv", [L, 128, 512])
    out_norm_g = din("out_norm_g", [L, D])
    w_out = din("w_out", [L, D, D])
    w_grp = din("w_grp", [L, D, 4])
    b_grp = din("b_grp", [L, 4])
    w_exp = din("w_exp", [L, D, NE])
    b_exp = din("b_exp", [L, NE])
    tiny = cfg.stop is not None and cfg.stop[1] < 8 and cfg.stop[0] == 0
    w_gate = din("w_gate", [L * NE * D, DE] if not tiny else [128, DE])
    w_up = din("w_up", [L * NE * D, DE] if not tiny else [128, DE])
    w_down = din("w_down", [L * NE * DE, D] if not tiny else [128, D])
    final_g = din("final_norm_g", [1, D])
    cosT_in = din("cosT", [128, NT])
    sinT_in = din("sinT", [128, NT])
    consts_f = din("consts_f", [128, 128 + 128 + 32 + 1 + 64])
    consts_b = din("consts_b", [128, 128 + 128 + 128], BF16)

    out = nc.dram_tensor("out", [NL, D], F32, kind="ExternalOutput").ap()

    X = dscr("X", [NT, D])
    MOD = dscr("MOD", [2, 6 * D])
    UT = dscr("UT", [256, NT])
    BGT = dscr("BGT", [256, NT])
    Y = dscr("Y", [NT, D])
    QN_T = dscr("QN_T", [256, NT], BF16)
    KN_T = dscr("KN_T", [256, NT], BF16)
    VN = dscr("VN", [NT, 260], BF16)
    QM_T = dscr("QM_T", [4, 96, NT], BF16)
    KM_T = dscr("KM_T", [4, 96, NT], BF16)
    VM = dscr("VM", [NT, 260], BF16)
    H2 = dscr("H2", [NT, D], BF16)
    XBUF = dscr("XBUF", [NBLK * BLK, D], BF16)
    YBUF = dscr("YBUF", [NBLK * BLK, D])

    es = ExitStack()
    with es:
        P = Prog(nc, es)

        uid = [0]

        def sb(st, name, shape, dt=F32):
            uid[0] += 1
            return st.enter_context(nc.sbuf_tensor("%s_%d" % (name, uid[0]), list(shape), dt))

        def ps(st, name, shape, dt=F32):
            uid[0] += 1
            return st.enter_context(nc.psum_tensor("%s_%d" % (name, uid[0]), list(shape), dt))

        cf = sb(es, "cf", [128, 353])
        cb = sb(es, "cb", [128, 384], BF16)
        ident_f = cf[:, 0:128]
        ones_f = cf[:, 128:256]
        iota_e = cf[:, 256:288]
        iota_p = cf[:, 288:289]
        blkstart = cf[:, 289:353]
        ident_b = cb[:, 0:128]
        ones_b = cb[:, 128:256]
        triu_b = cb[:, 256:384]
        cboth = sb(es, "cboth", [128, KC, 2])
        mask1 = sb(es, "mask1", [128, NTT, NE])
        mask2 = sb(es, "mask2", [128, NTT, NE])
        rank12 = sb(es, "rank12", [128, 2, NTT])
        gw12 = sb(es, "gw12", [128, 2, NTT])
        dest_i = sb(es, "dest_i", [128, 2, NTT], I32)
        carry = sb(es, "carry", [128, NE])
        widx = sb(es, "widx", [128, 2, NBLK], I32)

        def s0():
            with ExitStack() as st:
                craw = sb(st, "craw", [128, KC, 2])
                b_cf, b_cb, b_craw, b_cboth = P.buf("cf"), P.buf("cb"), P.buf("craw"), P.buf("cboth")
                P.dma("sp", cf[:], consts_f, b_cf, (), (b_cf,))
                P.dma("sp", cb[:], consts_b, b_cb, (), (b_cb,))
                for v in range(2):
                    P.dma("sp", craw[:, :, v], cc_in[v].rearrange("(c p) -> p c", p=128), b_craw, (), (b_craw,),
                          nowaw=True, slow=True)
                P.act(cboth[:], craw[:], AF.Silu, (b_craw,), (b_cboth,))
                zt = sb(st, "zt", [128, 4 * D], BF16)
                b_zt = P.buf("zt")
                P.memset("dve", zt[:], 0.0, (b_zt,))
                for b in range(NBLK):
                    P.dma("sp" if b % 2 else "act",
                          XBUF[b * BLK:(b + 1) * BLK, :].rearrange("(p i) d -> p (i d)", p=128), zt[:], b_zt,
                          (b_zt,), ())
                P.end()

        def x_src(l, t):
            if l == 0:
                if t < NTL:
                    return x_in[t * 128:(t + 1) * 128, :]
                return ctx_in[(t - NTL) * 128:(t - NTL + 1) * 128, :]
            return X[t * 128:(t + 1) * 128, :]

        def s1(l):
            with ExitStack() as st:
                wa = sb(st, "wa", [128, 2, KC, 512])
                bada = sb(st, "bada", [2, 6 * D])
                g12 = sb(st, "g12", [2, 2 * D])
                modsb = sb(st, "modsb", [2, 6 * D])
                pm = [ps(st, "pm%d" % i, [128, 512]) for i in range(2)]
                b_wa = P.bufs("wa", 2)
                b_pm = P.bufs("pm", 2)
                b_bada, b_g12, b_mod = P.buf("bada"), P.buf("g12"), P.buf("mod")
                for v in range(2):
                    P.dma("act", bada[v:v + 1, :], b_ada[l:l + 1, :], b_bada, (), (b_bada,), nowaw=True)
                    P.dma("act", g12[v:v + 1, 0:D], norm1_g[l:l + 1, :], b_g12, (), (b_g12,), nowaw=True)
                    P.dma("act", g12[v:v + 1, D:2 * D], norm2_g[l:l + 1, :], b_g12, (), (b_g12,), nowaw=True)
                for j in range(12):
                    s = j % 2
                    P.dma("sp", wa[:, s], w_ada[l][:, j * 512:(j + 1) * 512].rearrange("(c p) n -> p c n", p=128),
                          b_wa[s], (), (b_wa[s],))
                    for c in range(KC):
                        P.mm(pm[s][0:2, :], cboth[:, c, :], wa[:, s, c, :], c == 0, c == KC - 1,
                             (b_wa[s],), (b_pm[s],))
                    P.tt("dve", modsb[:, j * 512:(j + 1) * 512], pm[s][0:2, :], bada[:, j * 512:(j + 1) * 512],
                         ALU.add, (b_pm[s], b_bada), (b_mod,), nowaw=True)
                for k, go in ((1, 0), (4, D)):
                    P.stt("dve", modsb[:, k * D:(k + 1) * D], modsb[:, k * D:(k + 1) * D], 1.0,
                          g12[:, go:go + D], ALU.add, ALU.mult, (b_mod, b_g12), (b_mod,))
                P.dma("sp", MOD, modsb[:], b_mod, (b_mod,), ())
                P.end()

        def load_bc(q, tile_v, k, b):
            for v in range(2):
                P.dma(q, tile_v[:, v, :], MOD[v:v + 1, k * D:(k + 1) * D].partition_broadcast(128), b, (), (b,),
                      nowaw=True)

        def rms_rstd(ssq, rstd, n, r, w):
            P.ts("dve", rstd, ssq, 1.0 / n, EPS, ALU.mult, ALU.add, r, w)
            P.act(rstd, rstd, AF.Sqrt, w, w)
            P.add("dve", lambda e: e.reciprocal(rstd, rstd), w, w)

        def s2(l, last):
            with ExitStack() as st:
                win = sb(st, "win", [128, KC, IN_COLS], BF16)
                wkr = sb(st, "wkr", [128, KC, 2, 96], BF16)
                wuq = sb(st, "wuq", [128, 2, 384], BF16)
                wuqrot = sb(st, "wuqrot", [128, 2, 4, 96], BF16)
                wukv = sb(st, "wukv", [128, 512], BF16)
                wukv_v = sb(st, "wukv_v", [128, 256], BF16)
                sgw32 = sb(st, "sgw32", [128, 4, 128])
                sgwb = sb(st, "sgwb", [128, 4, 128], BF16)
                sgwT = sb(st, "sgwT", [128, 4, 128], BF16)
                sgb = sb(st, "sgb", [128, 4])
                qkvg = sb(st, "qkvg", [128, 3])
                gm_bc = sb(st, "gm_bc", [128, 2, D])
                sh_bc = sb(st, "sh_bc", [128, 2, D])
                cos_t = sb(st, "cos_t", [128, 2, 512])
                sin_t = sb(st, "sin_t", [128, 2, 512])
                xt = sb(st, "xt", [128, 2, D])
                junk = sb(st, "junk", [128, D], BF16)
                xn = sb(st, "xn", [128, 2, D])
                hb = sb(st, "hb", [128, 2, D], BF16)
                hT = sb(st, "hT", [128, 2, KC, 512], BF16)
                stat = sb(st, "stat", [128, 2, 8])
                cg_sb = sb(st, "cg_sb", [128, 2, 512])
                u_sb = sb(st, "u_sb", [128, 2, 512])
                bg_sb = sb(st, "bg_sb", [128, 2, 512])
                qk_sb = sb(st, "qk_sb", [128, 4, 512], BF16)
                vaug = sb(st, "vaug", [128, 2, 4, 65], BF16)
                vaug2 = sb(st, "vaug2", [128, 2, 4, 65], BF16)
                zb = sb(st, "zb", [128, 512])
                gt1 = sb(st, "gt1", [128, 512])
                gt2 = sb(st, "gt2", [128, 512])
                gg = sb(st, "gg", [128, 512])
                vn = sb(st, "vn", [128, 256], BF16)
                yb = sb(st, "yb", [128, 2, 256])
                cq_b = sb(st, "cq_b", [128, 2, 384], BF16)
                cqnT = sb(st, "cqnT", [128, 3, 512], BF16)
                rt1 = sb(st, "rt1", [128, 512])
                rt2 = sb(st, "rt2", [128, 512])
                qT = sb(st, "qT", [128, 4, 512], BF16)
                kn_sb = sb(st, "kn_sb", [128, 4, 512], BF16)
                kr_sb = sb(st, "kr_sb", [128, 512], BF16)
                pA = [ps(st, "pA%d" % i, [128, 1024], BF16) for i in range(4)]
                pB = [ps(st, "pB%d" % i, [128, 512]) for i in range(4)]
                b_pA = P.bufs("pA", 4)
                b_pB = P.bufs("pB", 4)
                b_w = P.buf("w")
                b_xt = P.bufs("xt", 2)
                b_stat = P.bufs("stat", 2)
                b_junk, b_xn = P.buf("junk"), P.bufs("xn", 2)
                b_hb = P.bufs("hb", 2)
                b_hT = P.bufs("hT", 2)
                b_cs = P.bufs("cs", 2)
                b_cg, b_u, b_bg, b_qk = P.bufs("cg", 2), P.bufs("u", 2), P.bufs("bg", 2), P.bufs("qk", 4)
                b_va, b_va2 = P.bufs("va", 2), P.bufs("va2", 2)
                b_zb, b_gt1, b_gt2, b_gg, b_vn = P.buf("zb"), P.buf("gt1"), P.buf("gt2"), P.buf("gg"), P.buf("vn")
                b_yb = P.bufs("yb", 2)
                b_cqb = P.bufs("cqb", 2)
                b_cqnT = P.buf("cqnT")
                b_rt1, b_rt2 = P.buf("rt1"), P.buf("rt2")
                b_qT = P.bufs("qT", 4)
                b_kn = P.bufs("kn", 4)
                b_kr = P.buf("kr")

                for c in range(KC):
                    for h2 in range(2):
                        P.dma("pool", win[:, c, h2 * 1232:(h2 + 1) * 1232],
                              w_in[l][c * 128:(c + 1) * 128, h2 * 1232:(h2 + 1) * 1232], b_w, (), (b_w,), nowaw=True)
                for c in range(2):
                    P.dma("pool", wuq[:, c, :], w_uq[l][c * 128:(c + 1) * 128, :], b_w, (), (b_w,), nowaw=True)
                P.dma("pool", wukv[:], w_ukv[l], b_w, (), (b_w,), nowaw=True)
                P.dma("act", sgw32[:], sg_w[l].rearrange("h p q -> p h q"), b_w, (), (b_w,), nowaw=True)
                P.dma("act", sgb[:], sg_b[l].rearrange("h p -> p h"), b_w, (), (b_w,), nowaw=True, slow=True)
                P.dma("act", qkvg[:, 0:2], q_norm_g[l].rearrange("(c p) -> p c", p=128), b_w, (), (b_w,),
                      nowaw=True, slow=True)
                P.dma("act", qkvg[:, 2:3], kv_norm_g[l].rearrange("(c p) -> p c", p=128), b_w, (), (b_w,),
                      nowaw=True, slow=True)
                load_bc("act", gm_bc, 1, b_w)
                load_bc("act", sh_bc, 0, b_w)
                b_wd = P.buf("wd")
                P.memset("pool", wkr[:], 0.0, (b_wd,))
                P.memset("pool", wuqrot[:], 0.0, (b_wd,))
                KR0 = MLA0 + 384
                P.cp("dve", wkr[:, :, 0, 64:96], win[:, :, KR0:KR0 + 32], (b_w,), (b_wd,))
                for (dst, src, sgn) in ((0, 8, -1.0), (8, 0, 1.0), (16, 24, -1.0), (24, 16, 1.0)):
                    P.ts("dve", wkr[:, :, 1, 64 + dst:72 + dst], win[:, :, KR0 + src:KR0 + src + 8], sgn, None,
                         ALU.mult, None, (b_w,), (b_wd,))
                    for h in range(4):
                        P.ts("dve", wuqrot[:, :, h, 64 + dst:72 + dst],
                             wuq[:, :, h * 96 + 64 + src:h * 96 + 72 + src], sgn, None, ALU.mult, None,
                             (b_w,), (b_wd,))
                for h in range(4):
                    P.cp("dve", wukv_v[:, h * 64:(h + 1) * 64], wukv[:, h * 128 + 64:(h + 1) * 128], (b_w,), (b_wd,))
                P.cp("dve", sgwb[:], sgw32[:], (b_w,), (b_wd,))
                for h in range(4):
                    P.tr(pA[0][:, h * 128:(h + 1) * 128], sgwb[:, h, :], ident_b, (b_wd,), (b_pA[0],), nowaw=(h > 0))
                P.cp("dve", sgwT[:].rearrange("p h q -> p (h q)"), pA[0][:, 0:512], (b_pA[0],), (b_wd,))
                for s in range(2):
                    P.memset("pool", vaug[:, s, :, 64:65], 1.0, (b_va[s],))
                    P.memset("pool", vaug2[:, s, :, 64:65], 1.0, (b_va2[s],))

                pbi = [0]

                def nextpb():
                    i = pbi[0] % 4
                    pbi[0] += 1
                    return i

                evi = [0]

                def evac_eng():
                    evi[0] += 1
                    return "act" if evi[0] % 2 else "dve"

                groups = cfg.groups(True)
                def front(gi, tiles, v):
                    nt = len(tiles)
                    N = 128 * nt
                    t0 = tiles[0] * 128
                    gs = gi % 2
                    P.dma("sp", cos_t[:, gs, 0:N], cosT_in[:, t0:t0 + N], b_cs[gs], (), (b_cs[gs],), nowaw=False)
                    P.dma("sp", sin_t[:, gs, 0:N], sinT_in[:, t0:t0 + N], b_cs[gs], (), (b_cs[gs],), nowaw=True)
                    for i, t in enumerate(tiles):
                        s = (gi * 4 + i) % 2
                        P.dma("sp", xt[:, s, :], x_src(l, t), b_xt[s], (), (b_xt[s],))
                        P.act(junk[:], xt[:, s, :], AF.Square, (b_xt[s],), (b_junk, b_stat[s]), accum=stat[:, s, 0:1])
                        rms_rstd(stat[:, s, 0:1], stat[:, s, 1:2], D, (b_stat[s],), (b_stat[s],))
                        P.stt("dve", xn[:, s, :], xt[:, s, :], stat[:, s, 1:2], gm_bc[:, v, :], ALU.mult, ALU.mult,
                              (b_xt[s], b_stat[s], b_w), (b_xn[s],))
                        P.tt("pool", hb[:, s, :], xn[:, s, :], sh_bc[:, v, :], ALU.add, (b_xn[s], b_w), (b_hb[s],))
                        for c in range(KC):
                            P.tr(pA[c // 2][:, (c % 2) * 512 + i * 128:(c % 2) * 512 + (i + 1) * 128],
                                 hb[:, s, c * 128:(c + 1) * 128], ident_b, (b_hb[s],), (b_pA[c // 2],),
                                 nowaw=not (i == 0 and c % 2 == 0))
                    for c in range(KC):
                        P.cp("act" if (c // 2) % 2 == 0 else "dve", hT[:, gs, c, 0:N],
                             pA[c // 2][:, (c % 2) * 512:(c % 2) * 512 + N],
                             (b_pA[c // 2],), (b_hT[gs],), nowaw=(c > 0))


                def back(gi, tiles, v):
                    nt = len(tiles)
                    N = 128 * nt
                    t0 = tiles[0] * 128
                    gs = gi % 2
                    def fm_block(col0, width=128):
                        pi = nextpb()
                        for c in range(KC):
                            P.mm(pB[pi][0:width, 0:N], win[:, c, col0:col0 + width], hT[:, gs, c, 0:N],
                                 c == 0, c == KC - 1, (b_w, b_hT[gs]), (b_pB[pi],))
                        return pi

                    for blk in range(2):
                        pi = fm_block(blk * 128)
                        P.cp("act", bg_sb[:, blk, 0:N], pB[pi][:, 0:N], (b_pB[pi],), (b_bg[blk],))
                        P.dma("sp", BGT[blk * 128:(blk + 1) * 128, t0:t0 + N], bg_sb[:, blk, 0:N], b_bg[blk],
                              (b_bg[blk],), ())
                    for blk in range(2):
                        pi = fm_block(256 + blk * 128)
                        P.cp("act", cg_sb[:, blk, 0:N], pB[pi][:, 0:N], (b_pB[pi],), (b_cg[blk],))
                    for blk in range(2):
                        pi = fm_block(512 + blk * 128)
                        P.tt("dve", u_sb[:, blk, 0:N], pB[pi][:, 0:N], cg_sb[:, blk, 0:N], ALU.mult,
                             (b_pB[pi], b_cg[blk]), (b_u[blk],))
                        P.dma("sp", UT[blk * 128:(blk + 1) * 128, t0:t0 + N], u_sb[:, blk, 0:N], b_u[blk],
                              (b_u[blk],), ())
                    for blk in range(2):
                        pi = fm_block(NA0 + blk * 128)
                        P.act(qk_sb[:, blk, 0:N], pB[pi][:, 0:N], AF.Copy, (b_pB[pi],), (b_qk[blk],), scale=0.125)
                        P.dma("sp", QN_T[blk * 128:(blk + 1) * 128, t0:t0 + N], qk_sb[:, blk, 0:N], b_qk[blk],
                              (b_qk[blk],), ())
                    for blk in range(2):
                        pi = fm_block(NA0 + 256 + blk * 128)
                        P.cp("act", qk_sb[:, 2 + blk, 0:N], pB[pi][:, 0:N], (b_pB[pi],), (b_qk[2 + blk],))
                        P.dma("sp", KN_T[blk * 128:(blk + 1) * 128, t0:t0 + N], qk_sb[:, 2 + blk, 0:N], b_qk[2 + blk],
                              (b_qk[2 + blk],), ())
                    pr = []
                    for j in range(2):
                        pi = nextpb()
                        for c in range(KC):
                            P.mm(pB[pi][0:96, 0:N], wkr[:, c, j, :], hT[:, gs, c, 0:N], c == 0, c == KC - 1,
                                 (b_wd, b_hT[gs]), (b_pB[pi],))
                        pr.append(pi)
                    P.tt("dve", rt1[64:96, 0:N], pB[pr[0]][64:96, 0:N], cos_t[64:96, gs, 0:N], ALU.mult,
                         (b_pB[pr[0]], b_cs[gs]), (b_rt1,))
                    P.tt("dve", rt2[64:96, 0:N], pB[pr[1]][64:96, 0:N], sin_t[64:96, gs, 0:N], ALU.mult,
                         (b_pB[pr[1]], b_cs[gs]), (b_rt2,))
                    P.tt("pool", kr_sb[64:96, 0:N], rt1[64:96, 0:N], rt2[64:96, 0:N], ALU.add,
                         (b_rt1, b_rt2), (b_kr,))
                    for h in range(4):
                        P.dma("sp", KM_T[h, 64:96, t0:t0 + N], kr_sb[64:96, 0:N], b_kr, (b_kr,), ())

                    for i, t in enumerate(tiles):
                        s = (gi * 4 + i) % 2
                        tok = slice(i * 128, (i + 1) * 128)
                        pi = nextpb()
                        for c in range(KC):
                            P.mm(pB[pi][:, 0:512], hT[:, gs, c, tok], win[:, c, SG0:SG0 + 512], c == 0, c == KC - 1,
                                 (b_w, b_hT[gs]), (b_pB[pi],))
                        P.cp("act", zb[:], pB[pi][:, 0:512], (b_pB[pi],), (b_zb,))
                        P.tt("dve", gt1[:], zb[:], zb[:], ALU.mult, (b_zb,), (b_gt1,))
                        P.ts("dve", gt1[:], gt1[:], 0.044715, 1.0, ALU.mult, ALU.add, (b_gt1,), (b_gt1,))
                        P.tt("dve", gt1[:], gt1[:], zb[:], ALU.mult, (b_gt1, b_zb), (b_gt1,))
                        P.act(gt2[:], gt1[:], AF.Sigmoid, (b_gt1,), (b_gt2,), scale=1.5957691216057308)
                        P.tt("dve", gg[:], gt2[:], zb[:], ALU.mult, (b_gt2, b_zb), (b_gg,))
                        P.red("dve", stat[:, s, 2:3], gg[:, 256:512], ALU.add, (b_gg,), (b_stat[s],))
                        P.act(junk[:, 0:256], gg[:, 256:512], AF.Square, (b_gg,), (b_junk, b_stat[s]),
                              accum=stat[:, s, 3:4])
                        P.ts("dve", stat[:, s, 2:3], stat[:, s, 2:3], 1.0 / 256, None, ALU.mult, None,
                             (b_stat[s],), (b_stat[s],))
                        P.tt("dve", stat[:, s, 4:5], stat[:, s, 2:3], stat[:, s, 2:3], ALU.mult,
                             (b_stat[s],), (b_stat[s],))
                        P.stt("dve", stat[:, s, 3:4], stat[:, s, 3:4], 1.0 / 256, stat[:, s, 4:5],
                              ALU.mult, ALU.subtract, (b_stat[s],), (b_stat[s],))
                        P.ts("dve", stat[:, s, 3:4], stat[:, s, 3:4], EPS, None, ALU.add, None,
                             (b_stat[s],), (b_stat[s],))
                        P.act(stat[:, s, 3:4], stat[:, s, 3:4], AF.Sqrt, (b_stat[s],), (b_stat[s],))
                        P.add("dve", lambda e, s=s: e.reciprocal(stat[:, s, 3:4], stat[:, s, 3:4]),
                              (b_stat[s],), (b_stat[s],))
                        P.ts("dve", vn[:], gg[:, 256:512], stat[:, s, 2:3], stat[:, s, 3:4], ALU.subtract, ALU.mult,
                             (b_gg, b_stat[s]), (b_vn,))
                        pj = nextpb()
                        for h in range(4):
                            P.mm(pB[pj][:, h * 64:(h + 1) * 64], sgwT[:, h, :], vn[:, h * 64:(h + 1) * 64],
                                 True, True, (b_wd, b_vn), (b_pB[pj],))
                        for h in range(4):
                            P.stt("dve", yb[:, s, h * 64:(h + 1) * 64], pB[pj][:, h * 64:(h + 1) * 64],
                                  sgb[:, h:h + 1], gg[:, h * 64:(h + 1) * 64], ALU.add, ALU.mult,
                                  (b_pB[pj], b_gg, b_w), (b_yb[s],), nowaw=(h > 0))
                        P.dma("sp", Y[t * 128:(t + 1) * 128, 256:512], yb[:, s, :], b_yb[s], (b_yb[s],), ())
                        pi = nextpb()
                        for c in range(KC):
                            P.mm(pB[pi][:, 0:256], hT[:, gs, c, tok], win[:, c, NA0 + 512:NA0 + 768], c == 0,
                                 c == KC - 1, (b_w, b_hT[gs]), (b_pB[pi],))
                        P.cp("act", vaug[:, s, :, 0:64], pB[pi][:, 0:256].rearrange("p (h d) -> p h d", h=4),
                             (b_pB[pi],), (b_va[s],))
                        P.dma("sp", VN[t * 128:(t + 1) * 128, :], vaug[:, s].rearrange("p h d -> p (h d)"),
                              b_va[s], (b_va[s],), ())
                        pi = nextpb()
                        for c in range(KC):
                            P.mm(pB[pi][:, 0:384], hT[:, gs, c, tok], win[:, c, MLA0:MLA0 + 384], c == 0,
                                 c == KC - 1, (b_w, b_hT[gs]), (b_pB[pi],))
                        P.act(junk[:, 0:256], pB[pi][:, 0:256], AF.Square, (b_pB[pi],), (b_junk, b_stat[s]),
                              accum=stat[:, s, 5:6])
                        P.act(junk[:, 256:384], pB[pi][:, 256:384], AF.Square, (b_pB[pi],), (b_junk, b_stat[s]),
                              accum=stat[:, s, 6:7])
                        rms_rstd(stat[:, s, 5:6], stat[:, s, 5:6], 256, (b_stat[s],), (b_stat[s],))
                        rms_rstd(stat[:, s, 6:7], stat[:, s, 6:7], 128, (b_stat[s],), (b_stat[s],))
                        P.act(cq_b[:, s, 0:256], pB[pi][:, 0:256], AF.Copy, (b_pB[pi], b_stat[s]), (b_cqb[s],),
                              scale=stat[:, s, 5:6])
                        P.act(cq_b[:, s, 256:384], pB[pi][:, 256:384], AF.Copy, (b_pB[pi], b_stat[s]), (b_cqb[s],),
                              scale=stat[:, s, 6:7], nowaw=True)
                        for b3 in range(3):
                            P.tr(pA[b3][:, i * 128:(i + 1) * 128], cq_b[:, s, b3 * 128:(b3 + 1) * 128], ident_b,
                                 (b_cqb[s],), (b_pA[b3],), nowaw=(i > 0))
                    for b3 in range(3):
                        P.ts("dve", cqnT[:, b3, 0:N], pA[b3][:, 0:N], qkvg[:, b3:b3 + 1], None, ALU.mult, None,
                             (b_pA[b3], b_w), (b_cqnT,), nowaw=(b3 > 0))
                    for h in range(4):
                        hs = h
                        p1 = nextpb()
                        for c in range(2):
                            P.mm(pB[p1][0:96, 0:N], wuq[:, c, h * 96:(h + 1) * 96], cqnT[:, c, 0:N], c == 0, c == 1,
                                 (b_w, b_cqnT), (b_pB[p1],))
                        p2 = nextpb()
                        for c in range(2):
                            P.mm(pB[p2][0:96, 0:N], wuqrot[:, c, h, :], cqnT[:, c, 0:N], c == 0, c == 1,
                                 (b_wd, b_cqnT), (b_pB[p2],))
                        P.cp("act", qT[0:64, hs, 0:N], pB[p1][0:64, 0:N], (b_pB[p1],), (b_qT[hs],))
                        P.tt("dve", rt1[64:96, 0:N], pB[p1][64:96, 0:N], cos_t[64:96, gs, 0:N], ALU.mult,
                             (b_pB[p1], b_cs[gs]), (b_rt1,))
                        P.tt("dve", rt2[64:96, 0:N], pB[p2][64:96, 0:N], sin_t[64:96, gs, 0:N], ALU.mult,
                             (b_pB[p2], b_cs[gs]), (b_rt2,))
                        P.tt("pool", qT[64:96, hs, 0:N], rt1[64:96, 0:N], rt2[64:96, 0:N], ALU.add,
                             (b_rt1, b_rt2), (b_qT[hs],), nowaw=True)
                        P.dma("sp", QM_T[h, :, t0:t0 + N], qT[0:96, hs, 0:N], b_qT[hs], (b_qT[hs],), ())
                    for h in range(4):
                        hs = h
                        pi = nextpb()
                        P.mm(pB[pi][0:64, 0:N], wukv[:, h * 128:h * 128 + 64], cqnT[:, 2, 0:N], True, True,
                             (b_w, b_cqnT), (b_pB[pi],))
                        P.cp("act", kn_sb[0:64, hs, 0:N], pB[pi][0:64, 0:N], (b_pB[pi],), (b_kn[hs],))
                        P.dma("sp", KM_T[h, 0:64, t0:t0 + N], kn_sb[0:64, hs, 0:N], b_kn[hs], (b_kn[hs],), ())
                    for i, t in enumerate(tiles):
                        s = (gi * 4 + i) % 2
                        pi = nextpb()
                        P.mm(pB[pi][:, 0:256], cqnT[:, 2, i * 128:(i + 1) * 128], wukv_v[:], True, True,
                             (b_wd, b_cqnT), (b_pB[pi],))
                        P.cp("act", vaug2[:, s, :, 0:64], pB[pi][:, 0:256].rearrange("p (h d) -> p h d", h=4),
                             (b_pB[pi],), (b_va2[s],))
                        P.dma("sp", VM[t * 128:(t + 1) * 128, :], vaug2[:, s].rearrange("p h d -> p (h d)"),
                              b_va2[s], (b_va2[s],), ())
                if groups:
                    front(0, *groups[0])
                for gi, (tiles, v) in enumerate(groups):
                    if gi + 1 < len(groups):
                        front(gi + 1, *groups[gi + 1])
                    back(gi, tiles, v)
                P.end()
        def s3(l, last):
            with ExitStack() as st:
                ut = sb(st, "ut", [128, 2, 2, 514])
                bgt = sb(st, "bgt", [128, 2, 2, 512])
                cw = sb(st, "cw", [128, 2, 3])
                acc = sb(st, "acc", [128, 2, 512])
                ya = sb(st, "ya", [128, 2, 512])
                yat = sb(st, "yat", [128, 2, 256])
                pF = [ps(st, "pF%d" % i, [128, 512]) for i in range(2)]
                b_ut, b_bgt = P.bufs("ut", 2), P.bufs("bgt", 2)
                b_cw, b_acc, b_ya = P.buf("cw"), P.bufs("acc", 2), P.bufs("ya", 2)
                b_yat, b_pF = P.bufs("yat", 2), P.bufs("pF", 2)
                for blk in range(2):
                    for k3 in range(3):
                        P.dma("act", cw[:, blk, k3:k3 + 1],
                              conv_w[l][k3, blk * 128:(blk + 1) * 128].rearrange("(p o) -> p o", o=1),
                              b_cw, (), (b_cw,), nowaw=True, slow=True)
                cnt = 0
                for gi, (tiles, v) in enumerate(cfg.groups(not last)):
                    nt = len(tiles)
                    N = 128 * nt
                    t0 = tiles[0] * 128
                    s0_, s1_ = (0, NL) if v == 0 else (NL, NT)
                    gs = gi % 2
                    lo = max(t0 - 1, s0_)
                    hi = min(t0 + N + 1, s1_)
                    off = lo - (t0 - 1)
                    if t0 - 1 < s0_:
                        P.memset("pool", ut[:, gs, :, 0:1], 0.0, (b_ut[gs],))
                    if t0 + N + 1 > s1_:
                        P.memset("pool", ut[:, gs, :, N + 1:N + 2], 0.0, (b_ut[gs],), nowaw=True)
                    for blk in range(2):
                        P.dma("sp", ut[:, gs, blk, off:off + hi - lo], UT[blk * 128:(blk + 1) * 128, lo:hi],
                              b_ut[gs], (), (b_ut[gs],), nowaw=True)
                        P.dma("sp", bgt[:, gs, blk, 0:N], BGT[blk * 128:(blk + 1) * 128, t0:t0 + N],
                              b_bgt[gs], (), (b_bgt[gs],), nowaw=True)
                    for blk in range(2):
                        P.ts("dve", acc[:, blk, 0:N], ut[:, gs, blk, 0:N], cw[:, blk, 0:1], None, ALU.mult, None,
                             (b_ut[gs], b_cw), (b_acc[blk],))
                        P.stt("dve", acc[:, blk, 0:N], ut[:, gs, blk, 1:N + 1], cw[:, blk, 1:2], acc[:, blk, 0:N],
                              ALU.mult, ALU.add, (b_ut[gs], b_cw, b_acc[blk]), (b_acc[blk],))
                        P.stt("dve", acc[:, blk, 0:N], ut[:, gs, blk, 2:N + 2], cw[:, blk, 2:3], acc[:, blk, 0:N],
                              ALU.mult, ALU.add, (b_ut[gs], b_cw, b_acc[blk]), (b_acc[blk],))
                        P.tt("pool", ya[:, blk, 0:N], acc[:, blk, 0:N], bgt[:, gs, blk, 0:N], ALU.mult,
                             (b_acc[blk], b_bgt[gs]), (b_ya[blk],))
                    for i, t in enumerate(tiles):
                        s = cnt % 2
                        cnt += 1
                        for blk in range(2):
                            P.tr(pF[s][:, blk * 128:(blk + 1) * 128], ya[:, blk, i * 128:(i + 1) * 128], ident_f,
                                 (b_ya[blk],), (b_pF[s],), nowaw=(blk > 0))
                        P.cp("act", yat[:, s, :], pF[s][:, 0:256], (b_pF[s],), (b_yat[s],))
                        P.dma("sp", Y[t * 128:(t + 1) * 128, 0:256], yat[:, s, :], b_yat[s], (b_yat[s],), ())
                P.end()

        def s4(l, last):
            with ExitStack() as st:
                kn = sb(st, "kn", [128, 2, NT], BF16)
                vns = sb(st, "vns", [128, NTT, 260], BF16)
                bias = sb(st, "bias", [128, 4, NV, 128], BF16)
                qt = sb(st, "qt", [128, 2, 2, 128], BF16)
                pp = sb(st, "pp", [128, 2, 8, 128], BF16)
                yc = sb(st, "yc", [128, 2, 256])
                rec = sb(st, "rec", [128, 2, 4])
                pS = [ps(st, "pS%d" % i, [128, 2, 512]) for i in range(2)]
                pO = [ps(st, "pO%d" % i, [128, 512]) for i in range(2)]
                b_kv, b_bias = P.buf("kv"), P.buf("bias")
                b_qt, b_pp, b_yc, b_rec = P.bufs("qt", 2), P.bufs("pp", 2), P.bufs("yc", 2), P.bufs("rec", 2)
                b_pS, b_pO = P.bufs("pS", 2), P.bufs("pO", 2)
                for blk in range(2):
                    for h0 in range(0, NT, 2048):
                        h1 = min(NT, h0 + 2048)
                        P.dma("sp", kn[:, blk, h0:h1], KN_T[blk * 128:(blk + 1) * 128, h0:h1], b_kv, (), (b_kv,),
                              nowaw=True)
                for t0_ in range(0, NTT, 8):
                    t1_ = min(NTT, t0_ + 8)
                    P.dma("act", vns[:, t0_:t1_, :], VN[t0_ * 128:t1_ * 128, :].rearrange("(t p) f -> p t f", p=128),
                          b_kv, (), (b_kv,), nowaw=True)
                P.dma("act", bias[:].rearrange("p h v q -> p (h v q)"),
                      na_bias[l].rearrange("p h v q -> p (h v q)"), b_bias, (), (b_bias,))
                tiles = list(range(NTL)) + ([] if last else list(range(NTL, NTT)))
                ctx_chunks = [(m, None) for m in range(NTL, NTT)]
                cnt = 0
                for ti, t in enumerate(tiles):
                    chunks = (cfg.plan[t] + ctx_chunks) if t < NTL else ctx_chunks
                    nch = len(chunks)
                    s = ti % 2
                    P.dma("sp", qt[:, s], QN_T[:, t * 128:(t + 1) * 128].rearrange("(b p) q -> p b q", p=128),
                          b_qt[s], (), (b_qt[s],))
                    for h in range(4):
                        u = cnt % 2
                        cnt += 1
                        hb_, base = h // 2, 64 * (h % 2)
                        for i, (m, var) in enumerate(chunks):
                            o = pS[u][:, i // 4, (i % 4) * 128:(i % 4 + 1) * 128]
                            P.mm(o, kn[base:base + 64, hb_, m * 128:(m + 1) * 128], qt[base:base + 64, s, hb_, :],
                                 True, var is None, (b_kv, b_qt[s]), (b_pS[u],))
                            if var is not None:
                                P.mm(o, ident_b, bias[:, h, var, :], False, True, (b_bias,), (b_pS[u],))
                        P.act(pp[:, u, 0:nch, :], pS[u][:].rearrange("p a (b q) -> p (a b) q", q=128)[:, 0:nch, :],
                              AF.Exp, (b_pS[u],), (b_pp[u],))
                        for i, (m, var) in enumerate(chunks):
                            P.mm(pO[s][:, h * 65:(h + 1) * 65], pp[:, u, i, :], vns[:, m, h * 65:(h + 1) * 65],
                                 i == 0, i == nch - 1, (b_pp[u], b_kv), (b_pO[s],))
                    cnt = cnt
                    o4 = pO[s][:, 0:260].rearrange("p (h d) -> p h d", h=4)
                    P.add("dve", lambda e, o4=o4, s=s: e.reciprocal(rec[:, s, :], o4[:, :, 64]),
                          (b_pO[s],), (b_rec[s],))
                    for h in range(4):
                        P.ts("dve", yc[:, s, h * 64:(h + 1) * 64], pO[s][:, h * 65:h * 65 + 64], rec[:, s, h:h + 1],
                             None, ALU.mult, None, (b_pO[s], b_rec[s]), (b_yc[s],), nowaw=(h > 0))
                    P.dma("sp", Y[t * 128:(t + 1) * 128, 512:768], yc[:, s, :], b_yc[s], (b_yc[s],), ())
                P.end()

        def s5(l, last):
            with ExitStack() as st:
                km = sb(st, "km", [128, 4, NT], BF16)
                vms = sb(st, "vms", [128, NTT, 260], BF16)
                qm = sb(st, "qm", [128, 2, 4, 512], BF16)
                pp = sb(st, "pp", [128, 4, 512], BF16)
                yd = sb(st, "yd", [128, 2, 4, 256])
                rec = sb(st, "rec", [128, 2, 4])
                NS = 5
                pS = [ps(st, "pS%d" % i, [128, 512]) for i in range(NS)]
                pO = [ps(st, "pO%d" % i, [128, 512]) for i in range(2)]
                b_kv = P.buf("kv")
                b_qm, b_pp, b_yd, b_rec = P.bufs("qm", 2), P.bufs("pp", 4), P.bufs("yd", 2), P.bufs("rec", 2)
                b_pS, b_pO = P.bufs("pS", NS), P.bufs("pO", 2)
                for h in range(4):
                    for h0 in range(0, NT, 2048):
                        h1 = min(NT, h0 + 2048)
                        P.dma("sp", km[0:96, h, h0:h1], KM_T[h, :, h0:h1], b_kv, (), (b_kv,), nowaw=True)
                for t0_ in range(0, NTT, 8):
                    t1_ = min(NTT, t0_ + 8)
                    P.dma("act", vms[:, t0_:t1_, :], VM[t0_ * 128:t1_ * 128, :].rearrange("(t p) f -> p t f", p=128),
                          b_kv, (), (b_kv,), nowaw=True)
                all_chunks = list(range(NTT))
                ctx_only = list(range(NTL, NTT))
                cnt = 0
                si = 0
                for gi, (tiles, v) in enumerate(cfg.groups(not last)):
                    nt = len(tiles)
                    N = 128 * nt
                    t0 = tiles[0] * 128
                    gs = gi % 2
                    chunks = all_chunks if v == 0 else ctx_only
                    nch = len(chunks)
                    for h in range(4):
                        P.dma("sp", qm[0:96, gs, h, 0:N], QM_T[h, :, t0:t0 + N], b_qm[gs], (), (b_qm[gs],), nowaw=True)
                    for h in range(4):
                        u = cnt % 2
                        cnt += 1
                        pend = []

                        def qk(ci):
                            nonlocal si
                            m = chunks[ci]
                            k = si % NS
                            si += 1
                            P.mm(pS[k][:, 0:N], km[0:96, h, m * 128:(m + 1) * 128], qm[0:96, gs, h, 0:N], True, True,
                                 (b_kv, b_qm[gs]), (b_pS[k],))
                            pend.append((ci, k))

                        def pv():
                            ci, k = pend.pop(0)
                            m = chunks[ci]
                            pslot = ci % 4
                            P.act(pp[:, pslot, 0:N], pS[k][:, 0:N], AF.Exp, (b_pS[k],), (b_pp[pslot],), scale=MLA_SCALE)
                            for sub in range(nt):
                                P.mm(pO[u][:, sub * 65:(sub + 1) * 65], pp[:, pslot, sub * 128:(sub + 1) * 128],
                                     vms[:, m, h * 65:(h + 1) * 65], ci == 0 and sub == 0,
                                     ci == nch - 1 and sub == nt - 1,
                                     (b_pp[pslot], b_kv), (b_pO[u],))

                        LOOK = 2
                        for ci in range(nch):
                            qk(ci)
                            if len(pend) > LOOK:
                                pv()
                        while pend:
                            pv()
                        o4 = pO[u][:, 0:nt * 65].rearrange("p (s d) -> p s d", d=65)
                        P.add("dve", lambda e, o4=o4, u=u, nt=nt: e.reciprocal(rec[:, u, 0:nt], o4[:, :, 64]),
                              (b_pO[u],), (b_rec[u],))
                        for sub in range(nt):
                            P.ts("dve", yd[:, gs, sub, h * 64:(h + 1) * 64], pO[u][:, sub * 65:sub * 65 + 64],
                                 rec[:, u, sub:sub + 1], None, ALU.mult, None, (b_pO[u], b_rec[u]), (b_yd[gs],),
                                 nowaw=not (h == 0 and sub == 0))
                    for sub, t in enumerate(tiles):
                        P.dma("sp", Y[t * 128:(t + 1) * 128, 768:1024], yd[:, gs, sub, :], b_yd[gs], (b_yd[gs],), ())
                P.end()
        def s6(l, last):
            with ExitStack() as st:
                wout = sb(st, "wout", [128, KC, D], BF16)
                wr = sb(st, "wr", [128, KC, 36], BF16)
                brt = sb(st, "brt", [128, 36])
                outg = sb(st, "outg", [128, KC])
                g1_bc = sb(st, "g1_bc", [128, 2, D])
                gm2_bc = sb(st, "gm2_bc", [128, 2, D])
                sh2_bc = sb(st, "sh2_bc", [128, 2, D])
                yt = sb(st, "yt", [128, 2, D])
                junk = sb(st, "junk", [128, D], BF16)
                ynb = sb(st, "ynb", [128, 2, D], BF16)
                ynT = sb(st, "ynT", [128, 2, KC, 128], BF16)
                xt = sb(st, "xt", [128, 2, D])
                xnew = sb(st, "xnew", [128, 2, D])
                tmp = sb(st, "tmp", [128, D])
                h2b = sb(st, "h2b", [128, 2, D], BF16)
                h2T = sb(st, "h2T", [128, 2, KC, 128], BF16)
                stat = sb(st, "stat", [128, 2, 16])
                lg = sb(st, "lg", [128, 2, 36])
                rtmp = sb(st, "rtmp", [128, 2, 4, NE])
                amask = sb(st, "amask", [128, 2, NE], BF16)
                pA = [ps(st, "pA%d" % i, [128, 1024], BF16) for i in range(2)]
                pO = [ps(st, "pO%d" % i, [128, 2, 512]) for i in range(2)]
                pR = [ps(st, "pR%d" % i, [128, 512]) for i in range(2)]
                b_w = P.buf("w")
                b_yt, b_ynb, b_ynT, b_xt = P.bufs("yt", 2), P.bufs("ynb", 2), P.bufs("ynT", 2), P.bufs("xt", 2)
                b_xnew, b_h2b, b_h2T = P.bufs("xnew", 2), P.bufs("h2b", 2), P.bufs("h2T", 2)
                b_stat, b_lg, b_rtmp, b_am = P.bufs("stat", 2), P.bufs("lg", 2), P.bufs("rtmp", 2), P.bufs("am", 2)
                b_junk, b_tmp = P.buf("junk"), P.buf("tmp")
                b_pA, b_pO, b_pR = P.bufs("pA", 2), P.bufs("pO", 2), P.bufs("pR", 2)
                b_rout, b_carry = P.buf("rout"), P.buf("carry")
                for c in range(KC):
                    P.dma("pool", wout[:, c, :], w_out[l][c * 128:(c + 1) * 128, :], b_w, (), (b_w,), nowaw=True)
                P.dma("pool", wr[:, :, 0:4], w_grp[l].rearrange("(c p) n -> p c n", p=128), b_w, (), (b_w,), nowaw=True)
                P.dma("pool", wr[:, :, 4:36], w_exp[l].rearrange("(c p) n -> p c n", p=128), b_w, (), (b_w,), nowaw=True)
                P.dma("act", brt[:, 0:4], b_grp[l:l + 1, :].partition_broadcast(128), b_w, (), (b_w,), nowaw=True)
                P.dma("act", brt[:, 4:36], b_exp[l:l + 1, :].partition_broadcast(128), b_w, (), (b_w,), nowaw=True)
                P.dma("act", outg[:], out_norm_g[l].rearrange("(c p) -> p c", p=128), b_w, (), (b_w,), nowaw=True,
                      slow=True)
                load_bc("act", g1_bc, 2, b_w)
                load_bc("act", gm2_bc, 4, b_w)
                load_bc("act", sh2_bc, 3, b_w)
                P.memset("dve", carry[:], 0.0, (b_carry,))
                tiles = [(t, 0) for t in range(NTL)] + ([] if last else [(t, 1) for t in range(NTL, NTT)])
                def phaseA(ti, t, v):
                    s = ti % 2
                    rows = slice(t * 128, (t + 1) * 128)
                    P.dma("sp", yt[:, s, :], Y[rows, :], b_yt[s], (), (b_yt[s],))
                    P.dma("sp", xt[:, s, :], x_src(l, t), b_xt[s], (), (b_xt[s],))
                    for g in range(4):
                        P.act(junk[:, g * 256:(g + 1) * 256], yt[:, s, g * 256:(g + 1) * 256], AF.Square,
                              (b_yt[s],), (b_junk, b_stat[s]), accum=stat[:, s, g:g + 1])
                    rms_rstd(stat[:, s, 0:4], stat[:, s, 4:8], 256, (b_stat[s],), (b_stat[s],))
                    for g in range(4):
                        P.act(ynb[:, s, g * 256:(g + 1) * 256], yt[:, s, g * 256:(g + 1) * 256], AF.Copy,
                              (b_yt[s], b_stat[s]), (b_ynb[s],), scale=stat[:, s, 4 + g:5 + g], nowaw=(g > 0))
                    for c in range(KC):
                        P.tr(pA[s][:, c * 128:(c + 1) * 128], ynb[:, s, c * 128:(c + 1) * 128], ident_b,
                             (b_ynb[s],), (b_pA[s],), nowaw=(c > 0))
                    for c in range(KC):
                        P.ts("dve", ynT[:, s, c, :], pA[s][:, c * 128:(c + 1) * 128],
                             outg[:, c:c + 1], None, ALU.mult, None, (b_pA[s], b_w), (b_ynT[s],), nowaw=(c > 0))
                    for half in range(2):
                        for c in range(KC):
                            P.mm(pO[s][:, half, :], ynT[:, s, c, :], wout[:, c, half * 512:(half + 1) * 512],
                                 c == 0, c == KC - 1, (b_ynT[s], b_w), (b_pO[s],))
                    P.tt("dve", tmp[:], pO[s][:].rearrange("p a b -> p (a b)"), g1_bc[:, v, :], ALU.mult,
                         (b_pO[s], b_w), (b_tmp,))
                    P.tt("pool", xnew[:, s, :], tmp[:], xt[:, s, :], ALU.add, (b_tmp, b_xt[s]), (b_xnew[s],))
                    P.dma("sp", X[rows, :], xnew[:, s, :], b_xnew[s], (b_xnew[s],), ())
                    P.act(junk[:], xnew[:, s, :], AF.Square, (b_xnew[s],), (b_junk, b_stat[s]), accum=stat[:, s, 8:9])
                    rms_rstd(stat[:, s, 8:9], stat[:, s, 9:10], D, (b_stat[s],), (b_stat[s],))
                    P.stt("dve", tmp[:], xnew[:, s, :], stat[:, s, 9:10], gm2_bc[:, v, :], ALU.mult, ALU.mult,
                          (b_xnew[s], b_stat[s], b_w), (b_tmp,))
                    P.tt("pool", h2b[:, s, :], tmp[:], sh2_bc[:, v, :], ALU.add, (b_tmp, b_w), (b_h2b[s],))
                    P.dma("sp", H2[rows, :], h2b[:, s, :], b_h2b[s], (b_h2b[s],), ())
                    for c in range(KC):
                        P.tr(pA[s][:, c * 128:(c + 1) * 128], h2b[:, s, c * 128:(c + 1) * 128], ident_b,
                             (b_h2b[s],), (b_pA[s],), nowaw=(c > 0))
                    P.cp("act", h2T[:, s].rearrange("p c q -> p (c q)"), pA[s][:, :], (b_pA[s],), (b_h2T[s],))
                    for c in range(KC):
                        P.mm(pR[s][:, 0:36], h2T[:, s, c, :], wr[:, c, :], c == 0, c == KC - 1,
                             (b_h2T[s], b_w), (b_pR[s],))
                    LG = lg[:, s, :]
                    P.tt("dve", LG, pR[s][:, 0:36], brt[:], ALU.add, (b_pR[s], b_w), (b_lg[s],))

                def phaseB(ti, t, v):
                    s = ti % 2
                    R_, W_ = (b_lg[s], b_stat[s], b_rtmp[s]), (b_stat[s], b_rtmp[s])
                    sm = stat[:, s, 10:11]
                    P.red("dve", sm, lg[:, s, 0:4], ALU.max, R_, W_)
                    goh = rtmp[:, s, 0, 0:4]
                    P.ts("dve", goh, lg[:, s, 0:4], sm, None, ALU.is_ge, None, R_, W_)
                    P.ts("dve", stat[:, s, 11:12], sm, -1.0, None, ALU.mult, None, R_, W_)
                    P.act(rtmp[:, s, 0, 4:8], lg[:, s, 0:4], AF.Exp, R_, W_, bias=stat[:, s, 11:12],
                          accum=stat[:, s, 12:13])
                    pg = stat[:, s, 13:14]
                    P.add("dve", lambda e, pg=pg, s=s: e.reciprocal(pg, stat[:, s, 12:13]), R_, W_)
                    ml = rtmp[:, s, 1, :]
                    pen = rtmp[:, s, 2, :]
                    P.ts("dve", pen.rearrange("p (g e) -> p g e", g=4),
                         goh.unsqueeze(2).to_broadcast([128, 4, 8]), -1.0, 1.0e4, ALU.add, ALU.mult, R_, W_)
                    P.tt("dve", ml, lg[:, s, 4:36], pen, ALU.add, R_, W_)
                    v1 = stat[:, s, 14:15]
                    v2 = stat[:, s, 15:16]
                    P.red("dve", v1, ml, ALU.max, R_, W_)
                    m1 = mask1[:, t, :]
                    m2 = mask2[:, t, :]
                    RW = W_ + (b_rout,)
                    P.ts("dve", m1, ml, v1, None, ALU.is_ge, None, R_, RW)
                    P.stt("dve", ml, m1, -1.0e4, ml, ALU.mult, ALU.add, R_ + (b_rout,), W_)
                    P.red("dve", v2, ml, ALU.max, R_, W_)
                    P.ts("dve", m2, ml, v2, None, ALU.is_ge, None, R_, RW)
                    dv = stat[:, s, 11:12]
                    P.tt("dve", dv, v2, v1, ALU.subtract, R_, W_)
                    P.act(dv, dv, AF.Exp, R_, W_)
                    P.ts("dve", dv, dv, 1.0, None, ALU.add, None, R_, W_)
                    P.add("dve", lambda e, dv=dv: e.reciprocal(dv, dv), R_, W_)
                    P.tt("dve", gw12[:, 0, t:t + 1], dv, pg, ALU.mult, R_, RW)
                    P.tt("dve", gw12[:, 1, t:t + 1], pg, gw12[:, 0, t:t + 1], ALU.subtract, R_ + (b_rout,), RW)
                    P.tt("dve", amask[:, s, :], m1, m2, ALU.add, (b_rout,), (b_am[s],))
                    P.mm(pR[s][:, 64:96], triu_b, amask[:, s, :], True, True, (b_am[s],), (b_pR[s],))
                    P.mm(pR[s][:, 128:160], ones_b, amask[:, s, :], True, True, (b_am[s],), (b_pR[s],))
                    rk = rtmp[:, s, 3, :]
                    P.tt("dve", rk, pR[s][:, 64:96], carry[:], ALU.add, (b_pR[s], b_carry) + R_, W_)
                    P.tt("dve", carry[:], carry[:], pR[s][:, 128:160], ALU.add, (b_pR[s], b_carry), (b_carry,))
                    for k, mk in ((0, m1), (1, m2)):
                        P.tt("dve", pen, rk, mk, ALU.mult, R_ + (b_rout,), W_)
                        P.red("dve", rank12[:, k, t:t + 1], pen, ALU.add, R_, RW)
                for ti, (t, v) in enumerate(tiles):
                    phaseA(ti, t, v)
                    phaseB(ti, t, v)
                P.end()

        def s7(l, last):
            with ExitStack() as st:
                pad = sb(st, "pad", [128, NE])
                pend = sb(st, "pend", [128, NE])
                pstart = sb(st, "pstart", [128, NE])
                big = sb(st, "big", [128, NTT, NE])
                destf = sb(st, "destf", [128, 2, NTT])
                cmp_ = sb(st, "cmp", [128, NBLK, NE])
                ebf = sb(st, "ebf", [128, NBLK])
                wif = sb(st, "wif", [128, 2, NBLK])
                h2r = sb(st, "h2r", [128, 3, D], BF16)
                b_p, b_big, b_dest, b_cmp, b_eb = P.buf("p"), P.buf("big"), P.buf("dest"), P.buf("cmp"), P.buf("eb")
                b_h2r = P.bufs("h2r", 3)
                JJ = (2 * NT) // BLK + 1
                cmpp = sb(st, "cmpp", [128, NE, JJ])
                P.tt("dve", cmpp[:], carry[:].unsqueeze(2).to_broadcast([128, NE, JJ]),
                     blkstart[:, 0:JJ].unsqueeze(1).to_broadcast([128, NE, JJ]), ALU.is_gt, (), (b_p,))
                P.red("dve", pad[:], cmpp[:], ALU.add, (b_p,), (b_p,))
                P.ts("dve", pad[:], pad[:], float(BLK), None, ALU.mult, None, (b_p,), (b_p,))
                P.cp("dve", pend[:, 0:1], pad[:, 0:1], (b_p,), (b_p,))
                for e_ in range(1, NE):
                    P.tt("dve", pend[:, e_:e_ + 1], pend[:, e_ - 1:e_], pad[:, e_:e_ + 1], ALU.add, (b_p,), (b_p,))
                P.tt("dve", pstart[:], pend[:], pad[:], ALU.subtract, (b_p,), (b_p,))
                ntt_used = NTL if last else NTT
                for k, mk in ((0, mask1), (1, mask2)):
                    P.tt("dve", big[:, 0:ntt_used, :], mk[:, 0:ntt_used, :],
                         pstart[:].unsqueeze(1).to_broadcast([128, ntt_used, NE]), ALU.mult, (b_p,), (b_big,))
                    P.red("dve", destf[:, k, 0:ntt_used], big[:, 0:ntt_used, :], ALU.add, (b_big,), (b_dest,))
                    P.tt("dve", destf[:, k, 0:ntt_used], destf[:, k, 0:ntt_used], rank12[:, k, 0:ntt_used], ALU.add,
                         (b_dest,), (b_dest,))
                P.cp("dve", dest_i[:, :, 0:ntt_used], destf[:, :, 0:ntt_used], (b_dest,), (b_dest,))
                P.tt("dve", cmp_[:], pend[:].unsqueeze(1).to_broadcast([128, NBLK, NE]),
                     blkstart[:, 0:NBLK].unsqueeze(2).to_broadcast([128, NBLK, NE]), ALU.is_le, (b_p,), (b_cmp,))
                P.red("dve", ebf[:], cmp_[:], ALU.add, (b_cmp,), (b_eb,))
                P.ts("dve", ebf[:], ebf[:], float(NE - 1), None, ALU.min, None, (b_eb,), (b_eb,))
                P.ts("dve", wif[:, 0, :], ebf[:], float(D), iota_p, ALU.mult, ALU.add, (b_eb,), (b_eb,))
                P.ts("dve", wif[:, 1, :], ebf[:], float(DE), iota_p, ALU.mult, ALU.add, (b_eb,), (b_eb,))
                P.cp("dve", widx[:], wif[:], (b_eb,), (b_eb,))
                for t in range(ntt_used):
                    s = t % 3
                    P.dma("sp", h2r[:, s, :], H2[t * 128:(t + 1) * 128, :], b_h2r[s], (), (b_h2r[s],))
                    for k in range(2):
                        P.scatter(XBUF, dest_i[:, k, t:t + 1], h2r[:, s, :], b_h2r[s], (b_h2r[s], b_dest), ())
                P.end()

        def s8(l, last):
            with ExitStack() as st:
                wg = sb(st, "wg", [128, 3, KC, DE], BF16)
                wu = sb(st, "wu", [128, 3, KC, DE], BF16)
                wd = sb(st, "wd", [128, 3, 4, D], BF16)
                xr = sb(st, "xr", [128, 2, 4, D], BF16)
                xT = sb(st, "xT", [128, 2, KC, 512], BF16)
                sg_ = sb(st, "sg", [128, 2, 512])
                hid = sb(st, "hid", [128, 2, 4, 512], BF16)
                yo = sb(st, "yo", [128, 2, D])
                pA = [ps(st, "pA%d" % i, [128, 1024], BF16) for i in range(2)]
                pG = [ps(st, "pG%d" % i, [128, 512]) for i in range(4)]
                pY = [ps(st, "pY%d" % i, [128, 512]) for i in range(2)]
                b_wg, b_wu, b_wd = P.bufs("wg", 3), P.bufs("wu", 3), P.bufs("wd", 3)
                b_xr, b_xT, b_sg, b_hid, b_yo = P.bufs("xr", 2), P.bufs("xT", 2), P.bufs("sg", 2), P.bufs("hid", 2), P.bufs("yo", 2)
                b_pA, b_pG, b_pY = P.bufs("pA", 2), P.bufs("pG", 4), P.bufs("pY", 2)
                nblk = NBLK
                pai = 0
                pgi = 0
                yi = 0
                for b in range(nblk):
                    s = b % 2
                    ws = b % 3
                    for c in range(KC):
                        P.gather(wg[:, ws, c, :], w_gate, widx[:, 0, b:b + 1], (l * NE * D + c * 128) * DE,
                                 b_wg[ws], (), (b_wg[ws],), nowaw=True)
                    for c in range(KC):
                        P.gather(wu[:, ws, c, :], w_up, widx[:, 0, b:b + 1], (l * NE * D + c * 128) * DE,
                                 b_wu[ws], (), (b_wu[ws],), nowaw=True)
                    for f in range(4):
                        P.gather(wd[:, ws, f, :], w_down, widx[:, 1, b:b + 1], (l * NE * DE + f * 128) * D,
                                 b_wd[ws], (), (b_wd[ws],), nowaw=True)
                    P.dma("sp", xr[:, s], XBUF[b * BLK:(b + 1) * BLK, :].rearrange("(i p) d -> p i d", p=128),
                          b_xr[s], (), (b_xr[s],))
                    for c in range(KC):
                        a = pai % 2
                        pai += 1
                        for i in range(4):
                            P.tr(pA[a][:, i * 128:(i + 1) * 128], xr[:, s, i, c * 128:(c + 1) * 128], ident_b,
                                 (b_xr[s],), (b_pA[a],), nowaw=(i > 0))
                        P.cp("act" if c % 2 else "dve", xT[:, s, c, :], pA[a][:, 0:512], (b_pA[a],), (b_xT[s],),
                             nowaw=(c > 0))
                    for f in range(4):
                        g0 = pgi % 4
                        g1 = (pgi + 1) % 4
                        pgi += 2
                        for c in range(KC):
                            P.mm(pG[g0][:, :], wg[:, ws, c, f * 128:(f + 1) * 128], xT[:, s, c, :], c == 0, c == KC - 1,
                                 (b_wg[ws], b_xT[s]), (b_pG[g0],))
                        for c in range(KC):
                            P.mm(pG[g1][:, :], wu[:, ws, c, f * 128:(f + 1) * 128], xT[:, s, c, :], c == 0, c == KC - 1,
                                 (b_wu[ws], b_xT[s]), (b_pG[g1],))
                        fs = f % 2
                        P.act(sg_[:, fs, :], pG[g0][:, :], AF.Silu, (b_pG[g0],), (b_sg[fs],))
                        P.tt("dve", hid[:, s, f, :], sg_[:, fs, :], pG[g1][:, :], ALU.mult, (b_sg[fs], b_pG[g1]),
                             (b_hid[s],), nowaw=(f > 0))
                    for i in range(4):
                        ys = yi % 2
                        yi += 1
                        for half in range(2):
                            for f in range(4):
                                P.mm(pY[half][:, :], hid[:, s, f, i * 128:(i + 1) * 128],
                                     wd[:, ws, f, half * 512:(half + 1) * 512], f == 0, f == 3,
                                     (b_hid[s], b_wd[ws]), (b_pY[half],))
                            P.cp("act" if half else "dve", yo[:, ys, half * 512:(half + 1) * 512], pY[half][:, :],
                                 (b_pY[half],), (b_yo[ys],), nowaw=(half > 0))
                        P.dma("sp", YBUF[b * BLK + i * 128:b * BLK + (i + 1) * 128, :], yo[:, ys, :], b_yo[ys],
                              (b_yo[ys],), ())
                P.end()

        def s9(l, last):
            with ExitStack() as st:
                g2_bc = sb(st, "g2_bc", [128, 2, D])
                fg_bc = sb(st, "fg_bc", [128, D])
                y1 = sb(st, "y1", [128, 4, D])
                y2 = sb(st, "y2", [128, 4, D])
                xt = sb(st, "xt", [128, 4, D])
                f1 = sb(st, "f1", [128, D])
                xo = sb(st, "xo", [128, 4, D])
                junk = sb(st, "junk", [128, D], BF16)
                stat = sb(st, "stat", [128, 4, 2])
                b_w = P.buf("w")
                b_y1, b_y2, b_xt, b_xo, b_stat = P.bufs("y1", 4), P.bufs("y2", 4), P.bufs("xt", 4), P.bufs("xo", 4), P.bufs("stat", 4)
                b_f1, b_junk = P.buf("f1"), P.buf("junk")
                load_bc("act", g2_bc, 5, b_w)
                if last:
                    P.dma("act", fg_bc[:], final_g.partition_broadcast(128), b_w, (), (b_w,), nowaw=True)
                tiles = [(t, 0) for t in range(NTL)] + ([] if last else [(t, 1) for t in range(NTL, NTT)])
                for ti, (t, v) in enumerate(tiles):
                    s = ti % 4
                    rows = slice(t * 128, (t + 1) * 128)
                    P.gather(y1[:, s, :], YBUF, dest_i[:, 0, t:t + 1], 0, b_y1[s], (), (b_y1[s],))
                    P.gather(y2[:, s, :], YBUF, dest_i[:, 1, t:t + 1], 0, b_y2[s], (), (b_y2[s],))
                    P.dma("sp", xt[:, s, :], X[rows, :], b_xt[s], (), (b_xt[s],))
                    P.ts("dve", f1[:], y1[:, s, :], gw12[:, 0, t:t + 1], None, ALU.mult, None, (b_y1[s],), (b_f1,))
                    P.stt("dve", f1[:], y2[:, s, :], gw12[:, 1, t:t + 1], f1[:], ALU.mult, ALU.add,
                          (b_y2[s], b_f1), (b_f1,))
                    P.tt("pool", f1[:], f1[:], g2_bc[:, v, :], ALU.mult, (b_f1, b_w), (b_f1,))
                    P.tt("dve", xo[:, s, :], f1[:], xt[:, s, :], ALU.add, (b_f1, b_xt[s]), (b_xo[s],))
                    if not last:
                        P.dma("sp", X[rows, :], xo[:, s, :], b_xo[s], (b_xo[s],), ())
                    else:
                        P.act(junk[:], xo[:, s, :], AF.Square, (b_xo[s],), (b_junk, b_stat[s]), accum=stat[:, s, 0:1])
                        rms_rstd(stat[:, s, 0:1], stat[:, s, 1:2], D, (b_stat[s],), (b_stat[s],))
                        P.stt("dve", xo[:, s, :], xo[:, s, :], stat[:, s, 1:2], fg_bc[:], ALU.mult, ALU.mult,
                              (b_xo[s], b_stat[s], b_w), (b_xo[s],))
                        P.dma("sp", out[rows, :], xo[:, s, :], b_xo[s], (b_xo[s],), ())
                P.end()

        stages = [s1, s2, s3, s4, s5, s6, s7, s8, s9]
        s0()
        done = cfg.stop is not None and cfg.stop[1] == 0
        for l in range(L):
            if done:
                break
            last = (l == L - 1)
            for si_, fn in enumerate(stages):
                if si_ == 0:
                    fn(l)
                else:
                    fn(l, last)
                if cfg.stop is not None and cfg.stop == (l, si_ + 1):
                    done = True
                    break
            if done:
                break
    return nc


def make_consts(cfg):
    cf = np.zeros((128, 353), np.float32)
    cf[:, 0:128] = np.eye(128, dtype=np.float32)
    cf[:, 128:256] = 1.0
    cf[:, 256:288] = np.arange(32, dtype=np.float32)[None, :]
    cf[:, 288] = np.arange(128, dtype=np.float32)
    cf[:, 289:353] = (np.arange(64, dtype=np.float32) * BLK)[None, :]
    cbm = np.zeros((128, 384), np.float32)
    cbm[:, 0:128] = np.eye(128)
    cbm[:, 128:256] = 1.0
    cbm[:, 256:384] = np.triu(np.ones((128, 128), np.float32), k=1)
    return cf, cbm.astype(ml_dtypes.bfloat16)


def core_inputs(cfg, inputs, b):
    L = cfg.L
    f = lambda a: np.ascontiguousarray(np.asarray(a, dtype=np.float32))
    cosT, sinT = rope_tables(cfg.NL, cfg.NCX)
    cf, cbm = make_consts(cfg)
    m = {
        "x": f(inputs["x"][b]),
        "ctx": f(inputs["ctx"][b]),
        "cc": f(np.stack([np.asarray(inputs["c"][b]), np.asarray(inputs["c_ctx"])], axis=0)),
        "na_bias": na_bias_tables(np.asarray(inputs["na_rpb"], dtype=np.float32)[:L], cfg.variants),
        "w_gate": f(inputs["w_gate"][:L]).reshape(L * NE * D, DE),
        "w_up": f(inputs["w_up"][:L]).reshape(L * NE * D, DE),
        "w_down": f(inputs["w_down"][:L]).reshape(L * NE * DE, D),
        "final_norm_g": f(inputs["final_norm_g"]).reshape(1, D),
        "cosT": cosT, "sinT": sinT, "consts_f": cf, "consts_b": cbm,
    }
    for k in ("w_ada", "b_ada", "norm1_g", "norm2_g", "w_in", "conv_w", "sg_w", "sg_b", "mla_q_norm_g",
              "mla_w_uq", "mla_kv_norm_g", "mla_w_ukv", "out_norm_g", "w_out", "w_grp", "b_grp", "w_exp", "b_exp"):
        m[k] = f(inputs[k][:L])
    return m


_NC_CACHE = {}


def kernel(**inputs):
    cfg = Cfg()
    if "nc" not in _NC_CACHE:
        _NC_CACHE["nc"] = build(cfg)
    nc = _NC_CACHE["nc"]
    per_b = [core_inputs(cfg, inputs, b) for b in range(4)]
    in_maps = [per_b[i % 4] for i in range(8)]
    res = run_bass_kernel_spmd(nc, in_maps, core_ids=list(range(8)))
    return np.stack([np.asarray(res.results[b]["out"], dtype=np.float32) for b in range(4)], axis=0)
```

```python
import numpy as np
import ml_dtypes
from contextlib import ExitStack
import concourse.bass as bass
import concourse.mybir as mybir
from concourse.bass_utils import run_bass_kernel_spmd

F32 = mybir.dt.float32
BF16 = mybir.dt.bfloat16
I32 = mybir.dt.int32
AF = mybir.ActivationFunctionType
ALU = mybir.AluOpType
AX = mybir.AxisListType

D = 1024
KC = 8
IN_COLS = 2464
SG0, NA0, MLA0 = 768, 1280, 2048
NE = 32
DE = 512
BLK = 512
EPS = 1e-6
MLA_SCALE = 96.0 ** -0.5
NEG = -30000.0
SAME_ENG_SYNC = True


class Buf:
    __slots__ = ("name", "writers", "readers", "gen", "excl")

    def __init__(self, name):
        self.name = name
        self.excl = (len(name) > 1 and name[0] == "p" and (name[1].isupper() or name[1] == "m"))
        self.writers = []
        self.readers = []
        self.gen = []


class Op:
    __slots__ = ("eng", "fn", "deps", "is_dma", "dbuf", "signal", "sem", "count")

    def __init__(self, eng, fn, is_dma, dbuf):
        self.eng = eng
        self.fn = fn
        self.deps = []
        self.is_dma = is_dma
        self.dbuf = dbuf
        self.signal = is_dma
        self.sem = None
        self.count = 0


ENGS = ("sp", "act", "dve", "pool", "pe")


class Prog:
    def __init__(self, nc, es, n_dsem=84):
        self.nc = nc
        self.csem = {e: es.enter_context(nc.semaphore("c_" + e)) for e in ("act", "dve", "pool", "pe")}
        self.ccount = {e: 0 for e in self.csem}
        n_sw = 24
        self.dsem = {False: [es.enter_context(nc.semaphore("d%d" % i)) for i in range(n_dsem - n_sw)],
                     True: [es.enter_context(nc.semaphore("w%d" % i)) for i in range(n_sw)]}
        self.dcount = {False: [0] * (n_dsem - n_sw), True: [0] * n_sw}
        self.waited = {e: {} for e in ENGS}
        self.nstage = 0
        self.begin()

    def begin(self):
        self.ops = {e: [] for e in ENGS}
        self.dmap = {}

    def buf(self, name):
        return Buf(name)

    def bufs(self, name, n):
        return [Buf("%s%d" % (name, i)) for i in range(n)]

    def add(self, eng, fn, reads=(), writes=(), dbuf=None, nowaw=False):
        op = Op(eng, fn, dbuf is not None, dbuf)
        deps = []
        xr = [b for b in reads if b.excl]
        reads = [b for b in reads if not b.excl]
        for b in reads:
            deps.extend(b.writers)
        newgen = []
        for b in xr:
            g = list(b.writers) + list(b.readers)
            deps.extend(g)
            newgen.append((b, g))
        writes = tuple(writes) + tuple(xr)
        for b in writes:
            if b in xr:
                continue
            if nowaw and not b.readers and b.writers:
                deps.extend(b.gen)
            else:
                g = list(b.writers) + list(b.readers)
                deps.extend(g)
                newgen.append((b, g))
        seen = set()
        for d in deps:
            if id(d) in seen:
                continue
            seen.add(id(d))
            if d.eng == "pe" and eng == "pe" and not d.is_dma and dbuf is None:
                continue
            if (not SAME_ENG_SYNC) and d.eng == eng and not d.is_dma and dbuf is None:
                continue
            d.signal = True
            op.deps.append(d)
        for b in reads:
            b.readers.append(op)
        ng = {id(b): g for b, g in newgen}
        for b in writes:
            if id(b) in ng:
                b.gen = ng[id(b)]
                b.writers = [op]
                b.readers = []
            else:
                b.writers.append(op)
        self.ops[eng].append(op)
        return op

    def mm(self, out, lhsT, rhs, start, stop, r, w):
        return self.add("pe", lambda e: e.matmul(out, lhsT, rhs, start=start, stop=stop), r, w, nowaw=not start)

    def tr(self, out, in_, ident, r, w, nowaw=True):
        return self.add("pe", lambda e: e.transpose(out, in_, ident), r, w, nowaw=nowaw)

    def act(self, out, in_, func, r, w, bias=None, scale=None, accum=None, nowaw=False):
        kw = {}
        if bias is not None:
            kw["bias"] = bias
        if scale is not None:
            kw["scale"] = scale
        if accum is not None:
            kw["accum_out"] = accum
        return self.add("act", lambda e: e.activation(out, in_, func, **kw), r, w, nowaw=nowaw)

    def tt(self, eng, out, in0, in1, op, r, w, nowaw=False):
        return self.add(eng, lambda e: e.tensor_tensor(out, in0, in1, op), r, w, nowaw=nowaw)

    def ts(self, eng, out, in0, s1, s2, op0, op1, r, w, nowaw=False):
        if s2 is None:
            return self.add(eng, lambda e: e.tensor_scalar(out, in0, s1, None, op0), r, w, nowaw=nowaw)
        return self.add(eng, lambda e: e.tensor_scalar(out, in0, s1, s2, op0, op1), r, w, nowaw=nowaw)

    def stt(self, eng, out, in0, scalar, in1, op0, op1, r, w, nowaw=False):
        return self.add(eng, lambda e: e.scalar_tensor_tensor(out, in0, scalar, in1, op0, op1), r, w, nowaw=nowaw)

    def cp(self, eng, out, in_, r, w, nowaw=False):
        if eng == "act":
            return self.add(eng, lambda e: e.copy(out, in_), r, w, nowaw=nowaw)
        return self.add(eng, lambda e: e.tensor_copy(out, in_), r, w, nowaw=nowaw)

    def memset(self, eng, ap, val, w, nowaw=False):
        return self.add(eng, lambda e: e.memset(ap, val), (), w, nowaw=nowaw)

    def red(self, eng, out, in_, op, r, w, nowaw=False):
        return self.add(eng, lambda e: e.tensor_reduce(out, in_, AX.X, op), r, w, nowaw=nowaw)

    def dma(self, q, out, in_, dbuf, r, w, nowaw=False, slow=False):
        if slow:
            return self.add(q, lambda e: e.dma_start(out=out, in_=in_, allow_slow_non_contiguous=True),
                            r, w, dbuf=dbuf, nowaw=nowaw)
        return self.add(q, lambda e: e.dma_start(out=out, in_=in_), r, w, dbuf=dbuf, nowaw=nowaw)

    def gather(self, out, in_, idx, elem_off, dbuf, r, w, nowaw=False, bound=None):
        if bound is not None:
            return self.add("pool", lambda e: e.indirect_dma_start(
                out=out, out_offset=None, in_=in_,
                in_offset=bass.IndirectOffsetOnAxis(ap=idx, axis=0), element_offset=elem_off,
                bounds_check=bound, oob_is_err=False),
                r, w, dbuf=dbuf, nowaw=nowaw)
        return self.add("pool", lambda e: e.indirect_dma_start(
            out=out, out_offset=None, in_=in_,
            in_offset=bass.IndirectOffsetOnAxis(ap=idx, axis=0), element_offset=elem_off),
            r, w, dbuf=dbuf, nowaw=nowaw)

    def scatter(self, out, idx, in_, dbuf, r, w):
        return self.add("pool", lambda e: e.indirect_dma_start(
            out=out, out_offset=bass.IndirectOffsetOnAxis(ap=idx, axis=0), in_=in_, in_offset=None),
            r, w, dbuf=dbuf)

    def end(self):
        nc = self.nc
        for e in ENGS:
            for op in self.ops[e]:
                if op.is_dma:
                    sw = (e == "pool")
                    k = (id(op.dbuf), sw)
                    if k not in self.dmap:
                        n = sum(1 for kk in self.dmap if kk[1] == sw)
                        assert n < len(self.dsem[sw]), "out of DMA semaphores"
                        self.dmap[k] = n
                    si = self.dmap[k]
                    self.dcount[sw][si] += 16
                    op.sem = self.dsem[sw][si]
                    op.count = self.dcount[sw][si]
                elif op.signal:
                    self.ccount[e] += 1
                    op.sem = self.csem[e]
                    op.count = self.ccount[e]
        final = [(self.dsem[sw][si], self.dcount[sw][si]) for (_, sw), si in self.dmap.items()]
        ops = self.ops
        waited = self.waited
        engmap = {"sp": "sync", "act": "scalar", "dve": "vector", "pool": "gpsimd", "pe": "tensor"}

        def run(ename, eng):
            wd = waited[ename]
            for op in ops[ename]:
                need = {}
                for d in op.deps:
                    k = id(d.sem)
                    if k not in need or need[k][1] < d.count:
                        need[k] = (d.sem, d.count)
                todo = []
                for k, (s, c) in need.items():
                    if wd.get(k, 0) < c:
                        todo.append((s, c))
                        wd[k] = c
                for s, c in todo[:-1]:
                    eng.wait_ge(s, c)
                ins = op.fn(eng)
                if todo:
                    ins._wait_ge(todo[-1][0], todo[-1][1])
                if op.signal:
                    ins.then_inc(op.sem, 16 if op.is_dma else 1)
            if ename == "sp":
                for s, c in final:
                    if wd.get(id(s), 0) < c:
                        eng.wait_ge(s, c)
                        wd[id(s)] = c

        with nc.Block() as block:
            for ename in ENGS:
                getattr(block, engmap[ename])(lambda eng, ename=ename: run(ename, eng))
        self.nstage += 1
        self.begin()


def rope_tables(NL, NCX):
    t = np.arange(NL)
    row = (t // 64).astype(np.float32)
    col = (t % 64).astype(np.float32)
    inv_freq = (np.float32(10000.0) ** (-np.arange(8, dtype=np.float32) / np.float32(8))).astype(np.float32)
    ang_r = row[:, None] * inv_freq
    ang_c = col[:, None] * inv_freq
    ang = np.concatenate([ang_r, ang_r, ang_c, ang_c], axis=-1).astype(np.float32)
    cos = np.cos(ang).astype(np.float32)
    sin = np.sin(ang).astype(np.float32)
    NT = NL + NCX
    cosT = np.zeros((128, NT), np.float32)
    sinT = np.zeros((128, NT), np.float32)
    cosT[64:96, :NL] = cos.T
    sinT[64:96, :NL] = sin.T
    cosT[64:96, NL:] = 1.0
    return cosT, sinT


def na_plan(R):
    def band(r):
        s = min(max(r - 4, 0), R - 8)
        return s
    variants = {}
    plan = []
    for j in range(R // 2):
        rows = (2 * j, 2 * j + 1)
        ms = set()
        for r in rows:
            s = band(r)
            for kr in range(s, s + 8):
                ms.add(kr // 2)
        lst = []
        for m in sorted(ms):
            key = []
            for qp in range(2):
                s = band(rows[qp])
                for kp in range(2):
                    kr = 2 * m + kp
                    key.append((s <= kr < s + 8, kr - rows[qp] + 7))
            key = tuple(key)
            if key not in variants:
                variants[key] = len(variants)
            lst.append((m, variants[key]))
        plan.append(lst)
    return plan, variants


def na_bias_tables(rpb, variants):
    L = rpb.shape[0]
    NV = len(variants)
    qc = np.arange(64)
    kc = np.arange(64)
    c0 = np.clip(qc - 8, 0, 48)
    in_win = (kc[:, None] >= c0[None, :]) & (kc[:, None] < c0[None, :] + 16)
    dc = np.clip(kc[:, None] - qc[None, :] + 15, 0, 30)
    out = np.full((L, 128, 4, NV, 128), NEG, np.float32)
    for key, v in variants.items():
        i = 0
        for qp in range(2):
            for kp in range(2):
                ok, dr = key[i]
                i += 1
                if not ok:
                    continue
                g = rpb[:, :, dr, :][:, :, dc]
                g = np.where(in_win[None, None], g, np.float32(NEG))
                out[:, kp * 64:(kp + 1) * 64, :, v, qp * 64:(qp + 1) * 64] = g.transpose(0, 2, 1, 3)
    return out.astype(ml_dtypes.bfloat16)


class Cfg:
    def __init__(self, NL=8192, NCX=256, L=4, debug=False, stop=None, part=None):
        self.part = part
        self.NL, self.NCX, self.L = NL, NCX, L
        self.NT = NL + NCX
        self.NTL = NL // 128
        self.NTC = NCX // 128
        self.NTT = self.NT // 128
        self.R = NL // 64
        self.debug = debug
        self.stop = stop
        nasg = 2 * self.NT
        self.NBLK = (nasg + NE * (BLK - 1)) // BLK
        self.plan, self.variants = na_plan(self.R)
        self.NV = len(self.variants)

    def groups(self, with_ctx=True):
        gs = []
        for g in range(self.NTL // 4):
            gs.append((list(range(4 * g, 4 * g + 4)), 0))
        if with_ctx:
            gs.append((list(range(self.NTL, self.NTL + self.NTC)), 1))
        return gs


def build(cfg):
    nc = bass.Bass("TRN2", target_bir_lowering=False)
    NL, NCX, NT, L = cfg.NL, cfg.NCX, cfg.NT, cfg.L
    NTT, NTL = cfg.NTT, cfg.NTL
    NV, NBLK = cfg.NV, cfg.NBLK

    def din(name, shape, dt=F32):
        return nc.dram_tensor(name, list(shape), dt, kind="ExternalInput").ap()

    skind = "ExternalOutput" if cfg.debug else "Internal"

    def dscr(name, shape, dt=F32):
        return nc.dram_tensor(name, list(shape), dt, kind=skind).ap()

    x_in = din("x", [NL, D])
    ctx_in = din("ctx", [NCX, D])
    cc_in = din("cc", [2, D])
    w_ada = din("w_ada", [L, D, 6 * D])
    b_ada = din("b_ada", [L, 6 * D])
    norm1_g = din("norm1_g", [L, D])
    norm2_g = din("norm2_g", [L, D])
    w_in = din("w_in", [L, D, IN_COLS])
    conv_w = din("conv_w", [L, 3, 256])
    sg_w = din("sg_w", [L, 4, 128, 128])
    sg_b = din("sg_b", [L, 4, 128])
    na_bias = din("na_bias", [L, 128, 4, NV, 128], BF16)
    q_norm_g = din("mla_q_norm_g", [L, 256])
    w_uq = din("mla_w_uq", [L, 256, 384])
    kv_norm_g = din("mla_kv_norm_g", [L, 128])
    w_ukv = din("mla_w_ukv", [L, 128, 512])
    out_norm_g = din("out_norm_g", [L, D])
    w_out = din("w_out", [L, D, D])
    w_grp = din("w_grp", [L, D, 4])
    b_grp = din("b_grp", [L, 4])
    w_exp = din("w_exp", [L, D, NE])
    b_exp = din("b_exp", [L, NE])
    tiny = cfg.stop is not None and cfg.stop[1] < 8 and cfg.stop[0] == 0
    w_gate = din("w_gate", [L * NE * D, DE] if not tiny else [128, DE])
    w_up = din("w_up", [L * NE * D, DE] if not tiny else [128, DE])
    w_down = din("w_down", [L * NE * DE, D] if not tiny else [128, D])
    final_g = din("final_norm_g", [1, D])
    cosT_in = din("cosT", [128, NT])
    sinT_in = din("sinT", [128, NT])
    consts_f = din("consts_f", [128, 128 + 128 + 32 + 1 + 64])
    consts_b = din("consts_b", [128, 128 + 128 + 128], BF16)

    out = nc.dram_tensor("out", [NL, D], F32, kind="ExternalOutput").ap()

    X = dscr("X", [NT, D])
    MOD = dscr("MOD", [2, 6 * D])
    UT = dscr("UT", [256, NT])
    BGT = dscr("BGT", [256, NT])
    Y = dscr("Y", [NT, D])
    QN_T = dscr("QN_T", [256, NT], BF16)
    KN_T = dscr("KN_T", [256, NT], BF16)
    VN = dscr("VN", [NT, 260], BF16)
    QM_T = dscr("QM_T", [4, 96, NT], BF16)
    KM_T = dscr("KM_T", [4, 96, NT], BF16)
    VM = dscr("VM", [NT, 260], BF16)
    H2 = dscr("H2", [NT, D], BF16)
    XBUF = dscr("XBUF", [NBLK * BLK, D], BF16)
    YBUF = dscr("YBUF", [NBLK * BLK, D])

    es = ExitStack()
    with es:
        P = Prog(nc, es)

        uid = [0]

        def sb(st, name, shape, dt=F32):
            uid[0] += 1
            return st.enter_context(nc.sbuf_tensor("%s_%d" % (name, uid[0]), list(shape), dt))

        def ps(st, name, shape, dt=F32):
            uid[0] += 1
            return st.enter_context(nc.psum_tensor("%s_%d" % (name, uid[0]), list(shape), dt))

        cf = sb(es, "cf", [128, 353])
        cb = sb(es, "cb", [128, 384], BF16)
        ident_f = cf[:, 0:128]
        ones_f = cf[:, 128:256]
        iota_e = cf[:, 256:288]
        iota_p = cf[:, 288:289]
        blkstart = cf[:, 289:353]
        ident_b = cb[:, 0:128]
        ones_b = cb[:, 128:256]
        triu_b = cb[:, 256:384]
        cboth = sb(es, "cboth", [128, KC, 2])
        mask1 = sb(es, "mask1", [128, NTT, NE])
        mask2 = sb(es, "mask2", [128, NTT, NE])
        rank12 = sb(es, "rank12", [128, 2, NTT])
        gw12 = sb(es, "gw12", [128, 2, NTT])
        dest_i = sb(es, "dest_i", [128, 2, NTT], I32)
        carry = sb(es, "carry", [128, NE])
        widx = sb(es, "widx", [128, 2, NBLK], I32)

        def s0():
            with ExitStack() as st:
                craw = sb(st, "craw", [128, KC, 2])
                b_cf, b_cb, b_craw, b_cboth = P.buf("cf"), P.buf("cb"), P.buf("craw"), P.buf("cboth")
                P.dma("sp", cf[:], consts_f, b_cf, (), (b_cf,))
                P.dma("sp", cb[:], consts_b, b_cb, (), (b_cb,))
                for v in range(2):
                    P.dma("sp", craw[:, :, v], cc_in[v].rearrange("(c p) -> p c", p=128), b_craw, (), (b_craw,),
                          nowaw=True, slow=True)
                P.act(cboth[:], craw[:], AF.Silu, (b_craw,), (b_cboth,))
                zt = sb(st, "zt", [128, 4 * D], BF16)
                b_zt = P.buf("zt")
                P.memset("dve", zt[:], 0.0, (b_zt,))
                for b in range(NBLK):
                    P.dma("sp" if b % 2 else "act",
                          XBUF[b * BLK:(b + 1) * BLK, :].rearrange("(p i) d -> p (i d)", p=128), zt[:], b_zt,
                          (b_zt,), ())
                P.end()

        def x_src(l, t):
            if l == 0:
                if t < NTL:
                    return x_in[t * 128:(t + 1) * 128, :]
                return ctx_in[(t - NTL) * 128:(t - NTL + 1) * 128, :]
            return X[t * 128:(t + 1) * 128, :]

        def s1(l):
            with ExitStack() as st:
                wa = sb(st, "wa", [128, 2, KC, 512])
                bada = sb(st, "bada", [2, 6 * D])
                g12 = sb(st, "g12", [2, 2 * D])
                modsb = sb(st, "modsb", [2, 6 * D])
                pm = [ps(st, "pm%d" % i, [128, 512]) for i in range(2)]
                b_wa = P.bufs("wa", 2)
                b_pm = P.bufs("pm", 2)
                b_bada, b_g12, b_mod = P.buf("bada"), P.buf("g12"), P.buf("mod")
                for v in range(2):
                    P.dma("act", bada[v:v + 1, :], b_ada[l:l + 1, :], b_bada, (), (b_bada,), nowaw=True)
                    P.dma("act", g12[v:v + 1, 0:D], norm1_g[l:l + 1, :], b_g12, (), (b_g12,), nowaw=True)
                    P.dma("act", g12[v:v + 1, D:2 * D], norm2_g[l:l + 1, :], b_g12, (), (b_g12,), nowaw=True)
                for j in range(12):
                    s = j % 2
                    P.dma("sp", wa[:, s], w_ada[l][:, j * 512:(j + 1) * 512].rearrange("(c p) n -> p c n", p=128),
                          b_wa[s], (), (b_wa[s],))
                    for c in range(KC):
                        P.mm(pm[s][0:2, :], cboth[:, c, :], wa[:, s, c, :], c == 0, c == KC - 1,
                             (b_wa[s],), (b_pm[s],))
                    P.tt("dve", modsb[:, j * 512:(j + 1) * 512], pm[s][0:2, :], bada[:, j * 512:(j + 1) * 512],
                         ALU.add, (b_pm[s], b_bada), (b_mod,), nowaw=True)
                for k, go in ((1, 0), (4, D)):
                    P.stt("dve", modsb[:, k * D:(k + 1) * D], modsb[:, k * D:(k + 1) * D], 1.0,
                          g12[:, go:go + D], ALU.add, ALU.mult, (b_mod, b_g12), (b_mod,))
                P.dma("sp", MOD, modsb[:], b_mod, (b_mod,), ())
                P.end()

        def load_bc(q, tile_v, k, b):
            for v in range(2):
                P.dma(q, tile_v[:, v, :], MOD[v:v + 1, k * D:(k + 1) * D].partition_broadcast(128), b, (), (b,),
                      nowaw=True)

        def rms_rstd(ssq, rstd, n, r, w):
            P.ts("dve", rstd, ssq, 1.0 / n, EPS, ALU.mult, ALU.add, r, w)
            P.act(rstd, rstd, AF.Sqrt, w, w)
            P.add("dve", lambda e: e.reciprocal(rstd, rstd), w, w)

        def s2(l, last):
            with ExitStack() as st:
                win = sb(st, "win", [128, KC, IN_COLS], BF16)
                wkr = sb(st, "wkr", [128, KC, 2, 96], BF16)
                wuq = sb(st, "wuq", [128, 2, 384], BF16)
                wuqrot = sb(st, "wuqrot", [128, 2, 4, 96], BF16)
                wukv = sb(st, "wukv", [128, 512], BF16)
                wukv_v = sb(st, "wukv_v", [128, 256], BF16)
                sgw32 = sb(st, "sgw32", [128, 4, 128])
                sgwb = sb(st, "sgwb", [128, 4, 128], BF16)
                sgwT = sb(st, "sgwT", [128, 4, 128], BF16)
                sgb = sb(st, "sgb", [128, 4])
                qkvg = sb(st, "qkvg", [128, 3])
                gm_bc = sb(st, "gm_bc", [128, 2, D])
                sh_bc = sb(st, "sh_bc", [128, 2, D])
                cos_t = sb(st, "cos_t", [128, 2, 512])
                sin_t = sb(st, "sin_t", [128, 2, 512])
                xt = sb(st, "xt", [128, 2, D])
                junk = sb(st, "junk", [128, D], BF16)
                xn = sb(st, "xn", [128, 2, D])
                hb = sb(st, "hb", [128, 2, D], BF16)
                hT = sb(st, "hT", [128, 2, KC, 512], BF16)
                stat = sb(st, "stat", [128, 2, 8])
                cg_sb = sb(st, "cg_sb", [128, 2, 512])
                u_sb = sb(st, "u_sb", [128, 2, 512])
                bg_sb = sb(st, "bg_sb", [128, 2, 512])
                qk_sb = sb(st, "qk_sb", [128, 4, 512], BF16)
                vaug = sb(st, "vaug", [128, 2, 4, 65], BF16)
                vaug2 = sb(st, "vaug2", [128, 2, 4, 65], BF16)
                zb = sb(st, "zb", [128, 512])
                gt1 = sb(st, "gt1", [128, 512])
                gt2 = sb(st, "gt2", [128, 512])
                gg = sb(st, "gg", [128, 512])
                vn = sb(st, "vn", [128, 256], BF16)
                yb = sb(st, "yb", [128, 2, 256])
                cq_b = sb(st, "cq_b", [128, 2, 384], BF16)
                cqnT = sb(st, "cqnT", [128, 3, 512], BF16)
                rt1 = sb(st, "rt1", [128, 512])
                rt2 = sb(st, "rt2", [128, 512])
                qT = sb(st, "qT", [128, 4, 512], BF16)
                kn_sb = sb(st, "kn_sb", [128, 4, 512], BF16)
                kr_sb = sb(st, "kr_sb", [128, 512], BF16)
                pA = [ps(st, "pA%d" % i, [128, 1024], BF16) for i in range(4)]
                pB = [ps(st, "pB%d" % i, [128, 512]) for i in range(4)]
                b_pA = P.bufs("pA", 4)
                b_pB = P.bufs("pB", 4)
                b_w = P.buf("w")
                b_xt = P.bufs("xt", 2)
                b_stat = P.bufs("stat", 2)
                b_junk, b_xn = P.buf("junk"), P.bufs("xn", 2)
                b_hb = P.bufs("hb", 2)
                b_hT = P.bufs("hT", 2)
                b_cs = P.bufs("cs", 2)
                b_cg, b_u, b_bg, b_qk = P.bufs("cg", 2), P.bufs("u", 2), P.bufs("bg", 2), P.bufs("qk", 4)
                b_va, b_va2 = P.bufs("va", 2), P.bufs("va2", 2)
                b_zb, b_gt1, b_gt2, b_gg, b_vn = P.buf("zb"), P.buf("gt1"), P.buf("gt2"), P.buf("gg"), P.buf("vn")
                b_yb = P.bufs("yb", 2)
                b_cqb = P.bufs("cqb", 2)
                b_cqnT = P.buf("cqnT")
                b_rt1, b_rt2 = P.buf("rt1"), P.buf("rt2")
                b_qT = P.bufs("qT", 4)
                b_kn = P.bufs("kn", 4)
                b_kr = P.buf("kr")

                for c in range(KC):
                    for h2 in range(2):
                        P.dma("pool", win[:, c, h2 * 1232:(h2 + 1) * 1232],
                              w_in[l][c * 128:(c + 1) * 128, h2 * 1232:(h2 + 1) * 1232], b_w, (), (b_w,), nowaw=True)
                for c in range(2):
                    P.dma("pool", wuq[:, c, :], w_uq[l][c * 128:(c + 1) * 128, :], b_w, (), (b_w,), nowaw=True)
                P.dma("pool", wukv[:], w_ukv[l], b_w, (), (b_w,), nowaw=True)
                P.dma("act", sgw32[:], sg_w[l].rearrange("h p q -> p h q"), b_w, (), (b_w,), nowaw=True)
                P.dma("act", sgb[:], sg_b[l].rearrange("h p -> p h"), b_w, (), (b_w,), nowaw=True, slow=True)
                P.dma("act", qkvg[:, 0:2], q_norm_g[l].rearrange("(c p) -> p c", p=128), b_w, (), (b_w,),
                      nowaw=True, slow=True)
                P.dma("act", qkvg[:, 2:3], kv_norm_g[l].rearrange("(c p) -> p c", p=128), b_w, (), (b_w,),
                      nowaw=True, slow=True)
                load_bc("act", gm_bc, 1, b_w)
                load_bc("act", sh_bc, 0, b_w)
                b_wd = P.buf("wd")
                P.memset("pool", wkr[:], 0.0, (b_wd,))
                P.memset("pool", wuqrot[:], 0.0, (b_wd,))
                KR0 = MLA0 + 384
                P.cp("dve", wkr[:, :, 0, 64:96], win[:, :, KR0:KR0 + 32], (b_w,), (b_wd,))
                for (dst, src, sgn) in ((0, 8, -1.0), (8, 0, 1.0), (16, 24, -1.0), (24, 16, 1.0)):
                    P.ts("dve", wkr[:, :, 1, 64 + dst:72 + dst], win[:, :, KR0 + src:KR0 + src + 8], sgn, None,
                         ALU.mult, None, (b_w,), (b_wd,))
                    for h in range(4):
                        P.ts("dve", wuqrot[:, :, h, 64 + dst:72 + dst],
                             wuq[:, :, h * 96 + 64 + src:h * 96 + 72 + src], sgn, None, ALU.mult, None,
                             (b_w,), (b_wd,))
                for h in range(4):
                    P.cp("dve", wukv_v[:, h * 64:(h + 1) * 64], wukv[:, h * 128 + 64:(h + 1) * 128], (b_w,), (b_wd,))
                P.cp("dve", sgwb[:], sgw32[:], (b_w,), (b_wd,))
                for h in range(4):
                    P.tr(pA[0][:, h * 128:(h + 1) * 128], sgwb[:, h, :], ident_b, (b_wd,), (b_pA[0],), nowaw=(h > 0))
                P.cp("dve", sgwT[:].rearrange("p h q -> p (h q)"), pA[0][:, 0:512], (b_pA[0],), (b_wd,))
                for s in range(2):
                    P.memset("pool", vaug[:, s, :, 64:65], 1.0, (b_va[s],))
                    P.memset("pool", vaug2[:, s, :, 64:65], 1.0, (b_va2[s],))

                pbi = [0]

                def nextpb():
                    i = pbi[0] % 4
                    pbi[0] += 1
                    return i

                evi = [0]

                def evac_eng():
                    evi[0] += 1
                    return "act" if evi[0] % 2 else "dve"

                groups = cfg.groups(True)
                def front(gi, tiles, v):
                    nt = len(tiles)
                    N = 128 * nt
                    t0 = tiles[0] * 128
                    gs = gi % 2
                    P.dma("sp", cos_t[:, gs, 0:N], cosT_in[:, t0:t0 + N], b_cs[gs], (), (b_cs[gs],), nowaw=False)
                    P.dma("sp", sin_t[:, gs, 0:N], sinT_in[:, t0:t0 + N], b_cs[gs], (), (b_cs[gs],), nowaw=True)
                    for i, t in enumerate(tiles):
                        s = (gi * 4 + i) % 2
                        P.dma("sp", xt[:, s, :], x_src(l, t), b_xt[s], (), (b_xt[s],))
                        P.act(junk[:], xt[:, s, :], AF.Square, (b_xt[s],), (b_junk, b_stat[s]), accum=stat[:, s, 0:1])
                        rms_rstd(stat[:, s, 0:1], stat[:, s, 1:2], D, (b_stat[s],), (b_stat[s],))
                        P.stt("dve", xn[:, s, :], xt[:, s, :], stat[:, s, 1:2], gm_bc[:, v, :], ALU.mult, ALU.mult,
                              (b_xt[s], b_stat[s], b_w), (b_xn[s],))
                        P.tt("pool", hb[:, s, :], xn[:, s, :], sh_bc[:, v, :], ALU.add, (b_xn[s], b_w), (b_hb[s],))
                        for c in range(KC):
                            P.tr(pA[c // 2][:, (c % 2) * 512 + i * 128:(c % 2) * 512 + (i + 1) * 128],
                                 hb[:, s, c * 128:(c + 1) * 128], ident_b, (b_hb[s],), (b_pA[c // 2],),
                                 nowaw=not (i == 0 and c % 2 == 0))
                    for c in range(KC):
                        P.cp("act" if (c // 2) % 2 == 0 else "dve", hT[:, gs, c, 0:N],
                             pA[c // 2][:, (c % 2) * 512:(c % 2) * 512 + N],
                             (b_pA[c // 2],), (b_hT[gs],), nowaw=(c > 0))


                def back(gi, tiles, v):
                    nt = len(tiles)
                    N = 128 * nt
                    t0 = tiles[0] * 128
                    gs = gi % 2
                    def fm_block(col0, width=128):
                        pi = nextpb()
                        for c in range(KC):
                            P.mm(pB[pi][0:width, 0:N], win[:, c, col0:col0 + width], hT[:, gs, c, 0:N],
                                 c == 0, c == KC - 1, (b_w, b_hT[gs]), (b_pB[pi],))
                        return pi

                    for blk in range(2):
                        pi = fm_block(blk * 128)
                        P.cp("act", bg_sb[:, blk, 0:N], pB[pi][:, 0:N], (b_pB[pi],), (b_bg[blk],))
                        P.dma("sp", BGT[blk * 128:(blk + 1) * 128, t0:t0 + N], bg_sb[:, blk, 0:N], b_bg[blk],
                              (b_bg[blk],), ())
                    for blk in range(2):
                        pi = fm_block(256 + blk * 128)
                        P.cp("act", cg_sb[:, blk, 0:N], pB[pi][:, 0:N], (b_pB[pi],), (b_cg[blk],))
                    for blk in range(2):
                        pi = fm_block(512 + blk * 128)
                        P.tt("dve", u_sb[:, blk, 0:N], pB[pi][:, 0:N], cg_sb[:, blk, 0:N], ALU.mult,
                             (b_pB[pi], b_cg[blk]), (b_u[blk],))
                        P.dma("sp", UT[blk * 128:(blk + 1) * 128, t0:t0 + N], u_sb[:, blk, 0:N], b_u[blk],
                              (b_u[blk],), ())
                    for blk in range(2):
                        pi = fm_block(NA0 + blk * 128)
                        P.act(qk_sb[:, blk, 0:N], pB[pi][:, 0:N], AF.Copy, (b_pB[pi],), (b_qk[blk],), scale=0.125)
                        P.dma("sp", QN_T[blk * 128:(blk + 1) * 128, t0:t0 + N], qk_sb[:, blk, 0:N], b_qk[blk],
                              (b_qk[blk],), ())
                    for blk in range(2):
                        pi = fm_block(NA0 + 256 + blk * 128)
                        P.cp("act", qk_sb[:, 2 + blk, 0:N], pB[pi][:, 0:N], (b_pB[pi],), (b_qk[2 + blk],))
                        P.dma("sp", KN_T[blk * 128:(blk + 1) * 128, t0:t0 + N], qk_sb[:, 2 + blk, 0:N], b_qk[2 + blk],
                              (b_qk[2 + blk],), ())
                    pr = []
                    for j in range(2):
                        pi = nextpb()
                        for c in range(KC):
                            P.mm(pB[pi][0:96, 0:N], wkr[:, c, j, :], hT[:, gs, c, 0:N], c == 0, c == KC - 1,
                                 (b_wd, b_hT[gs]), (b_pB[pi],))
                        pr.append(pi)
                    P.tt("dve", rt1[64:96, 0:N], pB[pr[0]][64:96, 0:N], cos_t[64:96, gs, 0:N], ALU.mult,
                         (b_pB[pr[0]], b_cs[gs]), (b_rt1,))
                    P.tt("dve", rt2[64:96, 0:N], pB[pr[1]][64:96, 0:N], sin_t[64:96, gs, 0:N], ALU.mult,
                         (b_pB[pr[1]], b_cs[gs]), (b_rt2,))
                    P.tt("pool", kr_sb[64:96, 0:N], rt1[64:96, 0:N], rt2[64:96, 0:N], ALU.add,
                         (b_rt1, b_rt2), (b_kr,))
                    for h in range(4):
                        P.dma("sp", KM_T[h, 64:96, t0:t0 + N], kr_sb[64:96, 0:N], b_kr, (b_kr,), ())

                    for i, t in enumerate(tiles):
                        s = (gi * 4 + i) % 2
                        tok = slice(i * 128, (i + 1) * 128)
                        pi = nextpb()
                        for c in range(KC):
                            P.mm(pB[pi][:, 0:512], hT[:, gs, c, tok], win[:, c, SG0:SG0 + 512], c == 0, c == KC - 1,
                                 (b_w, b_hT[gs]), (b_pB[pi],))
                        P.cp("act", zb[:], pB[pi][:, 0:512], (b_pB[pi],), (b_zb,))
                        P.tt("dve", gt1[:], zb[:], zb[:], ALU.mult, (b_zb,), (b_gt1,))
                        P.ts("dve", gt1[:], gt1[:], 0.044715, 1.0, ALU.mult, ALU.add, (b_gt1,), (b_gt1,))
                        P.tt("dve", gt1[:], gt1[:], zb[:], ALU.mult, (b_gt1, b_zb), (b_gt1,))
                        P.act(gt2[:], gt1[:], AF.Sigmoid, (b_gt1,), (b_gt2,), scale=1.5957691216057308)
                        P.tt("dve", gg[:], gt2[:], zb[:], ALU.mult, (b_gt2, b_zb), (b_gg,))
                        P.red("dve", stat[:, s, 2:3], gg[:, 256:512], ALU.add, (b_gg,), (b_stat[s],))
                        P.act(junk[:, 0:256], gg[:, 256:512], AF.Square, (b_gg,), (b_junk, b_stat[s]),
                              accum=stat[:, s, 3:4])
                        P.ts("dve", stat[:, s, 2:3], stat[:, s, 2:3], 1.0 / 256, None, ALU.mult, None,
                             (b_stat[s],), (b_stat[s],))
                        P.tt("dve", stat[:, s, 4:5], stat[:, s, 2:3], stat[:, s, 2:3], ALU.mult,
                             (b_stat[s],), (b_stat[s],))
                        P.stt("dve", stat[:, s, 3:4], stat[:, s, 3:4], 1.0 / 256, stat[:, s, 4:5],
                              ALU.mult, ALU.subtract, (b_stat[s],), (b_stat[s],))
                        P.ts("dve", stat[:, s, 3:4], stat[:, s, 3:4], EPS, None, ALU.add, None,
                             (b_stat[s],), (b_stat[s],))
                        P.act(stat[:, s, 3:4], stat[:, s, 3:4], AF.Sqrt, (b_stat[s],), (b_stat[s],))
                        P.add("dve", lambda e, s=s: e.reciprocal(stat[:, s, 3:4], stat[:, s, 3:4]),
                              (b_stat[s],), (b_stat[s],))
                        P.ts("dve", vn[:], gg[:, 256:512], stat[:, s, 2:3], stat[:, s, 3:4], ALU.subtract, ALU.mult,
                             (b_gg, b_stat[s]), (b_vn,))
                        pj = nextpb()
                        for h in range(4):
                            P.mm(pB[pj][:, h * 64:(h + 1) * 64], sgwT[:, h, :], vn[:, h * 64:(h + 1) * 64],
                                 True, True, (b_wd, b_vn), (b_pB[pj],))
                        for h in range(4):
                            P.stt("dve", yb[:, s, h * 64:(h + 1) * 64], pB[pj][:, h * 64:(h + 1) * 64],
                                  sgb[:, h:h + 1], gg[:, h * 64:(h + 1) * 64], ALU.add, ALU.mult,
                                  (b_pB[pj], b_gg, b_w), (b_yb[s],), nowaw=(h > 0))
                        P.dma("sp", Y[t * 128:(t + 1) * 128, 256:512], yb[:, s, :], b_yb[s], (b_yb[s],), ())
                        pi = nextpb()
                        for c in range(KC):
                            P.mm(pB[pi][:, 0:256], hT[:, gs, c, tok], win[:, c, NA0 + 512:NA0 + 768], c == 0,
                                 c == KC - 1, (b_w, b_hT[gs]), (b_pB[pi],))
                        P.cp("act", vaug[:, s, :, 0:64], pB[pi][:, 0:256].rearrange("p (h d) -> p h d", h=4),
                             (b_pB[pi],), (b_va[s],))
                        P.dma("sp", VN[t * 128:(t + 1) * 128, :], vaug[:, s].rearrange("p h d -> p (h d)"),
                              b_va[s], (b_va[s],), ())
                        pi = nextpb()
                        for c in range(KC):
                            P.mm(pB[pi][:, 0:384], hT[:, gs, c, tok], win[:, c, MLA0:MLA0 + 384], c == 0,
                                 c == KC - 1, (b_w, b_hT[gs]), (b_pB[pi],))
                        P.act(junk[:, 0:256], pB[pi][:, 0:256], AF.Square, (b_pB[pi],), (b_junk, b_stat[s]),
                              accum=stat[:, s, 5:6])
                        P.act(junk[:, 256:384], pB[pi][:, 256:384], AF.Square, (b_pB[pi],), (b_junk, b_stat[s]),
                              accum=stat[:, s, 6:7])
                        rms_rstd(stat[:, s, 5:6], stat[:, s, 5:6], 256, (b_stat[s],), (b_stat[s],))
                        rms_rstd(stat[:, s, 6:7], stat[:, s, 6:7], 128, (b_stat[s],), (b_stat[s],))
                        P.act(cq_b[:, s, 0:256], pB[pi][:, 0:256], AF.Copy, (b_pB[pi], b_stat[s]), (b_cqb[s],),
                              scale=stat[:, s, 5:6])
                        P.act(cq_b[:, s, 256:384], pB[pi][:, 256:384], AF.Copy, (b_pB[pi], b_stat[s]), (b_cqb[s],),
                              scale=stat[:, s, 6:7], nowaw=True)
                        for b3 in range(3):
                            P.tr(pA[b3][:, i * 128:(i + 1) * 128], cq_b[:, s, b3 * 128:(b3 + 1) * 128], ident_b,
                                 (b_cqb[s],), (b_pA[b3],), nowaw=(i > 0))
                    for b3 in range(3):
                        P.ts("dve", cqnT[:, b3, 0:N], pA[b3][:, 0:N], qkvg[:, b3:b3 + 1], None, ALU.mult, None,
                             (b_pA[b3], b_w), (b_cqnT,), nowaw=(b3 > 0))
                    for h in range(4):
                        hs = h
                        p1 = nextpb()
                        for c in range(2):
                            P.mm(pB[p1][0:96, 0:N], wuq[:, c, h * 96:(h + 1) * 96], cqnT[:, c, 0:N], c == 0, c == 1,
                                 (b_w, b_cqnT), (b_pB[p1],))
                        p2 = nextpb()
                        for c in range(2):
                            P.mm(pB[p2][0:96, 0:N], wuqrot[:, c, h, :], cqnT[:, c, 0:N], c == 0, c == 1,
                                 (b_wd, b_cqnT), (b_pB[p2],))
                        P.cp("act", qT[0:64, hs, 0:N], pB[p1][0:64, 0:N], (b_pB[p1],), (b_qT[hs],))
                        P.tt("dve", rt1[64:96, 0:N], pB[p1][64:96, 0:N], cos_t[64:96, gs, 0:N], ALU.mult,
                             (b_pB[p1], b_cs[gs]), (b_rt1,))
                        P.tt("dve", rt2[64:96, 0:N], pB[p2][64:96, 0:N], sin_t[64:96, gs, 0:N], ALU.mult,
                             (b_pB[p2], b_cs[gs]), (b_rt2,))
                        P.tt("pool", qT[64:96, hs, 0:N], rt1[64:96, 0:N], rt2[64:96, 0:N], ALU.add,
                             (b_rt1, b_rt2), (b_qT[hs],), nowaw=True)
                        P.dma("sp", QM_T[h, :, t0:t0 + N], qT[0:96, hs, 0:N], b_qT[hs], (b_qT[hs],), ())
                    for h in range(4):
                        hs = h
                        pi = nextpb()
                        P.mm(pB[pi][0:64, 0:N], wukv[:, h * 128:h * 128 + 64], cqnT[:, 2, 0:N], True, True,
                             (b_w, b_cqnT), (b_pB[pi],))
                        P.cp("act", kn_sb[0:64, hs, 0:N], pB[pi][0:64, 0:N], (b_pB[pi],), (b_kn[hs],))
                        P.dma("sp", KM_T[h, 0:64, t0:t0 + N], kn_sb[0:64, hs, 0:N], b_kn[hs], (b_kn[hs],), ())
                    for i, t in enumerate(tiles):
                        s = (gi * 4 + i) % 2
                        pi = nextpb()
                        P.mm(pB[pi][:, 0:256], cqnT[:, 2, i * 128:(i + 1) * 128], wukv_v[:], True, True,
                             (b_wd, b_cqnT), (b_pB[pi],))
                        P.cp("act", vaug2[:, s, :, 0:64], pB[pi][:, 0:256].rearrange("p (h d) -> p h d", h=4),
                             (b_pB[pi],), (b_va2[s],))
                        P.dma("sp", VM[t * 128:(t + 1) * 128, :], vaug2[:, s].rearrange("p h d -> p (h d)"),
                              b_va2[s], (b_va2[s],), ())
                if groups:
                    front(0, *groups[0])
                for gi, (tiles, v) in enumerate(groups):
                    if gi + 1 < len(groups):
                        front(gi + 1, *groups[gi + 1])
                    back(gi, tiles, v)
                P.end()
        def s3(l, last):
            with ExitStack() as st:
                ut = sb(st, "ut", [128, 2, 2, 514])
                bgt = sb(st, "bgt", [128, 2, 2, 512])
                cw = sb(st, "cw", [128, 2, 3])
                acc = sb(st, "acc", [128, 2, 512])
                ya = sb(st, "ya", [128, 2, 512])
                yat = sb(st, "yat", [128, 2, 256])
                pF = [ps(st, "pF%d" % i, [128, 512]) for i in range(2)]
                b_ut, b_bgt = P.bufs("ut", 2), P.bufs("bgt", 2)
                b_cw, b_acc, b_ya = P.buf("cw"), P.bufs("acc", 2), P.bufs("ya", 2)
                b_yat, b_pF = P.bufs("yat", 2), P.bufs("pF", 2)
                for blk in range(2):
                    for k3 in range(3):
                        P.dma("act", cw[:, blk, k3:k3 + 1],
                              conv_w[l][k3, blk * 128:(blk + 1) * 128].rearrange("(p o) -> p o", o=1),
                              b_cw, (), (b_cw,), nowaw=True, slow=True)
                cnt = 0
                for gi, (tiles, v) in enumerate(cfg.groups(not last)):
                    nt = len(tiles)
                    N = 128 * nt
                    t0 = tiles[0] * 128
                    s0_, s1_ = (0, NL) if v == 0 else (NL, NT)
                    gs = gi % 2
                    lo = max(t0 - 1, s0_)
                    hi = min(t0 + N + 1, s1_)
                    off = lo - (t0 - 1)
                    if t0 - 1 < s0_:
                        P.memset("pool", ut[:, gs, :, 0:1], 0.0, (b_ut[gs],))
                    if t0 + N + 1 > s1_:
                        P.memset("pool", ut[:, gs, :, N + 1:N + 2], 0.0, (b_ut[gs],), nowaw=True)
                    for blk in range(2):
                        P.dma("sp", ut[:, gs, blk, off:off + hi - lo], UT[blk * 128:(blk + 1) * 128, lo:hi],
                              b_ut[gs], (), (b_ut[gs],), nowaw=True)
                        P.dma("sp", bgt[:, gs, blk, 0:N], BGT[blk * 128:(blk + 1) * 128, t0:t0 + N],
                              b_bgt[gs], (), (b_bgt[gs],), nowaw=True)
                    for blk in range(2):
                        P.ts("dve", acc[:, blk, 0:N], ut[:, gs, blk, 0:N], cw[:, blk, 0:1], None, ALU.mult, None,
                             (b_ut[gs], b_cw), (b_acc[blk],))
                        P.stt("dve", acc[:, blk, 0:N], ut[:, gs, blk, 1:N + 1], cw[:, blk, 1:2], acc[:, blk, 0:N],
                              ALU.mult, ALU.add, (b_ut[gs], b_cw, b_acc[blk]), (b_acc[blk],))
                        P.stt("dve", acc[:, blk, 0:N], ut[:, gs, blk, 2:N + 2], cw[:, blk, 2:3], acc[:, blk, 0:N],
                              ALU.mult, ALU.add, (b_ut[gs], b_cw, b_acc[blk]), (b_acc[blk],))
                        P.tt("pool", ya[:, blk, 0:N], acc[:, blk, 0:N], bgt[:, gs, blk, 0:N], ALU.mult,
                             (b_acc[blk], b_bgt[gs]), (b_ya[blk],))
                    for i, t in enumerate(tiles):
                        s = cnt % 2
                        cnt += 1
                        for blk in range(2):
                            P.tr(pF[s][:, blk * 128:(blk + 1) * 128], ya[:, blk, i * 128:(i + 1) * 128], ident_f,
                                 (b_ya[blk],), (b_pF[s],), nowaw=(blk > 0))
                        P.cp("act", yat[:, s, :], pF[s][:, 0:256], (b_pF[s],), (b_yat[s],))
                        P.dma("sp", Y[t * 128:(t + 1) * 128, 0:256], yat[:, s, :], b_yat[s], (b_yat[s],), ())
                P.end()

        def s4(l, last):
            with ExitStack() as st:
                kn = sb(st, "kn", [128, 2, NT], BF16)
                vns = sb(st, "vns", [128, NTT, 260], BF16)
                bias = sb(st, "bias", [128, 4, NV, 128], BF16)
                qt = sb(st, "qt", [128, 2, 2, 128], BF16)
                pp = sb(st, "pp", [128, 2, 8, 128], BF16)
                yc = sb(st, "yc", [128, 2, 256])
                rec = sb(st, "rec", [128, 2, 4])
                pS = [ps(st, "pS%d" % i, [128, 2, 512]) for i in range(2)]
                pO = [ps(st, "pO%d" % i, [128, 512]) for i in range(2)]
                b_kv, b_bias = P.buf("kv"), P.buf("bias")
                b_qt, b_pp, b_yc, b_rec = P.bufs("qt", 2), P.bufs("pp", 2), P.bufs("yc", 2), P.bufs("rec", 2)
                b_pS, b_pO = P.bufs("pS", 2), P.bufs("pO", 2)
                for blk in range(2):
                    for h0 in range(0, NT, 2048):
                        h1 = min(NT, h0 + 2048)
                        P.dma("sp", kn[:, blk, h0:h1], KN_T[blk * 128:(blk + 1) * 128, h0:h1], b_kv, (), (b_kv,),
                              nowaw=True)
                for t0_ in range(0, NTT, 8):
                    t1_ = min(NTT, t0_ + 8)
                    P.dma("act", vns[:, t0_:t1_, :], VN[t0_ * 128:t1_ * 128, :].rearrange("(t p) f -> p t f", p=128),
                          b_kv, (), (b_kv,), nowaw=True)
                P.dma("act", bias[:].rearrange("p h v q -> p (h v q)"),
                      na_bias[l].rearrange("p h v q -> p (h v q)"), b_bias, (), (b_bias,))
                tiles = list(range(NTL)) + ([] if last else list(range(NTL, NTT)))
                ctx_chunks = [(m, None) for m in range(NTL, NTT)]
                cnt = 0
                for ti, t in enumerate(tiles):
                    chunks = (cfg.plan[t] + ctx_chunks) if t < NTL else ctx_chunks
                    nch = len(chunks)
                    s = ti % 2
                    P.dma("sp", qt[:, s], QN_T[:, t * 128:(t + 1) * 128].rearrange("(b p) q -> p b q", p=128),
                          b_qt[s], (), (b_qt[s],))
                    for h in range(4):
                        u = cnt % 2
                        cnt += 1
                        hb_, base = h // 2, 64 * (h % 2)
                        for i, (m, var) in enumerate(chunks):
                            o = pS[u][:, i // 4, (i % 4) * 128:(i % 4 + 1) * 128]
                            P.mm(o, kn[base:base + 64, hb_, m * 128:(m + 1) * 128], qt[base:base + 64, s, hb_, :],
                                 True, var is None, (b_kv, b_qt[s]), (b_pS[u],))
                            if var is not None:
                                P.mm(o, ident_b, bias[:, h, var, :], False, True, (b_bias,), (b_pS[u],))
                        P.act(pp[:, u, 0:nch, :], pS[u][:].rearrange("p a (b q) -> p (a b) q", q=128)[:, 0:nch, :],
                              AF.Exp, (b_pS[u],), (b_pp[u],))
                        for i, (m, var) in enumerate(chunks):
                            P.mm(pO[s][:, h * 65:(h + 1) * 65], pp[:, u, i, :], vns[:, m, h * 65:(h + 1) * 65],
                                 i == 0, i == nch - 1, (b_pp[u], b_kv), (b_pO[s],))
                    cnt = cnt
                    o4 = pO[s][:, 0:260].rearrange("p (h d) -> p h d", h=4)
                    P.add("dve", lambda e, o4=o4, s=s: e.reciprocal(rec[:, s, :], o4[:, :, 64]),
                          (b_pO[s],), (b_rec[s],))
                    for h in range(4):
                        P.ts("dve", yc[:, s, h * 64:(h + 1) * 64], pO[s][:, h * 65:h * 65 + 64], rec[:, s, h:h + 1],
                             None, ALU.mult, None, (b_pO[s], b_rec[s]), (b_yc[s],), nowaw=(h > 0))
                    P.dma("sp", Y[t * 128:(t + 1) * 128, 512:768], yc[:, s, :], b_yc[s], (b_yc[s],), ())
                P.end()

        def s5(l, last):
            with ExitStack() as st:
                km = sb(st, "km", [128, 4, NT], BF16)
                vms = sb(st, "vms", [128, NTT, 260], BF16)
                qm = sb(st, "qm", [128, 2, 4, 512], BF16)
                pp = sb(st, "pp", [128, 4, 512], BF16)
                yd = sb(st, "yd", [128, 2, 4, 256])
                rec = sb(st, "rec", [128, 2, 4])
                NS = 5
                pS = [ps(st, "pS%d" % i, [128, 512]) for i in range(NS)]
                pO = [ps(st, "pO%d" % i, [128, 512]) for i in range(2)]
                b_kv = P.buf("kv")
                b_qm, b_pp, b_yd, b_rec = P.bufs("qm", 2), P.bufs("pp", 4), P.bufs("yd", 2), P.bufs("rec", 2)
                b_pS, b_pO = P.bufs("pS", NS), P.bufs("pO", 2)
                for h in range(4):
                    for h0 in range(0, NT, 2048):
                        h1 = min(NT, h0 + 2048)
                        P.dma("sp", km[0:96, h, h0:h1], KM_T[h, :, h0:h1], b_kv, (), (b_kv,), nowaw=True)
                for t0_ in range(0, NTT, 8):
                    t1_ = min(NTT, t0_ + 8)
                    P.dma("act", vms[:, t0_:t1_, :], VM[t0_ * 128:t1_ * 128, :].rearrange("(t p) f -> p t f", p=128),
                          b_kv, (), (b_kv,), nowaw=True)
                all_chunks = list(range(NTT))
                ctx_only = list(range(NTL, NTT))
                cnt = 0
                si = 0
                for gi, (tiles, v) in enumerate(cfg.groups(not last)):
                    nt = len(tiles)
                    N = 128 * nt
                    t0 = tiles[0] * 128
                    gs = gi % 2
                    chunks = all_chunks if v == 0 else ctx_only
                    nch = len(chunks)
                    for h in range(4):
                        P.dma("sp", qm[0:96, gs, h, 0:N], QM_T[h, :, t0:t0 + N], b_qm[gs], (), (b_qm[gs],), nowaw=True)
                    for h in range(4):
                        u = cnt % 2
                        cnt += 1
                        pend = []

                        def qk(ci):
                            nonlocal si
                            m = chunks[ci]
                            k = si % NS
                            si += 1
                            P.mm(pS[k][:, 0:N], km[0:96, h, m * 128:(m + 1) * 128], qm[0:96, gs, h, 0:N], True, True,
                                 (b_kv, b_qm[gs]), (b_pS[k],))
                            pend.append((ci, k))

                        def pv():
                            ci, k = pend.pop(0)
                            m = chunks[ci]
                            pslot = ci % 4
                            P.act(pp[:, pslot, 0:N], pS[k][:, 0:N], AF.Exp, (b_pS[k],), (b_pp[pslot],), scale=MLA_SCALE)
                            for sub in range(nt):
                                P.mm(pO[u][:, sub * 65:(sub + 1) * 65], pp[:, pslot, sub * 128:(sub + 1) * 128],
                                     vms[:, m, h * 65:(h + 1) * 65], ci == 0 and sub == 0,
                                     ci == nch - 1 and sub == nt - 1,
                                     (b_pp[pslot], b_kv), (b_pO[u],))

                        LOOK = 2
                        for ci in range(nch):
                            qk(ci)
                            if len(pend) > LOOK:
                                pv()
                        while pend:
                            pv()
                        o4 = pO[u][:, 0:nt * 65].rearrange("p (s d) -> p s d", d=65)
                        P.add("dve", lambda e, o4=o4, u=u, nt=nt: e.reciprocal(rec[:, u, 0:nt], o4[:, :, 64]),
                              (b_pO[u],), (b_rec[u],))
                        for sub in range(nt):
                            P.ts("dve", yd[:, gs, sub, h * 64:(h + 1) * 64], pO[u][:, sub * 65:sub * 65 + 64],
                                 rec[:, u, sub:sub + 1], None, ALU.mult, None, (b_pO[u], b_rec[u]), (b_yd[gs],),
                                 nowaw=not (h == 0 and sub == 0))
                    for sub, t in enumerate(tiles):
                        P.dma("sp", Y[t * 128:(t + 1) * 128, 768:1024], yd[:, gs, sub, :], b_yd[gs], (b_yd[gs],), ())
                P.end()
        def s6(l, last):
            with ExitStack() as st:
                wout = sb(st, "wout", [128, KC, D], BF16)
                wr = sb(st, "wr", [128, KC, 36], BF16)
                brt = sb(st, "brt", [128, 36])
                outg = sb(st, "outg", [128, KC])
                g1_bc = sb(st, "g1_bc", [128, 2, D])
                gm2_bc = sb(st, "gm2_bc", [128, 2, D])
                sh2_bc = sb(st, "sh2_bc", [128, 2, D])
                yt = sb(st, "yt", [128, 2, D])
                junk = sb(st, "junk", [128, D], BF16)
                ynb = sb(st, "ynb", [128, 2, D], BF16)
                ynT = sb(st, "ynT", [128, 2, KC, 128], BF16)
                xt = sb(st, "xt", [128, 2, D])
                xnew = sb(st, "xnew", [128, 2, D])
                tmp = sb(st, "tmp", [128, D])
                h2b = sb(st, "h2b", [128, 2, D], BF16)
                h2T = sb(st, "h2T", [128, 2, KC, 128], BF16)
                stat = sb(st, "stat", [128, 2, 16])
                lg = sb(st, "lg", [128, 2, 36])
                rtmp = sb(st, "rtmp", [128, 2, 4, NE])
                amask = sb(st, "amask", [128, 2, NE], BF16)
                pA = [ps(st, "pA%d" % i, [128, 1024], BF16) for i in range(2)]
                pO = [ps(st, "pO%d" % i, [128, 2, 512]) for i in range(2)]
                pR = [ps(st, "pR%d" % i, [128, 512]) for i in range(2)]
                b_w = P.buf("w")
                b_yt, b_ynb, b_ynT, b_xt = P.bufs("yt", 2), P.bufs("ynb", 2), P.bufs("ynT", 2), P.bufs("xt", 2)
                b_xnew, b_h2b, b_h2T = P.bufs("xnew", 2), P.bufs("h2b", 2), P.bufs("h2T", 2)
                b_stat, b_lg, b_rtmp, b_am = P.bufs("stat", 2), P.bufs("lg", 2), P.bufs("rtmp", 2), P.bufs("am", 2)
                b_junk, b_tmp = P.buf("junk"), P.buf("tmp")
                b_pA, b_pO, b_pR = P.bufs("pA", 2), P.bufs("pO", 2), P.bufs("pR", 2)
                b_rout, b_carry = P.buf("rout"), P.buf("carry")
                for c in range(KC):
                    P.dma("pool", wout[:, c, :], w_out[l][c * 128:(c + 1) * 128, :], b_w, (), (b_w,), nowaw=True)
                P.dma("pool", wr[:, :, 0:4], w_grp[l].rearrange("(c p) n -> p c n", p=128), b_w, (), (b_w,), nowaw=True)
                P.dma("pool", wr[:, :, 4:36], w_exp[l].rearrange("(c p) n -> p c n", p=128), b_w, (), (b_w,), nowaw=True)
                P.dma("act", brt[:, 0:4], b_grp[l:l + 1, :].partition_broadcast(128), b_w, (), (b_w,), nowaw=True)
                P.dma("act", brt[:, 4:36], b_exp[l:l + 1, :].partition_broadcast(128), b_w, (), (b_w,), nowaw=True)
                P.dma("act", outg[:], out_norm_g[l].rearrange("(c p) -> p c", p=128), b_w, (), (b_w,), nowaw=True,
                      slow=True)
                load_bc("act", g1_bc, 2, b_w)
                load_bc("act", gm2_bc, 4, b_w)
                load_bc("act", sh2_bc, 3, b_w)
                P.memset("dve", carry[:], 0.0, (b_carry,))
                tiles = [(t, 0) for t in range(NTL)] + ([] if last else [(t, 1) for t in range(NTL, NTT)])
                def phaseA(ti, t, v):
                    s = ti % 2
                    rows = slice(t * 128, (t + 1) * 128)
                    P.dma("sp", yt[:, s, :], Y[rows, :], b_yt[s], (), (b_yt[s],))
                    P.dma("sp", xt[:, s, :], x_src(l, t), b_xt[s], (), (b_xt[s],))
                    for g in range(4):
                        P.act(junk[:, g * 256:(g + 1) * 256], yt[:, s, g * 256:(g + 1) * 256], AF.Square,
                              (b_yt[s],), (b_junk, b_stat[s]), accum=stat[:, s, g:g + 1])
                    rms_rstd(stat[:, s, 0:4], stat[:, s, 4:8], 256, (b_stat[s],), (b_stat[s],))
                    for g in range(4):
                        P.act(ynb[:, s, g * 256:(g + 1) * 256], yt[:, s, g * 256:(g + 1) * 256], AF.Copy,
                              (b_yt[s], b_stat[s]), (b_ynb[s],), scale=stat[:, s, 4 + g:5 + g], nowaw=(g > 0))
                    for c in range(KC):
                        P.tr(pA[s][:, c * 128:(c + 1) * 128], ynb[:, s, c * 128:(c + 1) * 128], ident_b,
                             (b_ynb[s],), (b_pA[s],), nowaw=(c > 0))
                    for c in range(KC):
                        P.ts("dve", ynT[:, s, c, :], pA[s][:, c * 128:(c + 1) * 128],
                             outg[:, c:c + 1], None, ALU.mult, None, (b_pA[s], b_w), (b_ynT[s],), nowaw=(c > 0))
                    for half in range(2):
                        for c in range(KC):
                            P.mm(pO[s][:, half, :], ynT[:, s, c, :], wout[:, c, half * 512:(half + 1) * 512],
                                 c == 0, c == KC - 1, (b_ynT[s], b_w), (b_pO[s],))
                    P.tt("dve", tmp[:], pO[s][:].rearrange("p a b -> p (a b)"), g1_bc[:, v, :], ALU.mult,
                         (b_pO[s], b_w), (b_tmp,))
                    P.tt("pool", xnew[:, s, :], tmp[:], xt[:, s, :], ALU.add, (b_tmp, b_xt[s]), (b_xnew[s],))
                    P.dma("sp", X[rows, :], xnew[:, s, :], b_xnew[s], (b_xnew[s],), ())
                    P.act(junk[:], xnew[:, s, :], AF.Square, (b_xnew[s],), (b_junk, b_stat[s]), accum=stat[:, s, 8:9])
                    rms_rstd(stat[:, s, 8:9], stat[:, s, 9:10], D, (b_stat[s],), (b_stat[s],))
                    P.stt("dve", tmp[:], xnew[:, s, :], stat[:, s, 9:10], gm2_bc[:, v, :], ALU.mult, ALU.mult,
                          (b_xnew[s], b_stat[s], b_w), (b_tmp,))
                    P.tt("pool", h2b[:, s, :], tmp[:], sh2_bc[:, v, :], ALU.add, (b_tmp, b_w), (b_h2b[s],))
                    P.dma("sp", H2[rows, :], h2b[:, s, :], b_h2b[s], (b_h2b[s],), ())
                    for c in range(KC):
                        P.tr(pA[s][:, c * 128:(c + 1) * 128], h2b[:, s, c * 128:(c + 1) * 128], ident_b,
                             (b_h2b[s],), (b_pA[s],), nowaw=(c > 0))
                    P.cp("act", h2T[:, s].rearrange("p c q -> p (c q)"), pA[s][:, :], (b_pA[s],), (b_h2T[s],))
                    for c in range(KC):
                        P.mm(pR[s][:, 0:36], h2T[:, s, c, :], wr[:, c, :], c == 0, c == KC - 1,
                             (b_h2T[s], b_w), (b_pR[s],))
                    LG = lg[:, s, :]
                    P.tt("dve", LG, pR[s][:, 0:36], brt[:], ALU.add, (b_pR[s], b_w), (b_lg[s],))

                def phaseB(ti, t, v):
                    s = ti % 2
                    R_, W_ = (b_lg[s], b_stat[s], b_rtmp[s]), (b_stat[s], b_rtmp[s])
                    sm = stat[:, s, 10:11]
                    P.red("dve", sm, lg[:, s, 0:4], ALU.max, R_, W_)
                    goh = rtmp[:, s, 0, 0:4]
                    P.ts("dve", goh, lg[:, s, 0:4], sm, None, ALU.is_ge, None, R_, W_)
                    P.ts("dve", stat[:, s, 11:12], sm, -1.0, None, ALU.mult, None, R_, W_)
                    P.act(rtmp[:, s, 0, 4:8], lg[:, s, 0:4], AF.Exp, R_, W_, bias=stat[:, s, 11:12],
                          accum=stat[:, s, 12:13])
                    pg = stat[:, s, 13:14]
                    P.add("dve", lambda e, pg=pg, s=s: e.reciprocal(pg, stat[:, s, 12:13]), R_, W_)
                    ml = rtmp[:, s, 1, :]
                    pen = rtmp[:, s, 2, :]
                    P.ts("dve", pen.rearrange("p (g e) -> p g e", g=4),
                         goh.unsqueeze(2).to_broadcast([128, 4, 8]), -1.0, 1.0e4, ALU.add, ALU.mult, R_, W_)
                    P.tt("dve", ml, lg[:, s, 4:36], pen, ALU.add, R_, W_)
                    v1 = stat[:, s, 14:15]
                    v2 = stat[:, s, 15:16]
                    P.red("dve", v1, ml, ALU.max, R_, W_)
                    m1 = mask1[:, t, :]
                    m2 = mask2[:, t, :]
                    RW = W_ + (b_rout,)
                    P.ts("dve", m1, ml, v1, None, ALU.is_ge, None, R_, RW)
                    P.stt("dve", ml, m1, -1.0e4, ml, ALU.mult, ALU.add, R_ + (b_rout,), W_)
                    P.red("dve", v2, ml, ALU.max, R_, W_)
                    P.ts("dve", m2, ml, v2, None, ALU.is_ge, None, R_, RW)
                    dv = stat[:, s, 11:12]
                    P.tt("dve", dv, v2, v1, ALU.subtract, R_, W_)
                    P.act(dv, dv, AF.Exp, R_, W_)
                    P.ts("dve", dv, dv, 1.0, None, ALU.add, None, R_, W_)
                    P.add("dve", lambda e, dv=dv: e.reciprocal(dv, dv), R_, W_)
                    P.tt("dve", gw12[:, 0, t:t + 1], dv, pg, ALU.mult, R_, RW)
                    P.tt("dve", gw12[:, 1, t:t + 1], pg, gw12[:, 0, t:t + 1], ALU.subtract, R_ + (b_rout,), RW)
                    P.tt("dve", amask[:, s, :], m1, m2, ALU.add, (b_rout,), (b_am[s],))
                    P.mm(pR[s][:, 64:96], triu_b, amask[:, s, :], True, True, (b_am[s],), (b_pR[s],))
                    P.mm(pR[s][:, 128:160], ones_b, amask[:, s, :], True, True, (b_am[s],), (b_pR[s],))
                    rk = rtmp[:, s, 3, :]
                    P.tt("dve", rk, pR[s][:, 64:96], carry[:], ALU.add, (b_pR[s], b_carry) + R_, W_)
                    P.tt("dve", carry[:], carry[:], pR[s][:, 128:160], ALU.add, (b_pR[s], b_carry), (b_carry,))
                    for k, mk in ((0, m1), (1, m2)):
                        P.tt("dve", pen, rk, mk, ALU.mult, R_ + (b_rout,), W_)
                        P.red("dve", rank12[:, k, t:t + 1], pen, ALU.add, R_, RW)
                for ti, (t, v) in enumerate(tiles):
                    phaseA(ti, t, v)
                    phaseB(ti, t, v)
                P.end()

        def s7(l, last):
            with ExitStack() as st:
                pad = sb(st, "pad", [128, NE])
                pend = sb(st, "pend", [128, NE])
                pstart = sb(st, "pstart", [128, NE])
                big = sb(st, "big", [128, NTT, NE])
                destf = sb(st, "destf", [128, 2, NTT])
                cmp_ = sb(st, "cmp", [128, NBLK, NE])
                ebf = sb(st, "ebf", [128, NBLK])
                wif = sb(st, "wif", [128, 2, NBLK])
                h2r = sb(st, "h2r", [128, 3, D], BF16)
                b_p, b_big, b_dest, b_cmp, b_eb = P.buf("p"), P.buf("big"), P.buf("dest"), P.buf("cmp"), P.buf("eb")
                b_h2r = P.bufs("h2r", 3)
                JJ = (2 * NT) // BLK + 1
                cmpp = sb(st, "cmpp", [128, NE, JJ])
                P.tt("dve", cmpp[:], carry[:].unsqueeze(2).to_broadcast([128, NE, JJ]),
                     blkstart[:, 0:JJ].unsqueeze(1).to_broadcast([128, NE, JJ]), ALU.is_gt, (), (b_p,))
                P.red("dve", pad[:], cmpp[:], ALU.add, (b_p,), (b_p,))
                P.ts("dve", pad[:], pad[:], float(BLK), None, ALU.mult, None, (b_p,), (b_p,))
                P.cp("dve", pend[:, 0:1], pad[:, 0:1], (b_p,), (b_p,))
                for e_ in range(1, NE):
                    P.tt("dve", pend[:, e_:e_ + 1], pend[:, e_ - 1:e_], pad[:, e_:e_ + 1], ALU.add, (b_p,), (b_p,))
                P.tt("dve", pstart[:], pend[:], pad[:], ALU.subtract, (b_p,), (b_p,))
                ntt_used = NTL if last else NTT
                for k, mk in ((0, mask1), (1, mask2)):
                    P.tt("dve", big[:, 0:ntt_used, :], mk[:, 0:ntt_used, :],
                         pstart[:].unsqueeze(1).to_broadcast([128, ntt_used, NE]), ALU.mult, (b_p,), (b_big,))
                    P.red("dve", destf[:, k, 0:ntt_used], big[:, 0:ntt_used, :], ALU.add, (b_big,), (b_dest,))
                    P.tt("dve", destf[:, k, 0:ntt_used], destf[:, k, 0:ntt_used], rank12[:, k, 0:ntt_used], ALU.add,
                         (b_dest,), (b_dest,))
                P.cp("dve", dest_i[:, :, 0:ntt_used], destf[:, :, 0:ntt_used], (b_dest,), (b_dest,))
                P.tt("dve", cmp_[:], pend[:].unsqueeze(1).to_broadcast([128, NBLK, NE]),
                     blkstart[:, 0:NBLK].unsqueeze(2).to_broadcast([128, NBLK, NE]), ALU.is_le, (b_p,), (b_cmp,))
                P.red("dve", ebf[:], cmp_[:], ALU.add, (b_cmp,), (b_eb,))
                P.ts("dve", ebf[:], ebf[:], float(NE - 1), None, ALU.min, None, (b_eb,), (b_eb,))
                P.ts("dve", wif[:, 0, :], ebf[:], float(D), iota_p, ALU.mult, ALU.add, (b_eb,), (b_eb,))
                P.ts("dve", wif[:, 1, :], ebf[:], float(DE), iota_p, ALU.mult, ALU.add, (b_eb,), (b_eb,))
                P.cp("dve", widx[:], wif[:], (b_eb,), (b_eb,))
                for t in range(ntt_used):
                    s = t % 3
                    P.dma("sp", h2r[:, s, :], H2[t * 128:(t + 1) * 128, :], b_h2r[s], (), (b_h2r[s],))
                    for k in range(2):
                        P.scatter(XBUF, dest_i[:, k, t:t + 1], h2r[:, s, :], b_h2r[s], (b_h2r[s], b_dest), ())
                P.end()

        def s8(l, last):
            with ExitStack() as st:
                wg = sb(st, "wg", [128, 3, KC, DE], BF16)
                wu = sb(st, "wu", [128, 3, KC, DE], BF16)
                wd = sb(st, "wd", [128, 3, 4, D], BF16)
                xr = sb(st, "xr", [128, 2, 4, D], BF16)
                xT = sb(st, "xT", [128, 2, KC, 512], BF16)
                sg_ = sb(st, "sg", [128, 2, 512])
                hid = sb(st, "hid", [128, 2, 4, 512], BF16)
                yo = sb(st, "yo", [128, 2, D])
                pA = [ps(st, "pA%d" % i, [128, 1024], BF16) for i in range(2)]
                pG = [ps(st, "pG%d" % i, [128, 512]) for i in range(4)]
                pY = [ps(st, "pY%d" % i, [128, 512]) for i in range(2)]
                b_wg, b_wu, b_wd = P.bufs("wg", 3), P.bufs("wu", 3), P.bufs("wd", 3)
                b_xr, b_xT, b_sg, b_hid, b_yo = P.bufs("xr", 2), P.bufs("xT", 2), P.bufs("sg", 2), P.bufs("hid", 2), P.bufs("yo", 2)
                b_pA, b_pG, b_pY = P.bufs("pA", 2), P.bufs("pG", 4), P.bufs("pY", 2)
                nblk = NBLK
                pai = 0
                pgi = 0
                yi = 0
                for b in range(nblk):
                    s = b % 2
                    ws = b % 3
                    for c in range(KC):
                        P.gather(wg[:, ws, c, :], w_gate, widx[:, 0, b:b + 1], (l * NE * D + c * 128) * DE,
                                 b_wg[ws], (), (b_wg[ws],), nowaw=True)
                    for c in range(KC):
                        P.gather(wu[:, ws, c, :], w_up, widx[:, 0, b:b + 1], (l * NE * D + c * 128) * DE,
                                 b_wu[ws], (), (b_wu[ws],), nowaw=True)
                    for f in range(4):
                        P.gather(wd[:, ws, f, :], w_down, widx[:, 1, b:b + 1], (l * NE * DE + f * 128) * D,
                                 b_wd[ws], (), (b_wd[ws],), nowaw=True)
                    P.dma("sp", xr[:, s], XBUF[b * BLK:(b + 1) * BLK, :].rearrange("(i p) d -> p i d", p=128),
                          b_xr[s], (), (b_xr[s],))
                    for c in range(KC):
                        a = pai % 2
                        pai += 1
                        for i in range(4):
                            P.tr(pA[a][:, i * 128:(i + 1) * 128], xr[:, s, i, c * 128:(c + 1) * 128], ident_b,
                                 (b_xr[s],), (b_pA[a],), nowaw=(i > 0))
                        P.cp("act" if c % 2 else "dve", xT[:, s, c, :], pA[a][:, 0:512], (b_pA[a],), (b_xT[s],),
                             nowaw=(c > 0))
                    for f in range(4):
                        g0 = pgi % 4
                        g1 = (pgi + 1) % 4
                        pgi += 2
                        for c in range(KC):
                            P.mm(pG[g0][:, :], wg[:, ws, c, f * 128:(f + 1) * 128], xT[:, s, c, :], c == 0, c == KC - 1,
                                 (b_wg[ws], b_xT[s]), (b_pG[g0],))
                        for c in range(KC):
                            P.mm(pG[g1][:, :], wu[:, ws, c, f * 128:(f + 1) * 128], xT[:, s, c, :], c == 0, c == KC - 1,
                                 (b_wu[ws], b_xT[s]), (b_pG[g1],))
                        fs = f % 2
                        P.act(sg_[:, fs, :], pG[g0][:, :], AF.Silu, (b_pG[g0],), (b_sg[fs],))
                        P.tt("dve", hid[:, s, f, :], sg_[:, fs, :], pG[g1][:, :], ALU.mult, (b_sg[fs], b_pG[g1]),
                             (b_hid[s],), nowaw=(f > 0))
                    for i in range(4):
                        ys = yi % 2
                        yi += 1
                        for half in range(2):
                            for f in range(4):
                                P.mm(pY[half][:, :], hid[:, s, f, i * 128:(i + 1) * 128],
                                     wd[:, ws, f, half * 512:(half + 1) * 512], f == 0, f == 3,
                                     (b_hid[s], b_wd[ws]), (b_pY[half],))
                            P.cp("act" if half else "dve", yo[:, ys, half * 512:(half + 1) * 512], pY[half][:, :],
                                 (b_pY[half],), (b_yo[ys],), nowaw=(half > 0))
                        P.dma("sp", YBUF[b * BLK + i * 128:b * BLK + (i + 1) * 128, :], yo[:, ys, :], b_yo[ys],
                              (b_yo[ys],), ())
                P.end()

        def s9(l, last):
            with ExitStack() as st:
                g2_bc = sb(st, "g2_bc", [128, 2, D])
                fg_bc = sb(st, "fg_bc", [128, D])
                y1 = sb(st, "y1", [128, 4, D])
                y2 = sb(st, "y2", [128, 4, D])
                xt = sb(st, "xt", [128, 4, D])
                f1 = sb(st, "f1", [128, D])
                xo = sb(st, "xo", [128, 4, D])
                junk = sb(st, "junk", [128, D], BF16)
                stat = sb(st, "stat", [128, 4, 2])
                b_w = P.buf("w")
                b_y1, b_y2, b_xt, b_xo, b_stat = P.bufs("y1", 4), P.bufs("y2", 4), P.bufs("xt", 4), P.bufs("xo", 4), P.bufs("stat", 4)
                b_f1, b_junk = P.buf("f1"), P.buf("junk")
                load_bc("act", g2_bc, 5, b_w)
                if last:
                    P.dma("act", fg_bc[:], final_g.partition_broadcast(128), b_w, (), (b_w,), nowaw=True)
                tiles = [(t, 0) for t in range(NTL)] + ([] if last else [(t, 1) for t in range(NTL, NTT)])
                for ti, (t, v) in enumerate(tiles):
                    s = ti % 4
                    rows = slice(t * 128, (t + 1) * 128)
                    P.gather(y1[:, s, :], YBUF, dest_i[:, 0, t:t + 1], 0, b_y1[s], (), (b_y1[s],))
                    P.gather(y2[:, s, :], YBUF, dest_i[:, 1, t:t + 1], 0, b_y2[s], (), (b_y2[s],))
                    P.dma("sp", xt[:, s, :], X[rows, :], b_xt[s], (), (b_xt[s],))
                    P.ts("dve", f1[:], y1[:, s, :], gw12[:, 0, t:t + 1], None, ALU.mult, None, (b_y1[s],), (b_f1,))
                    P.stt("dve", f1[:], y2[:, s, :], gw12[:, 1, t:t + 1], f1[:], ALU.mult, ALU.add,
                          (b_y2[s], b_f1), (b_f1,))
                    P.tt("dve", f1[:], f1[:], g2_bc[:, v, :], ALU.mult, (b_f1, b_w), (b_f1,))
                    P.tt("dve", xo[:, s, :], f1[:], xt[:, s, :], ALU.add, (b_f1, b_xt[s]), (b_xo[s],))
                    if not last:
                        P.dma("sp", X[rows, :], xo[:, s, :], b_xo[s], (b_xo[s],), ())
                    else:
                        P.act(junk[:], xo[:, s, :], AF.Square, (b_xo[s],), (b_junk, b_stat[s]), accum=stat[:, s, 0:1])
                        rms_rstd(stat[:, s, 0:1], stat[:, s, 1:2], D, (b_stat[s],), (b_stat[s],))
                        P.stt("dve", xo[:, s, :], xo[:, s, :], stat[:, s, 1:2], fg_bc[:], ALU.mult, ALU.mult,
                              (b_xo[s], b_stat[s], b_w), (b_xo[s],))
                        P.dma("sp", out[rows, :], xo[:, s, :], b_xo[s], (b_xo[s],), ())
                P.end()

        stages = [s1, s2, s3, s4, s5, s6, s7, s8, s9]
        s0()
        done = cfg.stop is not None and cfg.stop[1] == 0
        for l in range(L):
            if done:
                break
            last = (l == L - 1)
            for si_, fn in enumerate(stages):
                if si_ == 0:
                    fn(l)
                else:
                    fn(l, last)
                if cfg.stop is not None and cfg.stop == (l, si_ + 1):
                    done = True
                    break
            if done:
                break
    return nc


def make_consts(cfg):
    cf = np.zeros((128, 353), np.float32)
    cf[:, 0:128] = np.eye(128, dtype=np.float32)
    cf[:, 128:256] = 1.0
    cf[:, 256:288] = np.arange(32, dtype=np.float32)[None, :]
    cf[:, 288] = np.arange(128, dtype=np.float32)
    cf[:, 289:353] = (np.arange(64, dtype=np.float32) * BLK)[None, :]
    cbm = np.zeros((128, 384), np.float32)
    cbm[:, 0:128] = np.eye(128)
    cbm[:, 128:256] = 1.0
    cbm[:, 256:384] = np.triu(np.ones((128, 128), np.float32), k=1)
    return cf, cbm.astype(ml_dtypes.bfloat16)


def core_inputs(cfg, inputs, b):
    L = cfg.L
    f = lambda a: np.ascontiguousarray(np.asarray(a, dtype=np.float32))
    cosT, sinT = rope_tables(cfg.NL, cfg.NCX)
    cf, cbm = make_consts(cfg)
    m = {
        "x": f(inputs["x"][b]),
        "ctx": f(inputs["ctx"][b]),
        "cc": f(np.stack([np.asarray(inputs["c"][b]), np.asarray(inputs["c_ctx"])], axis=0)),
        "na_bias": na_bias_tables(np.asarray(inputs["na_rpb"], dtype=np.float32)[:L], cfg.variants),
        "w_gate": f(inputs["w_gate"][:L]).reshape(L * NE * D, DE),
        "w_up": f(inputs["w_up"][:L]).reshape(L * NE * D, DE),
        "w_down": f(inputs["w_down"][:L]).reshape(L * NE * DE, D),
        "final_norm_g": f(inputs["final_norm_g"]).reshape(1, D),
        "cosT": cosT, "sinT": sinT, "consts_f": cf, "consts_b": cbm,
    }
    for k in ("w_ada", "b_ada", "norm1_g", "norm2_g", "w_in", "conv_w", "sg_w", "sg_b", "mla_q_norm_g",
              "mla_w_uq", "mla_kv_norm_g", "mla_w_ukv", "out_norm_g", "w_out", "w_grp", "b_grp", "w_exp", "b_exp"):
        m[k] = f(inputs[k][:L])
    return m


_NC_CACHE = {}


def kernel(**inputs):
    cfg = Cfg()
    if "nc" not in _NC_CACHE:
        _NC_CACHE["nc"] = build(cfg)
    nc = _NC_CACHE["nc"]
    per_b = [core_inputs(cfg, inputs, b) for b in range(4)]
    in_maps = [per_b[i % 4] for i in range(8)]
    res = run_bass_kernel_spmd(nc, in_maps, core_ids=list(range(8)))
    return np.stack([np.asarray(res.results[b]["out"], dtype=np.float32) for b in range(4)], axis=0)
```

```python
import numpy as np
import ml_dtypes
from contextlib import ExitStack
import concourse.bass as bass
import concourse.mybir as mybir
from concourse.bass_utils import run_bass_kernel_spmd

F32 = mybir.dt.float32
BF16 = mybir.dt.bfloat16
I32 = mybir.dt.int32
AF = mybir.ActivationFunctionType
ALU = mybir.AluOpType
AX = mybir.AxisListType

D = 1024
KC = 8
IN_COLS = 2464
SG0, NA0, MLA0 = 768, 1280, 2048
NE = 32
DE = 512
BLK = 512
EPS = 1e-6
MLA_SCALE = 96.0 ** -0.5
NEG = -30000.0
SAME_ENG_SYNC = True


class Buf:
    __slots__ = ("name", "writers", "readers", "gen", "excl")

    def __init__(self, name):
        self.name = name
        self.excl = (len(name) > 1 and name[0] == "p" and (name[1].isupper() or name[1] == "m"))
        self.writers = []
        self.readers = []
        self.gen = []


class Op:
    __slots__ = ("eng", "fn", "deps", "is_dma", "dbuf", "signal", "sem", "count")

    def __init__(self, eng, fn, is_dma, dbuf):
        self.eng = eng
        self.fn = fn
        self.deps = []
        self.is_dma = is_dma
        self.dbuf = dbuf
        self.signal = is_dma
        self.sem = None
        self.count = 0


ENGS = ("sp", "act", "dve", "pool", "pe")


class Prog:
    def __init__(self, nc, es, n_dsem=84):
        self.nc = nc
        self.csem = {e: es.enter_context(nc.semaphore("c_" + e)) for e in ("act", "dve", "pool", "pe")}
        self.ccount = {e: 0 for e in self.csem}
        n_sw = 24
        self.dsem = {False: [es.enter_context(nc.semaphore("d%d" % i)) for i in range(n_dsem - n_sw)],
                     True: [es.enter_context(nc.semaphore("w%d" % i)) for i in range(n_sw)]}
        self.dcount = {False: [0] * (n_dsem - n_sw), True: [0] * n_sw}
        self.waited = {e: {} for e in ENGS}
        self.nstage = 0
        self.begin()

    def begin(self):
        self.ops = {e: [] for e in ENGS}
        self.dmap = {}

    def buf(self, name):
        return Buf(name)

    def bufs(self, name, n):
        return [Buf("%s%d" % (name, i)) for i in range(n)]

    def add(self, eng, fn, reads=(), writes=(), dbuf=None, nowaw=False):
        op = Op(eng, fn, dbuf is not None, dbuf)
        deps = []
        xr = [b for b in reads if b.excl]
        reads = [b for b in reads if not b.excl]
        for b in reads:
            deps.extend(b.writers)
        newgen = []
        for b in xr:
            g = list(b.writers) + list(b.readers)
            deps.extend(g)
            newgen.append((b, g))
        writes = tuple(writes) + tuple(xr)
        for b in writes:
            if b in xr:
                continue
            if nowaw and not b.readers and b.writers:
                deps.extend(b.gen)
            else:
                g = list(b.writers) + list(b.readers)
                deps.extend(g)
                newgen.append((b, g))
        seen = set()
        for d in deps:
            if id(d) in seen:
                continue
            seen.add(id(d))
            if d.eng == "pe" and eng == "pe" and not d.is_dma and dbuf is None:
                continue
            if (not SAME_ENG_SYNC) and d.eng == eng and not d.is_dma and dbuf is None:
                continue
            d.signal = True
            op.deps.append(d)
        for b in reads:
            b.readers.append(op)
        ng = {id(b): g for b, g in newgen}
        for b in writes:
            if id(b) in ng:
                b.gen = ng[id(b)]
                b.writers = [op]
                b.readers = []
            else:
                b.writers.append(op)
        self.ops[eng].append(op)
        return op

    def mm(self, out, lhsT, rhs, start, stop, r, w):
        return self.add("pe", lambda e: e.matmul(out, lhsT, rhs, start=start, stop=stop), r, w, nowaw=not start)

    def tr(self, out, in_, ident, r, w, nowaw=True):
        return self.add("pe", lambda e: e.transpose(out, in_, ident), r, w, nowaw=nowaw)

    def act(self, out, in_, func, r, w, bias=None, scale=None, accum=None, nowaw=False):
        kw = {}
        if bias is not None:
            kw["bias"] = bias
        if scale is not None:
            kw["scale"] = scale
        if accum is not None:
            kw["accum_out"] = accum
        return self.add("act", lambda e: e.activation(out, in_, func, **kw), r, w, nowaw=nowaw)

    def tt(self, eng, out, in0, in1, op, r, w, nowaw=False):
        return self.add(eng, lambda e: e.tensor_tensor(out, in0, in1, op), r, w, nowaw=nowaw)

    def ts(self, eng, out, in0, s1, s2, op0, op1, r, w, nowaw=False):
        if s2 is None:
            return self.add(eng, lambda e: e.tensor_scalar(out, in0, s1, None, op0), r, w, nowaw=nowaw)
        return self.add(eng, lambda e: e.tensor_scalar(out, in0, s1, s2, op0, op1), r, w, nowaw=nowaw)

    def stt(self, eng, out, in0, scalar, in1, op0, op1, r, w, nowaw=False):
        return self.add(eng, lambda e: e.scalar_tensor_tensor(out, in0, scalar, in1, op0, op1), r, w, nowaw=nowaw)

    def cp(self, eng, out, in_, r, w, nowaw=False):
        if eng == "act":
            return self.add(eng, lambda e: e.copy(out, in_), r, w, nowaw=nowaw)
        return self.add(eng, lambda e: e.tensor_copy(out, in_), r, w, nowaw=nowaw)

    def memset(self, eng, ap, val, w, nowaw=False):
        return self.add(eng, lambda e: e.memset(ap, val), (), w, nowaw=nowaw)

    def red(self, eng, out, in_, op, r, w, nowaw=False):
        return self.add(eng, lambda e: e.tensor_reduce(out, in_, AX.X, op), r, w, nowaw=nowaw)

    def dma(self, q, out, in_, dbuf, r, w, nowaw=False, slow=False):
        if slow:
            return self.add(q, lambda e: e.dma_start(out=out, in_=in_, allow_slow_non_contiguous=True),
                            r, w, dbuf=dbuf, nowaw=nowaw)
        return self.add(q, lambda e: e.dma_start(out=out, in_=in_), r, w, dbuf=dbuf, nowaw=nowaw)

    def gather(self, out, in_, idx, elem_off, dbuf, r, w, nowaw=False, bound=None):
        if bound is not None:
            return self.add("pool", lambda e: e.indirect_dma_start(
                out=out, out_offset=None, in_=in_,
                in_offset=bass.IndirectOffsetOnAxis(ap=idx, axis=0), element_offset=elem_off,
                bounds_check=bound, oob_is_err=False),
                r, w, dbuf=dbuf, nowaw=nowaw)
        return self.add("pool", lambda e: e.indirect_dma_start(
            out=out, out_offset=None, in_=in_,
            in_offset=bass.IndirectOffsetOnAxis(ap=idx, axis=0), element_offset=elem_off),
            r, w, dbuf=dbuf, nowaw=nowaw)

    def scatter(self, out, idx, in_, dbuf, r, w):
        return self.add("pool", lambda e: e.indirect_dma_start(
            out=out, out_offset=bass.IndirectOffsetOnAxis(ap=idx, axis=0), in_=in_, in_offset=None),
            r, w, dbuf=dbuf)

    def end(self):
        nc = self.nc
        for e in ENGS:
            for op in self.ops[e]:
                if op.is_dma:
                    sw = (e == "pool")
                    k = (id(op.dbuf), sw)
                    if k not in self.dmap:
                        n = sum(1 for kk in self.dmap if kk[1] == sw)
                        assert n < len(self.dsem[sw]), "out of DMA semaphores"
                        self.dmap[k] = n
                    si = self.dmap[k]
                    self.dcount[sw][si] += 16
                    op.sem = self.dsem[sw][si]
                    op.count = self.dcount[sw][si]
                elif op.signal:
                    self.ccount[e] += 1
                    op.sem = self.csem[e]
                    op.count = self.ccount[e]
        final = [(self.dsem[sw][si], self.dcount[sw][si]) for (_, sw), si in self.dmap.items()]
        ops = self.ops
        waited = self.waited
        engmap = {"sp": "sync", "act": "scalar", "dve": "vector", "pool": "gpsimd", "pe": "tensor"}

        def run(ename, eng):
            wd = waited[ename]
            for op in ops[ename]:
                need = {}
                for d in op.deps:
                    k = id(d.sem)
                    if k not in need or need[k][1] < d.count:
                        need[k] = (d.sem, d.count)
                todo = []
                for k, (s, c) in need.items():
                    if wd.get(k, 0) < c:
                        todo.append((s, c))
                        wd[k] = c
                for s, c in todo[:-1]:
                    eng.wait_ge(s, c)
                ins = op.fn(eng)
                if todo:
                    ins._wait_ge(todo[-1][0], todo[-1][1])
                if op.signal:
                    ins.then_inc(op.sem, 16 if op.is_dma else 1)
            if ename == "sp":
                for s, c in final:
                    if wd.get(id(s), 0) < c:
                        eng.wait_ge(s, c)
                        wd[id(s)] = c

        with nc.Block() as block:
            for ename in ENGS:
                getattr(block, engmap[ename])(lambda eng, ename=ename: run(ename, eng))
        self.nstage += 1
        self.begin()


def rope_tables(NL, NCX):
    t = np.arange(NL)
    row = (t // 64).astype(np.float32)
    col = (t % 64).astype(np.float32)
    inv_freq = (np.float32(10000.0) ** (-np.arange(8, dtype=np.float32) / np.float32(8))).astype(np.float32)
    ang_r = row[:, None] * inv_freq
    ang_c = col[:, None] * inv_freq
    ang = np.concatenate([ang_r, ang_r, ang_c, ang_c], axis=-1).astype(np.float32)
    cos = np.cos(ang).astype(np.float32)
    sin = np.sin(ang).astype(np.float32)
    NT = NL + NCX
    cosT = np.zeros((128, NT), np.float32)
    sinT = np.zeros((128, NT), np.float32)
    cosT[64:96, :NL] = cos.T
    sinT[64:96, :NL] = sin.T
    cosT[64:96, NL:] = 1.0
    return cosT, sinT


def na_plan(R):
    def band(r):
        s = min(max(r - 4, 0), R - 8)
        return s
    variants = {}
    plan = []
    for j in range(R // 2):
        rows = (2 * j, 2 * j + 1)
        ms = set()
        for r in rows:
            s = band(r)
            for kr in range(s, s + 8):
                ms.add(kr // 2)
        lst = []
        for m in sorted(ms):
            key = []
            for qp in range(2):
                s = band(rows[qp])
                for kp in range(2):
                    kr = 2 * m + kp
                    key.append((s <= kr < s + 8, kr - rows[qp] + 7))
            key = tuple(key)
            if key not in variants:
                variants[key] = len(variants)
            lst.append((m, variants[key]))
        plan.append(lst)
    return plan, variants


def na_bias_tables(rpb, variants):
    L = rpb.shape[0]
    NV = len(variants)
    qc = np.arange(64)
    kc = np.arange(64)
    c0 = np.clip(qc - 8, 0, 48)
    in_win = (kc[:, None] >= c0[None, :]) & (kc[:, None] < c0[None, :] + 16)
    dc = np.clip(kc[:, None] - qc[None, :] + 15, 0, 30)
    out = np.full((L, 128, 4, NV, 128), NEG, np.float32)
    for key, v in variants.items():
        i = 0
        for qp in range(2):
            for kp in range(2):
                ok, dr = key[i]
                i += 1
                if not ok:
                    continue
                g = rpb[:, :, dr, :][:, :, dc]
                g = np.where(in_win[None, None], g, np.float32(NEG))
                out[:, kp * 64:(kp + 1) * 64, :, v, qp * 64:(qp + 1) * 64] = g.transpose(0, 2, 1, 3)
    return out.astype(ml_dtypes.bfloat16)


class Cfg:
    def __init__(self, NL=8192, NCX=256, L=4, debug=False, stop=None, part=None):
        self.part = part
        self.NL, self.NCX, self.L = NL, NCX, L
        self.NT = NL + NCX
        self.NTL = NL // 128
        self.NTC = NCX // 128
        self.NTT = self.NT // 128
        self.R = NL // 64
        self.debug = debug
        self.stop = stop
        nasg = 2 * self.NT
        self.NBLK = (nasg + NE * (BLK - 1)) // BLK
        self.plan, self.variants = na_plan(self.R)
        self.NV = len(self.variants)

    def groups(self, with_ctx=True):
        gs = []
        for g in range(self.NTL // 4):
            gs.append((list(range(4 * g, 4 * g + 4)), 0))
        if with_ctx:
            gs.append((list(range(self.NTL, self.NTL + self.NTC)), 1))
        return gs


def build(cfg):
    nc = bass.Bass("TRN2", target_bir_lowering=False)
    NL, NCX, NT, L = cfg.NL, cfg.NCX, cfg.NT, cfg.L
    NTT, NTL = cfg.NTT, cfg.NTL
    NV, NBLK = cfg.NV, cfg.NBLK

    def din(name, shape, dt=F32):
        return nc.dram_tensor(name, list(shape), dt, kind="ExternalInput").ap()

    skind = "ExternalOutput" if cfg.debug else "Internal"

    def dscr(name, shape, dt=F32):
        return nc.dram_tensor(name, list(shape), dt, kind=skind).ap()

    x_in = din("x", [NL, D])
    ctx_in = din("ctx", [NCX, D])
    cc_in = din("cc", [2, D])
    w_ada = din("w_ada", [L, D, 6 * D])
    b_ada = din("b_ada", [L, 6 * D])
    norm1_g = din("norm1_g", [L, D])
    norm2_g = din("norm2_g", [L, D])
    w_in = din("w_in", [L, D, IN_COLS])
    conv_w = din("conv_w", [L, 3, 256])
    sg_w = din("sg_w", [L, 4, 128, 128])
    sg_b = din("sg_b", [L, 4, 128])
    na_bias = din("na_bias", [L, 128, 4, NV, 128], BF16)
    q_norm_g = din("mla_q_norm_g", [L, 256])
    w_uq = din("mla_w_uq", [L, 256, 384])
    kv_norm_g = din("mla_kv_norm_g", [L, 128])
    w_ukv = din("mla_w_ukv", [L, 128, 512])
    out_norm_g = din("out_norm_g", [L, D])
    w_out = din("w_out", [L, D, D])
    w_grp = din("w_grp", [L, D, 4])
    b_grp = din("b_grp", [L, 4])
    w_exp = din("w_exp", [L, D, NE])
    b_exp = din("b_exp", [L, NE])
    tiny = cfg.stop is not None and cfg.stop[1] < 8 and cfg.stop[0] == 0
    w_gate = din("w_gate", [L * NE * D, DE] if not tiny else [128, DE])
    w_up = din("w_up", [L * NE * D, DE] if not tiny else [128, DE])
    w_down = din("w_down", [L * NE * DE, D] if not tiny else [128, D])
    final_g = din("final_norm_g", [1, D])
    cosT_in = din("cosT", [128, NT])
    sinT_in = din("sinT", [128, NT])
    consts_f = din("consts_f", [128, 128 + 128 + 32 + 1 + 64])
    consts_b = din("consts_b", [128, 128 + 128 + 128], BF16)

    out = nc.dram_tensor("out", [NL, D], F32, kind="ExternalOutput").ap()

    X = dscr("X", [NT, D])
    MOD = dscr("MOD", [2, 6 * D])
    UT = dscr("UT", [256, NT])
    BGT = dscr("BGT", [256, NT])
    Y = dscr("Y", [NT, D])
    QN_T = dscr("QN_T", [256, NT], BF16)
    KN_T = dscr("KN_T", [256, NT], BF16)
    VN = dscr("VN", [NT, 260], BF16)
    QM_T = dscr("QM_T", [4, 96, NT], BF16)
    KM_T = dscr("KM_T", [4, 96, NT], BF16)
    VM = dscr("VM", [NT, 260], BF16)
    H2 = dscr("H2", [NT, D], BF16)
    XBUF = dscr("XBUF", [NBLK * BLK, D], BF16)
    YBUF = dscr("YBUF", [NBLK * BLK, D])

    es = ExitStack()
    with es:
        P = Prog(nc, es)

        uid = [0]

        def sb(st, name, shape, dt=F32):
            uid[0] += 1
            return st.enter_context(nc.sbuf_tensor("%s_%d" % (name, uid[0]), list(shape), dt))

        def ps(st, name, shape, dt=F32):
            uid[0] += 1
            return st.enter_context(nc.psum_tensor("%s_%d" % (name, uid[0]), list(shape), dt))

        cf = sb(es, "cf", [128, 353])
        cb = sb(es, "cb", [128, 384], BF16)
        ident_f = cf[:, 0:128]
        ones_f = cf[:, 128:256]
        iota_e = cf[:, 256:288]
        iota_p = cf[:, 288:289]
        blkstart = cf[:, 289:353]
        ident_b = cb[:, 0:128]
        ones_b = cb[:, 128:256]
        triu_b = cb[:, 256:384]
        cboth = sb(es, "cboth", [128, KC, 2])
        mask1 = sb(es, "mask1", [128, NTT, NE])
        mask2 = sb(es, "mask2", [128, NTT, NE])
        rank12 = sb(es, "rank12", [128, 2, NTT])
        gw12 = sb(es, "gw12", [128, 2, NTT])
        dest_i = sb(es, "dest_i", [128, 2, NTT], I32)
        carry = sb(es, "carry", [128, NE])
        widx = sb(es, "widx", [128, 2, NBLK], I32)

        def s0():
            with ExitStack() as st:
                craw = sb(st, "craw", [128, KC, 2])
                b_cf, b_cb, b_craw, b_cboth = P.buf("cf"), P.buf("cb"), P.buf("craw"), P.buf("cboth")
                P.dma("sp", cf[:], consts_f, b_cf, (), (b_cf,))
                P.dma("sp", cb[:], consts_b, b_cb, (), (b_cb,))
                for v in range(2):
                    P.dma("sp", craw[:, :, v], cc_in[v].rearrange("(c p) -> p c", p=128), b_craw, (), (b_craw,),
                          nowaw=True, slow=True)
                P.act(cboth[:], craw[:], AF.Silu, (b_craw,), (b_cboth,))
                zt = sb(st, "zt", [128, 4 * D], BF16)
                b_zt = P.buf("zt")
                P.memset("dve", zt[:], 0.0, (b_zt,))
                for b in range(NBLK):
                    P.dma("sp" if b % 2 else "act",
                          XBUF[b * BLK:(b + 1) * BLK, :].rearrange("(p i) d -> p (i d)", p=128), zt[:], b_zt,
                          (b_zt,), ())
                P.end()

        def x_src(l, t):
            if l == 0:
                if t < NTL:
                    return x_in[t * 128:(t + 1) * 128, :]
                return ctx_in[(t - NTL) * 128:(t - NTL + 1) * 128, :]
            return X[t * 128:(t + 1) * 128, :]

        def s1(l):
            with ExitStack() as st:
                wa = sb(st, "wa", [128, 2, KC, 512])
                bada = sb(st, "bada", [2, 6 * D])
                g12 = sb(st, "g12", [2, 2 * D])
                modsb = sb(st, "modsb", [2, 6 * D])
                pm = [ps(st, "pm%d" % i, [128, 512]) for i in range(2)]
                b_wa = P.bufs("wa", 2)
                b_pm = P.bufs("pm", 2)
                b_bada, b_g12, b_mod = P.buf("bada"), P.buf("g12"), P.buf("mod")
                for v in range(2):
                    P.dma("act", bada[v:v + 1, :], b_ada[l:l + 1, :], b_bada, (), (b_bada,), nowaw=True)
                    P.dma("act", g12[v:v + 1, 0:D], norm1_g[l:l + 1, :], b_g12, (), (b_g12,), nowaw=True)
                    P.dma("act", g12[v:v + 1, D:2 * D], norm2_g[l:l + 1, :], b_g12, (), (b_g12,), nowaw=True)
                for j in range(12):
                    s = j % 2
                    P.dma("sp", wa[:, s], w_ada[l][:, j * 512:(j + 1) * 512].rearrange("(c p) n -> p c n", p=128),
                          b_wa[s], (), (b_wa[s],))
                    for c in range(KC):
                        P.mm(pm[s][0:2, :], cboth[:, c, :], wa[:, s, c, :], c == 0, c == KC - 1,
                             (b_wa[s],), (b_pm[s],))
                    P.tt("dve", modsb[:, j * 512:(j + 1) * 512], pm[s][0:2, :], bada[:, j * 512:(j + 1) * 512],
                         ALU.add, (b_pm[s], b_bada), (b_mod,), nowaw=True)
                for k, go in ((1, 0), (4, D)):
                    P.stt("dve", modsb[:, k * D:(k + 1) * D], modsb[:, k * D:(k + 1) * D], 1.0,
                          g12[:, go:go + D], ALU.add, ALU.mult, (b_mod, b_g12), (b_mod,))
                P.dma("sp", MOD, modsb[:], b_mod, (b_mod,), ())
                P.end()

        def load_bc(q, tile_v, k, b):
            for v in range(2):
                P.dma(q, tile_v[:, v, :], MOD[v:v + 1, k * D:(k + 1) * D].partition_broadcast(128), b, (), (b,),
                      nowaw=True)

        def rms_rstd(ssq, rstd, n, r, w):
            P.ts("dve", rstd, ssq, 1.0 / n, EPS, ALU.mult, ALU.add, r, w)
            P.act(rstd, rstd, AF.Sqrt, w, w)
            P.add("dve", lambda e: e.reciprocal(rstd, rstd), w, w)

        def s2(l, last):
            with ExitStack() as st:
                win = sb(st, "win", [128, KC, IN_COLS], BF16)
                wkr = sb(st, "wkr", [128, KC, 2, 96], BF16)
                wuq = sb(st, "wuq", [128, 2, 384], BF16)
                wuqrot = sb(st, "wuqrot", [128, 2, 4, 96], BF16)
                wukv = sb(st, "wukv", [128, 512], BF16)
                wukv_v = sb(st, "wukv_v", [128, 256], BF16)
                sgw32 = sb(st, "sgw32", [128, 4, 128])
                sgwb = sb(st, "sgwb", [128, 4, 128], BF16)
                sgwT = sb(st, "sgwT", [128, 4, 128], BF16)
                sgb = sb(st, "sgb", [128, 4])
                qkvg = sb(st, "qkvg", [128, 3])
                gm_bc = sb(st, "gm_bc", [128, 2, D])
                sh_bc = sb(st, "sh_bc", [128, 2, D])
                cos_t = sb(st, "cos_t", [128, 2, 512])
                sin_t = sb(st, "sin_t", [128, 2, 512])
                xt = sb(st, "xt", [128, 2, D])
                junk = sb(st, "junk", [128, D], BF16)
                xn = sb(st, "xn", [128, 2, D])
                hb = sb(st, "hb", [128, 2, D], BF16)
                hT = sb(st, "hT", [128, 2, KC, 512], BF16)
                stat = sb(st, "stat", [128, 2, 8])
                cg_sb = sb(st, "cg_sb", [128, 2, 512])
                u_sb = sb(st, "u_sb", [128, 2, 512])
                bg_sb = sb(st, "bg_sb", [128, 2, 512])
                qk_sb = sb(st, "qk_sb", [128, 4, 512], BF16)
                vaug = sb(st, "vaug", [128, 2, 4, 65], BF16)
                vaug2 = sb(st, "vaug2", [128, 2, 4, 65], BF16)
                zb = sb(st, "zb", [128, 512])
                gt1 = sb(st, "gt1", [128, 512])
                gt2 = sb(st, "gt2", [128, 512])
                gg = sb(st, "gg", [128, 512])
                vn = sb(st, "vn", [128, 256], BF16)
                yb = sb(st, "yb", [128, 2, 256])
                cq_b = sb(st, "cq_b", [128, 2, 384], BF16)
                cqnT = sb(st, "cqnT", [128, 3, 512], BF16)
                rt1 = sb(st, "rt1", [128, 512])
                rt2 = sb(st, "rt2", [128, 512])
                qT = sb(st, "qT", [128, 4, 512], BF16)
                kn_sb = sb(st, "kn_sb", [128, 4, 512], BF16)
                kr_sb = sb(st, "kr_sb", [128, 512], BF16)
                pA = [ps(st, "pA%d" % i, [128, 1024], BF16) for i in range(4)]
                pB = [ps(st, "pB%d" % i, [128, 512]) for i in range(4)]
                b_pA = P.bufs("pA", 4)
                b_pB = P.bufs("pB", 4)
                b_w = P.buf("w")
                b_xt = P.bufs("xt", 2)
                b_stat = P.bufs("stat", 2)
                b_junk, b_xn = P.buf("junk"), P.bufs("xn", 2)
                b_hb = P.bufs("hb", 2)
                b_hT = P.bufs("hT", 2)
                b_cs = P.bufs("cs", 2)
                b_cg, b_u, b_bg, b_qk = P.bufs("cg", 2), P.bufs("u", 2), P.bufs("bg", 2), P.bufs("qk", 4)
                b_va, b_va2 = P.bufs("va", 2), P.bufs("va2", 2)
                b_zb, b_gt1, b_gt2, b_gg, b_vn = P.buf("zb"), P.buf("gt1"), P.buf("gt2"), P.buf("gg"), P.buf("vn")
                b_yb = P.bufs("yb", 2)
                b_cqb = P.bufs("cqb", 2)
                b_cqnT = P.buf("cqnT")
                b_rt1, b_rt2 = P.buf("rt1"), P.buf("rt2")
                b_qT = P.bufs("qT", 4)
                b_kn = P.bufs("kn", 4)
                b_kr = P.buf("kr")

                for c in range(KC):
                    for h2 in range(2):
                        P.dma("pool", win[:, c, h2 * 1232:(h2 + 1) * 1232],
                              w_in[l][c * 128:(c + 1) * 128, h2 * 1232:(h2 + 1) * 1232], b_w, (), (b_w,), nowaw=True)
                for c in range(2):
                    P.dma("pool", wuq[:, c, :], w_uq[l][c * 128:(c + 1) * 128, :], b_w, (), (b_w,), nowaw=True)
                P.dma("pool", wukv[:], w_ukv[l], b_w, (), (b_w,), nowaw=True)
                P.dma("act", sgw32[:], sg_w[l].rearrange("h p q -> p h q"), b_w, (), (b_w,), nowaw=True)
                P.dma("act", sgb[:], sg_b[l].rearrange("h p -> p h"), b_w, (), (b_w,), nowaw=True, slow=True)
                P.dma("act", qkvg[:, 0:2], q_norm_g[l].rearrange("(c p) -> p c", p=128), b_w, (), (b_w,),
                      nowaw=True, slow=True)
                P.dma("act", qkvg[:, 2:3], kv_norm_g[l].rearrange("(c p) -> p c", p=128), b_w, (), (b_w,),
                      nowaw=True, slow=True)
                load_bc("act", gm_bc, 1, b_w)
                load_bc("act", sh_bc, 0, b_w)
                b_wd = P.buf("wd")
                P.memset("pool", wkr[:], 0.0, (b_wd,))
                P.memset("pool", wuqrot[:], 0.0, (b_wd,))
                KR0 = MLA0 + 384
                P.cp("dve", wkr[:, :, 0, 64:96], win[:, :, KR0:KR0 + 32], (b_w,), (b_wd,))
                for (dst, src, sgn) in ((0, 8, -1.0), (8, 0, 1.0), (16, 24, -1.0), (24, 16, 1.0)):
                    P.ts("dve", wkr[:, :, 1, 64 + dst:72 + dst], win[:, :, KR0 + src:KR0 + src + 8], sgn, None,
                         ALU.mult, None, (b_w,), (b_wd,))
                    for h in range(4):
                        P.ts("dve", wuqrot[:, :, h, 64 + dst:72 + dst],
                             wuq[:, :, h * 96 + 64 + src:h * 96 + 72 + src], sgn, None, ALU.mult, None,
                             (b_w,), (b_wd,))
                for h in range(4):
                    P.cp("dve", wukv_v[:, h * 64:(h + 1) * 64], wukv[:, h * 128 + 64:(h + 1) * 128], (b_w,), (b_wd,))
                P.cp("dve", sgwb[:], sgw32[:], (b_w,), (b_wd,))
                for h in range(4):
                    P.tr(pA[0][:, h * 128:(h + 1) * 128], sgwb[:, h, :], ident_b, (b_wd,), (b_pA[0],), nowaw=(h > 0))
                P.cp("dve", sgwT[:].rearrange("p h q -> p (h q)"), pA[0][:, 0:512], (b_pA[0],), (b_wd,))
                for s in range(2):
                    P.memset("pool", vaug[:, s, :, 64:65], 1.0, (b_va[s],))
                    P.memset("pool", vaug2[:, s, :, 64:65], 1.0, (b_va2[s],))

                pbi = [0]

                def nextpb():
                    i = pbi[0] % 4
                    pbi[0] += 1
                    return i

                evi = [0]

                def evac_eng():
                    evi[0] += 1
                    return "act" if evi[0] % 2 else "dve"

                groups = cfg.groups(True)
                def front(gi, tiles, v):
                    nt = len(tiles)
                    N = 128 * nt
                    t0 = tiles[0] * 128
                    gs = gi % 2
                    P.dma("sp", cos_t[:, gs, 0:N], cosT_in[:, t0:t0 + N], b_cs[gs], (), (b_cs[gs],), nowaw=False)
                    P.dma("sp", sin_t[:, gs, 0:N], sinT_in[:, t0:t0 + N], b_cs[gs], (), (b_cs[gs],), nowaw=True)
                    for i, t in enumerate(tiles):
                        s = (gi * 4 + i) % 2
                        P.dma("sp", xt[:, s, :], x_src(l, t), b_xt[s], (), (b_xt[s],))
                        P.act(junk[:], xt[:, s, :], AF.Square, (b_xt[s],), (b_junk, b_stat[s]), accum=stat[:, s, 0:1])
                        rms_rstd(stat[:, s, 0:1], stat[:, s, 1:2], D, (b_stat[s],), (b_stat[s],))
                        P.stt("dve", xn[:, s, :], xt[:, s, :], stat[:, s, 1:2], gm_bc[:, v, :], ALU.mult, ALU.mult,
                              (b_xt[s], b_stat[s], b_w), (b_xn[s],))
                        P.tt("pool", hb[:, s, :], xn[:, s, :], sh_bc[:, v, :], ALU.add, (b_xn[s], b_w), (b_hb[s],))
                        for c in range(KC):
                            P.tr(pA[c // 2][:, (c % 2) * 512 + i * 128:(c % 2) * 512 + (i + 1) * 128],
                                 hb[:, s, c * 128:(c + 1) * 128], ident_b, (b_hb[s],), (b_pA[c // 2],),
                                 nowaw=not (i == 0 and c % 2 == 0))
                    for c in range(KC):
                        P.cp("act" if (c // 2) % 2 == 0 else "dve", hT[:, gs, c, 0:N],
                             pA[c // 2][:, (c % 2) * 512:(c % 2) * 512 + N],
                             (b_pA[c // 2],), (b_hT[gs],), nowaw=(c > 0))


                def back(gi, tiles, v):
                    nt = len(tiles)
                    N = 128 * nt
                    t0 = tiles[0] * 128
                    gs = gi % 2
                    def fm_block(col0, width=128):
                        pi = nextpb()
                        for c in range(KC):
                            P.mm(pB[pi][0:width, 0:N], win[:, c, col0:col0 + width], hT[:, gs, c, 0:N],
                                 c == 0, c == KC - 1, (b_w, b_hT[gs]), (b_pB[pi],))
                        return pi

                    for blk in range(2):
                        pi = fm_block(blk * 128)
                        P.cp("act", bg_sb[:, blk, 0:N], pB[pi][:, 0:N], (b_pB[pi],), (b_bg[blk],))
                        P.dma("sp", BGT[blk * 128:(blk + 1) * 128, t0:t0 + N], bg_sb[:, blk, 0:N], b_bg[blk],
                              (b_bg[blk],), ())
                    for blk in range(2):
                        pi = fm_block(256 + blk * 128)
                        P.cp("act", cg_sb[:, blk, 0:N], pB[pi][:, 0:N], (b_pB[pi],), (b_cg[blk],))
                    for blk in range(2):
                        pi = fm_block(512 + blk * 128)
                        P.tt("dve", u_sb[:, blk, 0:N], pB[pi][:, 0:N], cg_sb[:, blk, 0:N], ALU.mult,
                             (b_pB[pi], b_cg[blk]), (b_u[blk],))
                        P.dma("sp", UT[blk * 128:(blk + 1) * 128, t0:t0 + N], u_sb[:, blk, 0:N], b_u[blk],
                              (b_u[blk],), ())
                    for blk in range(2):
                        pi = fm_block(NA0 + blk * 128)
                        P.act(qk_sb[:, blk, 0:N], pB[pi][:, 0:N], AF.Copy, (b_pB[pi],), (b_qk[blk],), scale=0.125)
                        P.dma("sp", QN_T[blk * 128:(blk + 1) * 128, t0:t0 + N], qk_sb[:, blk, 0:N], b_qk[blk],
                              (b_qk[blk],), ())
                    for blk in range(2):
                        pi = fm_block(NA0 + 256 + blk * 128)
                        P.cp("act", qk_sb[:, 2 + blk, 0:N], pB[pi][:, 0:N], (b_pB[pi],), (b_qk[2 + blk],))
                        P.dma("sp", KN_T[blk * 128:(blk + 1) * 128, t0:t0 + N], qk_sb[:, 2 + blk, 0:N], b_qk[2 + blk],
                              (b_qk[2 + blk],), ())
                    pr = []
                    for j in range(2):
                        pi = nextpb()
                        for c in range(KC):
                            P.mm(pB[pi][0:96, 0:N], wkr[:, c, j, :], hT[:, gs, c, 0:N], c == 0, c == KC - 1,
                                 (b_wd, b_hT[gs]), (b_pB[pi],))
                        pr.append(pi)
                    P.tt("dve", rt1[64:96, 0:N], pB[pr[0]][64:96, 0:N], cos_t[64:96, gs, 0:N], ALU.mult,
                         (b_pB[pr[0]], b_cs[gs]), (b_rt1,))
                    P.tt("dve", rt2[64:96, 0:N], pB[pr[1]][64:96, 0:N], sin_t[64:96, gs, 0:N], ALU.mult,
                         (b_pB[pr[1]], b_cs[gs]), (b_rt2,))
                    P.tt("pool", kr_sb[64:96, 0:N], rt1[64:96, 0:N], rt2[64:96, 0:N], ALU.add,
                         (b_rt1, b_rt2), (b_kr,))
                    for h in range(4):
                        P.dma("sp", KM_T[h, 64:96, t0:t0 + N], kr_sb[64:96, 0:N], b_kr, (b_kr,), ())

                    for i, t in enumerate(tiles):
                        s = (gi * 4 + i) % 2
                        tok = slice(i * 128, (i + 1) * 128)
                        pi = nextpb()
                        for c in range(KC):
                            P.mm(pB[pi][:, 0:512], hT[:, gs, c, tok], win[:, c, SG0:SG0 + 512], c == 0, c == KC - 1,
                                 (b_w, b_hT[gs]), (b_pB[pi],))
                        P.cp("act", zb[:], pB[pi][:, 0:512], (b_pB[pi],), (b_zb,))
                        P.tt("dve", gt1[:], zb[:], zb[:], ALU.mult, (b_zb,), (b_gt1,))
                        P.ts("dve", gt1[:], gt1[:], 0.044715, 1.0, ALU.mult, ALU.add, (b_gt1,), (b_gt1,))
                        P.tt("dve", gt1[:], gt1[:], zb[:], ALU.mult, (b_gt1, b_zb), (b_gt1,))
                        P.act(gt2[:], gt1[:], AF.Sigmoid, (b_gt1,), (b_gt2,), scale=1.5957691216057308)
                        P.tt("dve", gg[:], gt2[:], zb[:], ALU.mult, (b_gt2, b_zb), (b_gg,))
                        P.red("dve", stat[:, s, 2:3], gg[:, 256:512], ALU.add, (b_gg,), (b_stat[s],))
                        P.act(junk[:, 0:256], gg[:, 256:512], AF.Square, (b_gg,), (b_junk, b_stat[s]),
                              accum=stat[:, s, 3:4])
                        P.ts("dve", stat[:, s, 2:3], stat[:, s, 2:3], 1.0 / 256, None, ALU.mult, None,
                             (b_stat[s],), (b_stat[s],))
                        P.tt("dve", stat[:, s, 4:5], stat[:, s, 2:3], stat[:, s, 2:3], ALU.mult,
                             (b_stat[s],), (b_stat[s],))
                        P.stt("dve", stat[:, s, 3:4], stat[:, s, 3:4], 1.0 / 256, stat[:, s, 4:5],
                              ALU.mult, ALU.subtract, (b_stat[s],), (b_stat[s],))
                        P.ts("dve", stat[:, s, 3:4], stat[:, s, 3:4], EPS, None, ALU.add, None,
                             (b_stat[s],), (b_stat[s],))
                        P.act(stat[:, s, 3:4], stat[:, s, 3:4], AF.Sqrt, (b_stat[s],), (b_stat[s],))
                        P.add("dve", lambda e, s=s: e.reciprocal(stat[:, s, 3:4], stat[:, s, 3:4]),
                              (b_stat[s],), (b_stat[s],))
                        P.ts("dve", vn[:], gg[:, 256:512], stat[:, s, 2:3], stat[:, s, 3:4], ALU.subtract, ALU.mult,
                             (b_gg, b_stat[s]), (b_vn,))
                        pj = nextpb()
                        for h in range(4):
                            P.mm(pB[pj][:, h * 64:(h + 1) * 64], sgwT[:, h, :], vn[:, h * 64:(h + 1) * 64],
                                 True, True, (b_wd, b_vn), (b_pB[pj],))
                        for h in range(4):
                            P.stt("dve", yb[:, s, h * 64:(h + 1) * 64], pB[pj][:, h * 64:(h + 1) * 64],
                                  sgb[:, h:h + 1], gg[:, h * 64:(h + 1) * 64], ALU.add, ALU.mult,
                                  (b_pB[pj], b_gg, b_w), (b_yb[s],), nowaw=(h > 0))
                        P.dma("sp", Y[t * 128:(t + 1) * 128, 256:512], yb[:, s, :], b_yb[s], (b_yb[s],), ())
                        pi = nextpb()
                        for c in range(KC):
                            P.mm(pB[pi][:, 0:256], hT[:, gs, c, tok], win[:, c, NA0 + 512:NA0 + 768], c == 0,
                                 c == KC - 1, (b_w, b_hT[gs]), (b_pB[pi],))
                        P.cp("act", vaug[:, s, :, 0:64], pB[pi][:, 0:256].rearrange("p (h d) -> p h d", h=4),
                             (b_pB[pi],), (b_va[s],))
                        P.dma("sp", VN[t * 128:(t + 1) * 128, :], vaug[:, s].rearrange("p h d -> p (h d)"),
                              b_va[s], (b_va[s],), ())
                        pi = nextpb()
                        for c in range(KC):
                            P.mm(pB[pi][:, 0:384], hT[:, gs, c, tok], win[:, c, MLA0:MLA0 + 384], c == 0,
                                 c == KC - 1, (b_w, b_hT[gs]), (b_pB[pi],))
                        P.act(junk[:, 0:256], pB[pi][:, 0:256], AF.Square, (b_pB[pi],), (b_junk, b_stat[s]),
                              accum=stat[:, s, 5:6])
                        P.act(junk[:, 256:384], pB[pi][:, 256:384], AF.Square, (b_pB[pi],), (b_junk, b_stat[s]),
                              accum=stat[:, s, 6:7])
                        rms_rstd(stat[:, s, 5:6], stat[:, s, 5:6], 256, (b_stat[s],), (b_stat[s],))
                        rms_rstd(stat[:, s, 6:7], stat[:, s, 6:7], 128, (b_stat[s],), (b_stat[s],))
                        P.act(cq_b[:, s, 0:256], pB[pi][:, 0:256], AF.Copy, (b_pB[pi], b_stat[s]), (b_cqb[s],),
                              scale=stat[:, s, 5:6])
                        P.act(cq_b[:, s, 256:384], pB[pi][:, 256:384], AF.Copy, (b_pB[pi], b_stat[s]), (b_cqb[s],),
                              scale=stat[:, s, 6:7], nowaw=True)
                        for b3 in range(3):
                            P.tr(pA[b3][:, i * 128:(i + 1) * 128], cq_b[:, s, b3 * 128:(b3 + 1) * 128], ident_b,
                                 (b_cqb[s],), (b_pA[b3],), nowaw=(i > 0))
                    for b3 in range(3):
                        P.ts("dve", cqnT[:, b3, 0:N], pA[b3][:, 0:N], qkvg[:, b3:b3 + 1], None, ALU.mult, None,
                             (b_pA[b3], b_w), (b_cqnT,), nowaw=(b3 > 0))
                    for h in range(4):
                        hs = h
                        p1 = nextpb()
                        for c in range(2):
                            P.mm(pB[p1][0:96, 0:N], wuq[:, c, h * 96:(h + 1) * 96], cqnT[:, c, 0:N], c == 0, c == 1,
                                 (b_w, b_cqnT), (b_pB[p1],))
                        p2 = nextpb()
                        for c in range(2):
                            P.mm(pB[p2][0:96, 0:N], wuqrot[:, c, h, :], cqnT[:, c, 0:N], c == 0, c == 1,
                                 (b_wd, b_cqnT), (b_pB[p2],))
                        P.cp("act", qT[0:64, hs, 0:N], pB[p1][0:64, 0:N], (b_pB[p1],), (b_qT[hs],))
                        P.tt("dve", rt1[64:96, 0:N], pB[p1][64:96, 0:N], cos_t[64:96, gs, 0:N], ALU.mult,
                             (b_pB[p1], b_cs[gs]), (b_rt1,))
                        P.tt("dve", rt2[64:96, 0:N], pB[p2][64:96, 0:N], sin_t[64:96, gs, 0:N], ALU.mult,
                             (b_pB[p2], b_cs[gs]), (b_rt2,))
                        P.tt("pool", qT[64:96, hs, 0:N], rt1[64:96, 0:N], rt2[64:96, 0:N], ALU.add,
                             (b_rt1, b_rt2), (b_qT[hs],), nowaw=True)
                        P.dma("sp", QM_T[h, :, t0:t0 + N], qT[0:96, hs, 0:N], b_qT[hs], (b_qT[hs],), ())
                    for h in range(4):
                        hs = h
                        pi = nextpb()
                        P.mm(pB[pi][0:64, 0:N], wukv[:, h * 128:h * 128 + 64], cqnT[:, 2, 0:N], True, True,
                             (b_w, b_cqnT), (b_pB[pi],))
                        P.cp("act", kn_sb[0:64, hs, 0:N], pB[pi][0:64, 0:N], (b_pB[pi],), (b_kn[hs],))
                        P.dma("sp", KM_T[h, 0:64, t0:t0 + N], kn_sb[0:64, hs, 0:N], b_kn[hs], (b_kn[hs],), ())
                    for i, t in enumerate(tiles):
                        s = (gi * 4 + i) % 2
                        pi = nextpb()
                        P.mm(pB[pi][:, 0:256], cqnT[:, 2, i * 128:(i + 1) * 128], wukv_v[:], True, True,
                             (b_wd, b_cqnT), (b_pB[pi],))
                        P.cp("act", vaug2[:, s, :, 0:64], pB[pi][:, 0:256].rearrange("p (h d) -> p h d", h=4),
                             (b_pB[pi],), (b_va2[s],))
                        P.dma("sp", VM[t * 128:(t + 1) * 128, :], vaug2[:, s].rearrange("p h d -> p (h d)"),
                              b_va2[s], (b_va2[s],), ())
                if groups:
                    front(0, *groups[0])
                for gi, (tiles, v) in enumerate(groups):
                    if gi + 1 < len(groups):
                        front(gi + 1, *groups[gi + 1])
                    back(gi, tiles, v)
                P.end()
        def s3(l, last):
            with ExitStack() as st:
                ut = sb(st, "ut", [128, 2, 2, 514])
                bgt = sb(st, "bgt", [128, 2, 2, 512])
                cw = sb(st, "cw", [128, 2, 3])
                acc = sb(st, "acc", [128, 2, 512])
                ya = sb(st, "ya", [128, 2, 512])
                yat = sb(st, "yat", [128, 2, 256])
                pF = [ps(st, "pF%d" % i, [128, 512]) for i in range(2)]
                b_ut, b_bgt = P.bufs("ut", 2), P.bufs("bgt", 2)
                b_cw, b_acc, b_ya = P.buf("cw"), P.bufs("acc", 2), P.bufs("ya", 2)
                b_yat, b_pF = P.bufs("yat", 2), P.bufs("pF", 2)
                for blk in range(2):
                    for k3 in range(3):
                        P.dma("act", cw[:, blk, k3:k3 + 1],
                              conv_w[l][k3, blk * 128:(blk + 1) * 128].rearrange("(p o) -> p o", o=1),
                              b_cw, (), (b_cw,), nowaw=True, slow=True)
                cnt = 0
                for gi, (tiles, v) in enumerate(cfg.groups(not last)):
                    nt = len(tiles)
                    N = 128 * nt
                    t0 = tiles[0] * 128
                    s0_, s1_ = (0, NL) if v == 0 else (NL, NT)
                    gs = gi % 2
                    lo = max(t0 - 1, s0_)
                    hi = min(t0 + N + 1, s1_)
                    off = lo - (t0 - 1)
                    if t0 - 1 < s0_:
                        P.memset("pool", ut[:, gs, :, 0:1], 0.0, (b_ut[gs],))
                    if t0 + N + 1 > s1_:
                        P.memset("pool", ut[:, gs, :, N + 1:N + 2], 0.0, (b_ut[gs],), nowaw=True)
                    for blk in range(2):
                        P.dma("sp", ut[:, gs, blk, off:off + hi - lo], UT[blk * 128:(blk + 1) * 128, lo:hi],
                              b_ut[gs], (), (b_ut[gs],), nowaw=True)
                        P.dma("sp", bgt[:, gs, blk, 0:N], BGT[blk * 128:(blk + 1) * 128, t0:t0 + N],
                              b_bgt[gs], (), (b_bgt[gs],), nowaw=True)
                    for blk in range(2):
                        P.ts("dve", acc[:, blk, 0:N], ut[:, gs, blk, 0:N], cw[:, blk, 0:1], None, ALU.mult, None,
                             (b_ut[gs], b_cw), (b_acc[blk],))
                        P.stt("dve", acc[:, blk, 0:N], ut[:, gs, blk, 1:N + 1], cw[:, blk, 1:2], acc[:, blk, 0:N],
                              ALU.mult, ALU.add, (b_ut[gs], b_cw, b_acc[blk]), (b_acc[blk],))
                        P.stt("dve", acc[:, blk, 0:N], ut[:, gs, blk, 2:N + 2], cw[:, blk, 2:3], acc[:, blk, 0:N],
                              ALU.mult, ALU.add, (b_ut[gs], b_cw, b_acc[blk]), (b_acc[blk],))
                        P.tt("pool", ya[:, blk, 0:N], acc[:, blk, 0:N], bgt[:, gs, blk, 0:N], ALU.mult,
                             (b_acc[blk], b_bgt[gs]), (b_ya[blk],))
                    for i, t in enumerate(tiles):
                        s = cnt % 2
                        cnt += 1
                        for blk in range(2):
                            P.tr(pF[s][:, blk * 128:(blk + 1) * 128], ya[:, blk, i * 128:(i + 1) * 128], ident_f,
                                 (b_ya[blk],), (b_pF[s],), nowaw=(blk > 0))
                        P.cp("act", yat[:, s, :], pF[s][:, 0:256], (b_pF[s],), (b_yat[s],))
                        P.dma("sp", Y[t * 128:(t + 1) * 128, 0:256], yat[:, s, :], b_yat[s], (b_yat[s],), ())
                P.end()

        def s4(l, last):
            with ExitStack() as st:
                kn = sb(st, "kn", [128, 2, NT], BF16)
                vns = sb(st, "vns", [128, NTT, 260], BF16)
                bias = sb(st, "bias", [128, 4, NV, 128], BF16)
                qt = sb(st, "qt", [128, 2, 2, 128], BF16)
                pp = sb(st, "pp", [128, 2, 8, 128], BF16)
                yc = sb(st, "yc", [128, 2, 256])
                rec = sb(st, "rec", [128, 2, 4])
                pS = [ps(st, "pS%d" % i, [128, 2, 512]) for i in range(2)]
                pO = [ps(st, "pO%d" % i, [128, 512]) for i in range(2)]
                b_kv, b_bias = P.buf("kv"), P.buf("bias")
                b_qt, b_pp, b_yc, b_rec = P.bufs("qt", 2), P.bufs("pp", 2), P.bufs("yc", 2), P.bufs("rec", 2)
                b_pS, b_pO = P.bufs("pS", 2), P.bufs("pO", 2)
                for blk in range(2):
                    for h0 in range(0, NT, 2048):
                        h1 = min(NT, h0 + 2048)
                        P.dma("sp", kn[:, blk, h0:h1], KN_T[blk * 128:(blk + 1) * 128, h0:h1], b_kv, (), (b_kv,),
                              nowaw=True)
                for t0_ in range(0, NTT, 8):
                    t1_ = min(NTT, t0_ + 8)
                    P.dma("act", vns[:, t0_:t1_, :], VN[t0_ * 128:t1_ * 128, :].rearrange("(t p) f -> p t f", p=128),
                          b_kv, (), (b_kv,), nowaw=True)
                P.dma("act", bias[:].rearrange("p h v q -> p (h v q)"),
                      na_bias[l].rearrange("p h v q -> p (h v q)"), b_bias, (), (b_bias,))
                tiles = list(range(NTL)) + ([] if last else list(range(NTL, NTT)))
                ctx_chunks = [(m, None) for m in range(NTL, NTT)]
                cnt = 0
                for ti, t in enumerate(tiles):
                    chunks = (cfg.plan[t] + ctx_chunks) if t < NTL else ctx_chunks
                    nch = len(chunks)
                    s = ti % 2
                    P.dma("sp", qt[:, s], QN_T[:, t * 128:(t + 1) * 128].rearrange("(b p) q -> p b q", p=128),
                          b_qt[s], (), (b_qt[s],))
                    for h in range(4):
                        u = cnt % 2
                        cnt += 1
                        hb_, base = h // 2, 64 * (h % 2)
                        for i, (m, var) in enumerate(chunks):
                            o = pS[u][:, i // 4, (i % 4) * 128:(i % 4 + 1) * 128]
                            P.mm(o, kn[base:base + 64, hb_, m * 128:(m + 1) * 128], qt[base:base + 64, s, hb_, :],
                                 True, var is None, (b_kv, b_qt[s]), (b_pS[u],))
                            if var is not None:
                                P.mm(o, ident_b, bias[:, h, var, :], False, True, (b_bias,), (b_pS[u],))
                        P.act(pp[:, u, 0:nch, :], pS[u][:].rearrange("p a (b q) -> p (a b) q", q=128)[:, 0:nch, :],
                              AF.Exp, (b_pS[u],), (b_pp[u],))
                        for i, (m, var) in enumerate(chunks):
                            P.mm(pO[s][:, h * 65:(h + 1) * 65], pp[:, u, i, :], vns[:, m, h * 65:(h + 1) * 65],
                                 i == 0, i == nch - 1, (b_pp[u], b_kv), (b_pO[s],))
                    cnt = cnt
                    o4 = pO[s][:, 0:260].rearrange("p (h d) -> p h d", h=4)
                    P.add("dve", lambda e, o4=o4, s=s: e.reciprocal(rec[:, s, :], o4[:, :, 64]),
                          (b_pO[s],), (b_rec[s],))
                    for h in range(4):
                        P.ts("dve", yc[:, s, h * 64:(h + 1) * 64], pO[s][:, h * 65:h * 65 + 64], rec[:, s, h:h + 1],
                             None, ALU.mult, None, (b_pO[s], b_rec[s]), (b_yc[s],), nowaw=(h > 0))
                    P.dma("sp", Y[t * 128:(t + 1) * 128, 512:768], yc[:, s, :], b_yc[s], (b_yc[s],), ())
                P.end()

        def s5(l, last):
            with ExitStack() as st:
                km = sb(st, "km", [128, 4, NT], BF16)
                vms = sb(st, "vms", [128, NTT, 260], BF16)
                qm = sb(st, "qm", [128, 2, 4, 512], BF16)
                pp = sb(st, "pp", [128, 4, 512], BF16)
                yd = sb(st, "yd", [128, 2, 4, 256])
                rec = sb(st, "rec", [128, 2, 4])
                NS = 5
                pS = [ps(st, "pS%d" % i, [128, 512]) for i in range(NS)]
                pO = [ps(st, "pO%d" % i, [128, 512]) for i in range(2)]
                b_kv = P.buf("kv")
                b_qm, b_pp, b_yd, b_rec = P.bufs("qm", 2), P.bufs("pp", 4), P.bufs("yd", 2), P.bufs("rec", 2)
                b_pS, b_pO = P.bufs("pS", NS), P.bufs("pO", 2)
                for h in range(4):
                    for h0 in range(0, NT, 2048):
                        h1 = min(NT, h0 + 2048)
                        P.dma("sp", km[0:96, h, h0:h1], KM_T[h, :, h0:h1], b_kv, (), (b_kv,), nowaw=True)
                for t0_ in range(0, NTT, 8):
                    t1_ = min(NTT, t0_ + 8)
                    P.dma("act", vms[:, t0_:t1_, :], VM[t0_ * 128:t1_ * 128, :].rearrange("(t p) f -> p t f", p=128),
                          b_kv, (), (b_kv,), nowaw=True)
                all_chunks = list(range(NTT))
                ctx_only = list(range(NTL, NTT))
                cnt = 0
                si = 0
                for gi, (tiles, v) in enumerate(cfg.groups(not last)):
                    nt = len(tiles)
                    N = 128 * nt
                    t0 = tiles[0] * 128
                    gs = gi % 2
                    chunks = all_chunks if v == 0 else ctx_only
                    nch = len(chunks)
                    for h in range(4):
                        P.dma("sp", qm[0:96, gs, h, 0:N], QM_T[h, :, t0:t0 + N], b_qm[gs], (), (b_qm[gs],), nowaw=True)
                    for h in range(4):
                        u = cnt % 2
                        cnt += 1
                        pend = []

                        def qk(ci):
                            nonlocal si
                            m = chunks[ci]
                            k = si % NS
                            si += 1
                            P.mm(pS[k][:, 0:N], km[0:96, h, m * 128:(m + 1) * 128], qm[0:96, gs, h, 0:N], True, True,
                                 (b_kv, b_qm[gs]), (b_pS[k],))
                            pend.append((ci, k))

                        def pv():
                            ci, k = pend.pop(0)
                            m = chunks[ci]
                            pslot = ci % 4
                            P.act(pp[:, pslot, 0:N], pS[k][:, 0:N], AF.Exp, (b_pS[k],), (b_pp[pslot],), scale=MLA_SCALE)
                            for sub in range(nt):
                                P.mm(pO[u][:, sub * 65:(sub + 1) * 65], pp[:, pslot, sub * 128:(sub + 1) * 128],
                                     vms[:, m, h * 65:(h + 1) * 65], ci == 0 and sub == 0,
                                     ci == nch - 1 and sub == nt - 1,
                                     (b_pp[pslot], b_kv), (b_pO[u],))

                        LOOK = 2
                        for ci in range(nch):
                            qk(ci)
                            if len(pend) > LOOK:
                                pv()
                        while pend:
                            pv()
                        o4 = pO[u][:, 0:nt * 65].rearrange("p (s d) -> p s d", d=65)
                        P.add("dve", lambda e, o4=o4, u=u, nt=nt: e.reciprocal(rec[:, u, 0:nt], o4[:, :, 64]),
                              (b_pO[u],), (b_rec[u],))
                        for sub in range(nt):
                            P.ts("dve", yd[:, gs, sub, h * 64:(h + 1) * 64], pO[u][:, sub * 65:sub * 65 + 64],
                                 rec[:, u, sub:sub + 1], None, ALU.mult, None, (b_pO[u], b_rec[u]), (b_yd[gs],),
                                 nowaw=not (h == 0 and sub == 0))
                    for sub, t in enumerate(tiles):
                        P.dma("sp", Y[t * 128:(t + 1) * 128, 768:1024], yd[:, gs, sub, :], b_yd[gs], (b_yd[gs],), ())
                P.end()
        def s6(l, last):
            with ExitStack() as st:
                wout = sb(st, "wout", [128, KC, D], BF16)
                wr = sb(st, "wr", [128, KC, 36], BF16)
                brt = sb(st, "brt", [128, 36])
                outg = sb(st, "outg", [128, KC])
                g1_bc = sb(st, "g1_bc", [128, 2, D])
                gm2_bc = sb(st, "gm2_bc", [128, 2, D])
                sh2_bc = sb(st, "sh2_bc", [128, 2, D])
                yt = sb(st, "yt", [128, 2, D])
                junk = sb(st, "junk", [128, D], BF16)
                ynb = sb(st, "ynb", [128, 2, D], BF16)
                ynT = sb(st, "ynT", [128, 2, KC, 128], BF16)
                xt = sb(st, "xt", [128, 2, D])
                xnew = sb(st, "xnew", [128, 2, D])
                tmp = sb(st, "tmp", [128, D])
                h2b = sb(st, "h2b", [128, 2, D], BF16)
                h2T = sb(st, "h2T", [128, 2, KC, 128], BF16)
                stat = sb(st, "stat", [128, 2, 16])
                lg = sb(st, "lg", [128, 2, 36])
                rtmp = sb(st, "rtmp", [128, 2, 4, NE])
                amask = sb(st, "amask", [128, 2, NE], BF16)
                pA = [ps(st, "pA%d" % i, [128, 1024], BF16) for i in range(2)]
                pO = [ps(st, "pO%d" % i, [128, 2, 512]) for i in range(2)]
                pR = [ps(st, "pR%d" % i, [128, 512]) for i in range(2)]
                b_w = P.buf("w")
                b_yt, b_ynb, b_ynT, b_xt = P.bufs("yt", 2), P.bufs("ynb", 2), P.bufs("ynT", 2), P.bufs("xt", 2)
                b_xnew, b_h2b, b_h2T = P.bufs("xnew", 2), P.bufs("h2b", 2), P.bufs("h2T", 2)
                b_stat, b_lg, b_rtmp, b_am = P.bufs("stat", 2), P.bufs("lg", 2), P.bufs("rtmp", 2), P.bufs("am", 2)
                b_junk, b_tmp = P.buf("junk"), P.buf("tmp")
                b_pA, b_pO, b_pR = P.bufs("pA", 2), P.bufs("pO", 2), P.bufs("pR", 2)
                b_rout, b_carry = P.buf("rout"), P.buf("carry")
                for c in range(KC):
                    P.dma("pool", wout[:, c, :], w_out[l][c * 128:(c + 1) * 128, :], b_w, (), (b_w,), nowaw=True)
                P.dma("pool", wr[:, :, 0:4], w_grp[l].rearrange("(c p) n -> p c n", p=128), b_w, (), (b_w,), nowaw=True)
                P.dma("pool", wr[:, :, 4:36], w_exp[l].rearrange("(c p) n -> p c n", p=128), b_w, (), (b_w,), nowaw=True)
                P.dma("act", brt[:, 0:4], b_grp[l:l + 1, :].partition_broadcast(128), b_w, (), (b_w,), nowaw=True)
                P.dma("act", brt[:, 4:36], b_exp[l:l + 1, :].partition_broadcast(128), b_w, (), (b_w,), nowaw=True)
                P.dma("act", outg[:], out_norm_g[l].rearrange("(c p) -> p c", p=128), b_w, (), (b_w,), nowaw=True,
                      slow=True)
                load_bc("act", g1_bc, 2, b_w)
                load_bc("act", gm2_bc, 4, b_w)
                load_bc("act", sh2_bc, 3, b_w)
                P.memset("dve", carry[:], 0.0, (b_carry,))
                tiles = [(t, 0) for t in range(NTL)] + ([] if last else [(t, 1) for t in range(NTL, NTT)])
                def phaseA(ti, t, v):
                    s = ti % 2
                    rows = slice(t * 128, (t + 1) * 128)
                    P.dma("sp", yt[:, s, :], Y[rows, :], b_yt[s], (), (b_yt[s],))
                    P.dma("sp", xt[:, s, :], x_src(l, t), b_xt[s], (), (b_xt[s],))
                    for g in range(4):
                        P.act(junk[:, g * 256:(g + 1) * 256], yt[:, s, g * 256:(g + 1) * 256], AF.Square,
                              (b_yt[s],), (b_junk, b_stat[s]), accum=stat[:, s, g:g + 1])
                    rms_rstd(stat[:, s, 0:4], stat[:, s, 4:8], 256, (b_stat[s],), (b_stat[s],))
                    for g in range(4):
                        P.act(ynb[:, s, g * 256:(g + 1) * 256], yt[:, s, g * 256:(g + 1) * 256], AF.Copy,
                              (b_yt[s], b_stat[s]), (b_ynb[s],), scale=stat[:, s, 4 + g:5 + g], nowaw=(g > 0))
                    for c in range(KC):
                        P.tr(pA[s][:, c * 128:(c + 1) * 128], ynb[:, s, c * 128:(c + 1) * 128], ident_b,
                             (b_ynb[s],), (b_pA[s],), nowaw=(c > 0))
                    for c in range(KC):
                        P.ts("dve", ynT[:, s, c, :], pA[s][:, c * 128:(c + 1) * 128],
                             outg[:, c:c + 1], None, ALU.mult, None, (b_pA[s], b_w), (b_ynT[s],), nowaw=(c > 0))
                    for half in range(2):
                        for c in range(KC):
                            P.mm(pO[s][:, half, :], ynT[:, s, c, :], wout[:, c, half * 512:(half + 1) * 512],
                                 c == 0, c == KC - 1, (b_ynT[s], b_w), (b_pO[s],))
                    P.tt("dve", tmp[:], pO[s][:].rearrange("p a b -> p (a b)"), g1_bc[:, v, :], ALU.mult,
                         (b_pO[s], b_w), (b_tmp,))
                    P.tt("pool", xnew[:, s, :], tmp[:], xt[:, s, :], ALU.add, (b_tmp, b_xt[s]), (b_xnew[s],))
                    P.dma("sp", X[rows, :], xnew[:, s, :], b_xnew[s], (b_xnew[s],), ())
                    P.act(junk[:], xnew[:, s, :], AF.Square, (b_xnew[s],), (b_junk, b_stat[s]), accum=stat[:, s, 8:9])
                    rms_rstd(stat[:, s, 8:9], stat[:, s, 9:10], D, (b_stat[s],), (b_stat[s],))
                    P.stt("dve", tmp[:], xnew[:, s, :], stat[:, s, 9:10], gm2_bc[:, v, :], ALU.mult, ALU.mult,
                          (b_xnew[s], b_stat[s], b_w), (b_tmp,))
                    P.tt("pool", h2b[:, s, :], tmp[:], sh2_bc[:, v, :], ALU.add, (b_tmp, b_w), (b_h2b[s],))
                    P.dma("sp", H2[rows, :], h2b[:, s, :], b_h2b[s], (b_h2b[s],), ())
                    for c in range(KC):
                        P.tr(pA[s][:, c * 128:(c + 1) * 128], h2b[:, s, c * 128:(c + 1) * 128], ident_b,
                             (b_h2b[s],), (b_pA[s],), nowaw=(c > 0))
                    P.cp("act", h2T[:, s].rearrange("p c q -> p (c q)"), pA[s][:, :], (b_pA[s],), (b_h2T[s],))
                    for c in range(KC):
                        P.mm(pR[s][:, 0:36], h2T[:, s, c, :], wr[:, c, :], c == 0, c == KC - 1,
                             (b_h2T[s], b_w), (b_pR[s],))
                    LG = lg[:, s, :]
                    P.tt("dve", LG, pR[s][:, 0:36], brt[:], ALU.add, (b_pR[s], b_w), (b_lg[s],))

                def phaseB(ti, t, v):
                    s = ti % 2
                    R_, W_ = (b_lg[s], b_stat[s], b_rtmp[s]), (b_stat[s], b_rtmp[s])
                    sm = stat[:, s, 10:11]
                    P.red("dve", sm, lg[:, s, 0:4], ALU.max, R_, W_)
                    goh = rtmp[:, s, 0, 0:4]
                    P.ts("dve", goh, lg[:, s, 0:4], sm, None, ALU.is_ge, None, R_, W_)
                    P.ts("dve", stat[:, s, 11:12], sm, -1.0, None, ALU.mult, None, R_, W_)
                    P.act(rtmp[:, s, 0, 4:8], lg[:, s, 0:4], AF.Exp, R_, W_, bias=stat[:, s, 11:12],
                          accum=stat[:, s, 12:13])
                    pg = stat[:, s, 13:14]
                    P.add("dve", lambda e, pg=pg, s=s: e.reciprocal(pg, stat[:, s, 12:13]), R_, W_)
                    ml = rtmp[:, s, 1, :]
                    pen = rtmp[:, s, 2, :]
                    P.ts("dve", pen.rearrange("p (g e) -> p g e", g=4),
                         goh.unsqueeze(2).to_broadcast([128, 4, 8]), -1.0, 1.0e4, ALU.add, ALU.mult, R_, W_)
                    P.tt("dve", ml, lg[:, s, 4:36], pen, ALU.add, R_, W_)
                    v1 = stat[:, s, 14:15]
                    v2 = stat[:, s, 15:16]
                    P.red("dve", v1, ml, ALU.max, R_, W_)
                    m1 = mask1[:, t, :]
                    m2 = mask2[:, t, :]
                    RW = W_ + (b_rout,)
                    P.ts("dve", m1, ml, v1, None, ALU.is_ge, None, R_, RW)
                    P.stt("dve", ml, m1, -1.0e4, ml, ALU.mult, ALU.add, R_ + (b_rout,), W_)
                    P.red("dve", v2, ml, ALU.max, R_, W_)
                    P.ts("dve", m2, ml, v2, None, ALU.is_ge, None, R_, RW)
                    dv = stat[:, s, 11:12]
                    P.tt("dve", dv, v2, v1, ALU.subtract, R_, W_)
                    P.act(dv, dv, AF.Exp, R_, W_)
                    P.ts("dve", dv, dv, 1.0, None, ALU.add, None, R_, W_)
                    P.add("dve", lambda e, dv=dv: e.reciprocal(dv, dv), R_, W_)
                    P.tt("dve", gw12[:, 0, t:t + 1], dv, pg, ALU.mult, R_, RW)
                    P.tt("dve", gw12[:, 1, t:t + 1], pg, gw12[:, 0, t:t + 1], ALU.subtract, R_ + (b_rout,), RW)
                    P.tt("dve", amask[:, s, :], m1, m2, ALU.add, (b_rout,), (b_am[s],))
                    P.mm(pR[s][:, 64:96], triu_b, amask[:, s, :], True, True, (b_am[s],), (b_pR[s],))
                    P.mm(pR[s][:, 128:160], ones_b, amask[:, s, :], True, True, (b_am[s],), (b_pR[s],))
                    rk = rtmp[:, s, 3, :]
                    P.tt("dve", rk, pR[s][:, 64:96], carry[:], ALU.add, (b_pR[s], b_carry) + R_, W_)
                    P.tt("dve", carry[:], carry[:], pR[s][:, 128:160], ALU.add, (b_pR[s], b_carry), (b_carry,))
                    for k, mk in ((0, m1), (1, m2)):
                        P.tt("dve", pen, rk, mk, ALU.mult, R_ + (b_rout,), W_)
                        P.red("dve", rank12[:, k, t:t + 1], pen, ALU.add, R_, RW)
                for ti, (t, v) in enumerate(tiles):
                    phaseA(ti, t, v)
                    phaseB(ti, t, v)
                P.end()

        def s7(l, last):
            with ExitStack() as st:
                pad = sb(st, "pad", [128, NE])
                pend = sb(st, "pend", [128, NE])
                pstart = sb(st, "pstart", [128, NE])
                big = sb(st, "big", [128, NTT, NE])
                destf = sb(st, "destf", [128, 2, NTT])
                cmp_ = sb(st, "cmp", [128, NBLK, NE])
                ebf = sb(st, "ebf", [128, NBLK])
                wif = sb(st, "wif", [128, 2, NBLK])
                h2r = sb(st, "h2r", [128, 3, D], BF16)
                b_p, b_big, b_dest, b_cmp, b_eb = P.buf("p"), P.buf("big"), P.buf("dest"), P.buf("cmp"), P.buf("eb")
                b_h2r = P.bufs("h2r", 3)
                JJ = (2 * NT) // BLK + 1
                cmpp = sb(st, "cmpp", [128, NE, JJ])
                P.tt("dve", cmpp[:], carry[:].unsqueeze(2).to_broadcast([128, NE, JJ]),
                     blkstart[:, 0:JJ].unsqueeze(1).to_broadcast([128, NE, JJ]), ALU.is_gt, (), (b_p,))
                P.red("dve", pad[:], cmpp[:], ALU.add, (b_p,), (b_p,))
                P.ts("dve", pad[:], pad[:], float(BLK), None, ALU.mult, None, (b_p,), (b_p,))
                P.cp("dve", pend[:, 0:1], pad[:, 0:1], (b_p,), (b_p,))
                for e_ in range(1, NE):
                    P.tt("dve", pend[:, e_:e_ + 1], pend[:, e_ - 1:e_], pad[:, e_:e_ + 1], ALU.add, (b_p,), (b_p,))
                P.tt("dve", pstart[:], pend[:], pad[:], ALU.subtract, (b_p,), (b_p,))
                ntt_used = NTL if last else NTT
                for k, mk in ((0, mask1), (1, mask2)):
                    P.tt("dve", big[:, 0:ntt_used, :], mk[:, 0:ntt_used, :],
                         pstart[:].unsqueeze(1).to_broadcast([128, ntt_used, NE]), ALU.mult, (b_p,), (b_big,))
                    P.red("dve", destf[:, k, 0:ntt_used], big[:, 0:ntt_used, :], ALU.add, (b_big,), (b_dest,))
                    P.tt("dve", destf[:, k, 0:ntt_used], destf[:, k, 0:ntt_used], rank12[:, k, 0:ntt_used], ALU.add,
                         (b_dest,), (b_dest,))
                P.cp("dve", dest_i[:, :, 0:ntt_used], destf[:, :, 0:ntt_used], (b_dest,), (b_dest,))
                P.tt("dve", cmp_[:], pend[:].unsqueeze(1).to_broadcast([128, NBLK, NE]),
                     blkstart[:, 0:NBLK].unsqueeze(2).to_broadcast([128, NBLK, NE]), ALU.is_le, (b_p,), (b_cmp,))
                P.red("dve", ebf[:], cmp_[:], ALU.add, (b_cmp,), (b_eb,))
                P.ts("dve", ebf[:], ebf[:], float(NE - 1), None, ALU.min, None, (b_eb,), (b_eb,))
                P.ts("dve", wif[:, 0, :], ebf[:], float(D), iota_p, ALU.mult, ALU.add, (b_eb,), (b_eb,))
                P.ts("dve", wif[:, 1, :], ebf[:], float(DE), iota_p, ALU.mult, ALU.add, (b_eb,), (b_eb,))
                P.cp("dve", widx[:], wif[:], (b_eb,), (b_eb,))
                for t in range(ntt_used):
                    s = t % 3
                    P.dma("sp", h2r[:, s, :], H2[t * 128:(t + 1) * 128, :], b_h2r[s], (), (b_h2r[s],))
                    for k in range(2):
                        P.scatter(XBUF, dest_i[:, k, t:t + 1], h2r[:, s, :], b_h2r[s], (b_h2r[s], b_dest), ())
                P.end()

        def s8(l, last):
            with ExitStack() as st:
                wg = sb(st, "wg", [128, 3, KC, DE], BF16)
                wu = sb(st, "wu", [128, 3, KC, DE], BF16)
                wd = sb(st, "wd", [128, 3, 4, D], BF16)
                xr = sb(st, "xr", [128, 2, 4, D], BF16)
                xT = sb(st, "xT", [128, 2, KC, 512], BF16)
                sg_ = sb(st, "sg", [128, 2, 512])
                hid = sb(st, "hid", [128, 2, 4, 512], BF16)
                yo = sb(st, "yo", [128, 2, D])
                pA = [ps(st, "pA%d" % i, [128, 1024], BF16) for i in range(2)]
                pG = [ps(st, "pG%d" % i, [128, 512]) for i in range(4)]
                pY = [ps(st, "pY%d" % i, [128, 512]) for i in range(2)]
                b_wg, b_wu, b_wd = P.bufs("wg", 3), P.bufs("wu", 3), P.bufs("wd", 3)
                b_xr, b_xT, b_sg, b_hid, b_yo = P.bufs("xr", 2), P.bufs("xT", 2), P.bufs("sg", 2), P.bufs("hid", 2), P.bufs("yo", 2)
                b_pA, b_pG, b_pY = P.bufs("pA", 2), P.bufs("pG", 4), P.bufs("pY", 2)
                nblk = NBLK
                pai = 0
                pgi = 0
                yi = 0
                for b in range(nblk):
                    s = b % 2
                    ws = b % 3
                    for c in range(KC):
                        P.gather(wg[:, ws, c, :], w_gate, widx[:, 0, b:b + 1], (l * NE * D + c * 128) * DE,
                                 b_wg[ws], (), (b_wg[ws],), nowaw=True)
                    for c in range(KC):
                        P.gather(wu[:, ws, c, :], w_up, widx[:, 0, b:b + 1], (l * NE * D + c * 128) * DE,
                                 b_wu[ws], (), (b_wu[ws],), nowaw=True)
                    for f in range(4):
                        P.gather(wd[:, ws, f, :], w_down, widx[:, 1, b:b + 1], (l * NE * DE + f * 128) * D,
                                 b_wd[ws], (), (b_wd[ws],), nowaw=True)
                    P.dma("sp", xr[:, s], XBUF[b * BLK:(b + 1) * BLK, :].rearrange("(i p) d -> p i d", p=128),
                          b_xr[s], (), (b_xr[s],))
                    for c in range(KC):
                        a = pai % 2
                        pai += 1
                        for i in range(4):
                            P.tr(pA[a][:, i * 128:(i + 1) * 128], xr[:, s, i, c * 128:(c + 1) * 128], ident_b,
                                 (b_xr[s],), (b_pA[a],), nowaw=(i > 0))
                        P.cp("act" if c % 2 else "dve", xT[:, s, c, :], pA[a][:, 0:512], (b_pA[a],), (b_xT[s],),
                             nowaw=(c > 0))
                    for f in range(4):
                        g0 = pgi % 4
                        g1 = (pgi + 1) % 4
                        pgi += 2
                        for c in range(KC):
                            P.mm(pG[g0][:, :], wg[:, ws, c, f * 128:(f + 1) * 128], xT[:, s, c, :], c == 0, c == KC - 1,
                                 (b_wg[ws], b_xT[s]), (b_pG[g0],))
                        for c in range(KC):
                            P.mm(pG[g1][:, :], wu[:, ws, c, f * 128:(f + 1) * 128], xT[:, s, c, :], c == 0, c == KC - 1,
                                 (b_wu[ws], b_xT[s]), (b_pG[g1],))
                        fs = f % 2
                        P.act(sg_[:, fs, :], pG[g0][:, :], AF.Silu, (b_pG[g0],), (b_sg[fs],))
                        P.tt("dve", hid[:, s, f, :], sg_[:, fs, :], pG[g1][:, :], ALU.mult, (b_sg[fs], b_pG[g1]),
                             (b_hid[s],), nowaw=(f > 0))
                    for i in range(4):
                        ys = yi % 2
                        yi += 1
                        for half in range(2):
                            for f in range(4):
                                P.mm(pY[half][:, :], hid[:, s, f, i * 128:(i + 1) * 128],
                                     wd[:, ws, f, half * 512:(half + 1) * 512], f == 0, f == 3,
                                     (b_hid[s], b_wd[ws]), (b_pY[half],))
                            P.cp("act" if half else "dve", yo[:, ys, half * 512:(half + 1) * 512], pY[half][:, :],
                                 (b_pY[half],), (b_yo[ys],), nowaw=(half > 0))
                        P.dma("sp", YBUF[b * BLK + i * 128:b * BLK + (i + 1) * 128, :], yo[:, ys, :], b_yo[ys],
                              (b_yo[ys],), ())
                P.end()

        def s9(l, last):
            with ExitStack() as st:
                g2_bc = sb(st, "g2_bc", [128, 2, D])
                fg_bc = sb(st, "fg_bc", [128, D])
                y1 = sb(st, "y1", [128, 4, D])
                y2 = sb(st, "y2", [128, 4, D])
                xt = sb(st, "xt", [128, 4, D])
                f1 = sb(st, "f1", [128, D])
                xo = sb(st, "xo", [128, 4, D])
                junk = sb(st, "junk", [128, D], BF16)
                stat = sb(st, "stat", [128, 4, 2])
                b_w = P.buf("w")
                b_y1, b_y2, b_xt, b_xo, b_stat = P.bufs("y1", 4), P.bufs("y2", 4), P.bufs("xt", 4), P.bufs("xo", 4), P.bufs("stat", 4)
                b_f1, b_junk = P.buf("f1"), P.buf("junk")
                load_bc("act", g2_bc, 5, b_w)
                if last:
                    P.dma("act", fg_bc[:], final_g.partition_broadcast(128), b_w, (), (b_w,), nowaw=True)
                tiles = [(t, 0) for t in range(NTL)] + ([] if last else [(t, 1) for t in range(NTL, NTT)])
                for ti, (t, v) in enumerate(tiles):
                    s = ti % 4
                    rows = slice(t * 128, (t + 1) * 128)
                    P.gather(y1[:, s, :], YBUF, dest_i[:, 0, t:t + 1], 0, b_y1[s], (), (b_y1[s],))
                    P.gather(y2[:, s, :], YBUF, dest_i[:, 1, t:t + 1], 0, b_y2[s], (), (b_y2[s],))
                    P.dma("sp", xt[:, s, :], X[rows, :], b_xt[s], (), (b_xt[s],))
                    P.ts("dve", f1[:], y1[:, s, :], gw12[:, 0, t:t + 1], None, ALU.mult, None, (b_y1[s],), (b_f1,))
                    P.stt("dve", f1[:], y2[:, s, :], gw12[:, 1, t:t + 1], f1[:], ALU.mult, ALU.add,
                          (b_y2[s], b_f1), (b_f1,))
                    P.tt("dve", f1[:], f1[:], g2_bc[:, v, :], ALU.mult, (b_f1, b_w), (b_f1,))
                    P.tt("dve", xo[:, s, :], f1[:], xt[:, s, :], ALU.add, (b_f1, b_xt[s]), (b_xo[s],))
                    if not last:
                        P.dma("act", X[rows, :], xo[:, s, :], b_xo[s], (b_xo[s],), ())
                    else:
                        P.act(junk[:], xo[:, s, :], AF.Square, (b_xo[s],), (b_junk, b_stat[s]), accum=stat[:, s, 0:1])
                        rms_rstd(stat[:, s, 0:1], stat[:, s, 1:2], D, (b_stat[s],), (b_stat[s],))
                        P.stt("dve", xo[:, s, :], xo[:, s, :], stat[:, s, 1:2], fg_bc[:], ALU.mult, ALU.mult,
                              (b_xo[s], b_stat[s], b_w), (b_xo[s],))
                        P.dma("act", out[rows, :], xo[:, s, :], b_xo[s], (b_xo[s],), ())
                P.end()

        stages = [s1, s2, s3, s4, s5, s6, s7, s8, s9]
        s0()
        done = cfg.stop is not None and cfg.stop[1] == 0
        for l in range(L):
            if done:
                break
            last = (l == L - 1)
            for si_, fn in enumerate(stages):
                if si_ == 0:
                    fn(l)
                else:
                    fn(l, last)
                if cfg.stop is not None and cfg.stop == (l, si_ + 1):
                    done = True
                    break
            if done:
                break
    return nc


def make_consts(cfg):
    cf = np.zeros((128, 353), np.float32)
    cf[:, 0:128] = np.eye(128, dtype=np.float32)
    cf[:, 128:256] = 1.0
    cf[:, 256:288] = np.arange(32, dtype=np.float32)[None, :]
    cf[:, 288] = np.arange(128, dtype=np.float32)
    cf[:, 289:353] = (np.arange(64, dtype=np.float32) * BLK)[None, :]
    cbm = np.zeros((128, 384), np.float32)
    cbm[:, 0:128] = np.eye(128)
    cbm[:, 128:256] = 1.0
    cbm[:, 256:384] = np.triu(np.ones((128, 128), np.float32), k=1)
    return cf, cbm.astype(ml_dtypes.bfloat16)


def core_inputs(cfg, inputs, b):
    L = cfg.L
    f = lambda a: np.ascontiguousarray(np.asarray(a, dtype=np.float32))
    cosT, sinT = rope_tables(cfg.NL, cfg.NCX)
    cf, cbm = make_consts(cfg)
    m = {
        "x": f(inputs["x"][b]),
        "ctx": f(inputs["ctx"][b]),
        "cc": f(np.stack([np.asarray(inputs["c"][b]), np.asarray(inputs["c_ctx"])], axis=0)),
        "na_bias": na_bias_tables(np.asarray(inputs["na_rpb"], dtype=np.float32)[:L], cfg.variants),
        "w_gate": f(inputs["w_gate"][:L]).reshape(L * NE * D, DE),
        "w_up": f(inputs["w_up"][:L]).reshape(L * NE * D, DE),
        "w_down": f(inputs["w_down"][:L]).reshape(L * NE * DE, D),
        "final_norm_g": f(inputs["final_norm_g"]).reshape(1, D),
        "cosT": cosT, "sinT": sinT, "consts_f": cf, "consts_b": cbm,
    }
    for k in ("w_ada", "b_ada", "norm1_g", "norm2_g", "w_in", "conv_w", "sg_w", "sg_b", "mla_q_norm_g",
              "mla_w_uq", "mla_kv_norm_g", "mla_w_ukv", "out_norm_g", "w_out", "w_grp", "b_grp", "w_exp", "b_exp"):
        m[k] = f(inputs[k][:L])
    return m


_NC_CACHE = {}


def kernel(**inputs):
    cfg = Cfg()
    if "nc" not in _NC_CACHE:
        _NC_CACHE["nc"] = build(cfg)
    nc = _NC_CACHE["nc"]
    per_b = [core_inputs(cfg, inputs, b) for b in range(4)]
    in_maps = [per_b[i % 4] for i in range(8)]
    res = run_bass_kernel_spmd(nc, in_maps, core_ids=list(range(8)))
    return np.stack([np.asarray(res.results[b]["out"], dtype=np.float32) for b in range(4)], axis=0)
```
